# Optimizing a Trainium2 kernel written in Bass

```python
import math
import jax, jax.numpy as jnp
from jax import lax
import numpy as np

D_MODEL = 1024
BATCH = 32
SEQ = 2048
DEPTH = 1

D_MIX = D_MODEL
DIFF_HEADS = 4
DIFF_QK_DIM = 64
DIFF_V_DIM = 2 * DIFF_QK_DIM
DIFF_WIDTH = DIFF_HEADS * DIFF_V_DIM
DSA_HEADS = 4
DSA_LATENT = 128
DSA_V_DIM = 128
DSA_WIDTH = DSA_HEADS * DSA_V_DIM
IDX_HEADS = 8
IDX_DIM = 64
TOPK_MAX = 256
REL_BUCKETS = 32
REL_MAX_DIST = 128
N_ATTN_HEADS = DIFF_HEADS + DSA_HEADS
PEER_HEADS = 8
PEER_NKEYS = 128
PEER_QDIM = 128
PEER_HALF = PEER_QDIM // 2
PEER_TOPK = 16
N_EXPERTS = PEER_NKEYS * PEER_NKEYS
PEER_CHUNK = 128
Q_BLOCK = 128
RMS_EPS = 1e-6
D_IN_PROJ = (2 * DIFF_HEADS * 2 * DIFF_QK_DIM + DIFF_WIDTH + DSA_HEADS * DSA_LATENT
             + DSA_LATENT + IDX_HEADS * IDX_DIM + IDX_DIM + IDX_HEADS)

kernel_name = "hymba_diffattn_dsa_peer_block"


def rms_norm(x, g):
    xf = x.astype(jnp.float32)
    y = xf * lax.rsqrt(jnp.mean(xf * xf, axis=-1, keepdims=True) + RMS_EPS)
    return (y * g.astype(jnp.float32)).astype(x.dtype)


def rel_bucket(dist):
    n = jnp.maximum(dist, 0)
    max_exact = REL_BUCKETS // 2
    nf = jnp.maximum(n, max_exact).astype(jnp.float32)
    large = max_exact + (jnp.log(nf / max_exact) / math.log(REL_MAX_DIST / max_exact)
                         * (REL_BUCKETS - max_exact)).astype(jnp.int32)
    large = jnp.minimum(large, REL_BUCKETS - 1)
    return jnp.where(n < max_exact, n, large)


def to_blocks(a):
    B, S = a.shape[0], a.shape[1]
    a = a.reshape((B, S // Q_BLOCK, Q_BLOCK) + a.shape[2:])
    return jnp.moveaxis(a, 1, 0)


def from_blocks(a):
    a = jnp.moveaxis(a, 0, 1)
    return a.reshape((a.shape[0], a.shape[1] * a.shape[2]) + a.shape[3:])


def diff_attention(q1, q2, k1, k2, v, lam, bias_table):
    S = q1.shape[1]
    nb = S // Q_BLOCK
    scale = DIFF_QK_DIM ** -0.5
    s_pos = jnp.arange(S, dtype=jnp.int32)

    def block(args):
        q1b, q2b, blk = args
        t_pos = blk * Q_BLOCK + jnp.arange(Q_BLOCK, dtype=jnp.int32)
        dist = t_pos[:, None] - s_pos[None, :]
        bias = jnp.transpose(bias_table[rel_bucket(dist)], (2, 0, 1)).astype(jnp.float32)
        mask = dist >= 0
        l1 = jnp.einsum('bqhd,bshd->bhqs', q1b, k1).astype(jnp.float32) * scale + bias
        l2 = jnp.einsum('bqhd,bshd->bhqs', q2b, k2).astype(jnp.float32) * scale + bias
        p1 = jax.nn.softmax(jnp.where(mask, l1, -jnp.inf), axis=-1)
        p2 = jax.nn.softmax(jnp.where(mask, l2, -jnp.inf), axis=-1)
        attn = (p1 - lam * p2).astype(v.dtype)
        return jnp.einsum('bhqs,bshd->bqhd', attn, v)

    out = lax.map(block, (to_blocks(q1), to_blocks(q2), jnp.arange(nb, dtype=jnp.int32)))
    return from_blocks(out)


def dsa_attention(q, kv, iq, ik, iw, w_uv, bias_table):
    S = q.shape[1]
    nb = S // Q_BLOCK
    k_top = min(TOPK_MAX, S // 4)
    scale = DSA_LATENT ** -0.5
    idx_scale = IDX_DIM ** -0.5
    s_pos = jnp.arange(S, dtype=jnp.int32)
    gather_rows = jax.vmap(lambda a, i: a[i])

    def block(args):
        qb, iqb, iwb, blk = args
        t_pos = blk * Q_BLOCK + jnp.arange(Q_BLOCK, dtype=jnp.int32)
        dots = jax.nn.relu(jnp.einsum('bqhd,bsd->bqhs', iqb, ik).astype(jnp.float32) * idx_scale)
        score = jnp.einsum('bqhs,bqh->bqs', dots, iwb.astype(jnp.float32))
        score = jnp.where(s_pos[None, None, :] <= t_pos[None, :, None], score, -jnp.inf)
        _, sel = lax.top_k(score, k_top)
        dist = t_pos[None, :, None] - sel
        valid = dist >= 0
        kv_sel = gather_rows(kv, sel)
        bias = jnp.transpose(bias_table[rel_bucket(dist)], (0, 3, 1, 2)).astype(jnp.float32)
        logits = jnp.einsum('bqhd,bqkd->bhqk', qb, kv_sel).astype(jnp.float32) * scale + bias
        p = jax.nn.softmax(jnp.where(valid[:, None], logits, -jnp.inf), axis=-1).astype(kv.dtype)
        o = jnp.einsum('bhqk,bqkd->bqhd', p, kv_sel)
        return jnp.einsum('bqhc,hcd->bqhd', o, w_uv)

    out = lax.map(block, (to_blocks(q), to_blocks(iq), to_blocks(iw), jnp.arange(nb, dtype=jnp.int32)))
    return from_blocks(out)


def peer(h, w_q, keys, u_tab, v_tab):
    B, S, D = h.shape
    T = B * S
    xc = h.reshape(T // PEER_CHUNK, PEER_CHUNK, D)

    def chunk(xt):
        q = (xt @ w_q).reshape(PEER_CHUNK, PEER_HEADS, 2, PEER_HALF)
        sc = jnp.einsum('thpd,hpnd->thpn', q, keys).astype(jnp.float32)
        s1, i1 = lax.top_k(sc[:, :, 0], PEER_TOPK)
        s2, i2 = lax.top_k(sc[:, :, 1], PEER_TOPK)
        cand = (s1[..., :, None] + s2[..., None, :]).reshape(PEER_CHUNK, PEER_HEADS, PEER_TOPK * PEER_TOPK)
        cand_idx = (i1[..., :, None] * PEER_NKEYS + i2[..., None, :]).reshape(PEER_CHUNK, PEER_HEADS, PEER_TOPK * PEER_TOPK)
        top, pos = lax.top_k(cand, PEER_TOPK)
        e_idx = jnp.take_along_axis(cand_idx, pos, axis=-1)
        g = jax.nn.softmax(top, axis=-1)
        u_sel = u_tab[e_idx]
        v_sel = v_tab[e_idx]
        act = jax.nn.gelu(jnp.einsum('td,thkd->thk', xt, u_sel).astype(jnp.float32), approximate=False)
        return jnp.einsum('thk,thkd->td', (g * act).astype(xt.dtype), v_sel)

    return lax.map(chunk, xc).reshape(B, S, D)


def setup_inputs(seed: int = 0) -> dict:
    key = jax.random.key(seed)
    ks = jax.random.split(key, 24)
    f = jnp.float32
    D = D_MODEL
    nrm = lambda k, shape, s: jax.random.normal(k, shape, f) * s
    return {
        "x": nrm(ks[0], (BATCH, SEQ, D), 1.0),
        "c": nrm(ks[1], (BATCH, D), 1.0),
        "w_ada": nrm(ks[2], (DEPTH, D, 6 * D), 0.5 * D ** -0.5),
        "b_ada": nrm(ks[3], (DEPTH, 6 * D), 0.01),
        "g_norm_mix": 1.0 + nrm(ks[4], (DEPTH, D), 0.02),
        "w_in": nrm(ks[5], (DEPTH, D, D_IN_PROJ), D ** -0.5),
        "lam_q1": nrm(ks[6], (DEPTH, DIFF_QK_DIM), 0.1),
        "lam_k1": nrm(ks[7], (DEPTH, DIFF_QK_DIM), 0.1),
        "lam_q2": nrm(ks[8], (DEPTH, DIFF_QK_DIM), 0.1),
        "lam_k2": nrm(ks[9], (DEPTH, DIFF_QK_DIM), 0.1),
        "g_subln": 1.0 + nrm(ks[10], (DEPTH, DIFF_V_DIM), 0.02),
        "g_kv_norm": 1.0 + nrm(ks[11], (DEPTH, DSA_LATENT), 0.02),
        "w_uv": nrm(ks[12], (DEPTH, DSA_HEADS, DSA_LATENT, DSA_V_DIM), DSA_LATENT ** -0.5),
        "w_out": nrm(ks[13], (DEPTH, D_MIX, D), D_MIX ** -0.5),
        "g_norm_ffn": 1.0 + nrm(ks[14], (DEPTH, D), 0.02),
        "w_peer_q": nrm(ks[15], (DEPTH, D, PEER_HEADS * PEER_QDIM), D ** -0.5),
        "peer_keys": nrm(ks[16], (DEPTH, PEER_HEADS, 2, PEER_NKEYS, PEER_HALF), PEER_HALF ** -0.5),
        "peer_u": nrm(ks[17], (DEPTH, N_EXPERTS, D), D ** -0.5),
        "peer_v": nrm(ks[18], (DEPTH, N_EXPERTS, D), 1.0),
        "rel_bias": nrm(ks[19], (REL_BUCKETS, N_ATTN_HEADS), 0.2),
        "g_final": 1.0 + nrm(ks[20], (D,), 0.02),
    }


def reference(x, c, w_ada, b_ada, g_norm_mix, w_in, lam_q1, lam_k1, lam_q2, lam_k2,
              g_subln, g_kv_norm, w_uv, w_out, g_norm_ffn, w_peer_q, peer_keys,
              peer_u, peer_v, rel_bias, g_final):
    B, S, D = x.shape
    sizes = (DIFF_HEADS * 2 * DIFF_QK_DIM, DIFF_HEADS * 2 * DIFF_QK_DIM, DIFF_WIDTH,
             DSA_HEADS * DSA_LATENT, DSA_LATENT, IDX_HEADS * IDX_DIM, IDX_DIM, IDX_HEADS)
    offsets = [int(o) for o in np.cumsum(sizes)[:-1]]
    bias_diff = rel_bias[:, :DIFF_HEADS]
    bias_dsa = rel_bias[:, DIFF_HEADS:]
    c_act = jax.nn.silu(c)

    for l in range(DEPTH):
        mod = c_act @ w_ada[l] + b_ada[l]
        shift_a, scale_a, gate_a, shift_f, scale_f, gate_f = [m[:, None, :] for m in jnp.split(mod, 6, axis=-1)]

        h = rms_norm(x, g_norm_mix[l]) * (1.0 + scale_a) + shift_a
        proj = h @ w_in[l]
        dq, dk, dv, sq, skv, iq, ik, iw = jnp.split(proj, offsets, axis=-1)

        dq = dq.reshape(B, S, DIFF_HEADS, 2, DIFF_QK_DIM)
        dk = dk.reshape(B, S, DIFF_HEADS, 2, DIFF_QK_DIM)
        dv = dv.reshape(B, S, DIFF_HEADS, DIFF_V_DIM)
        lambda_init = 0.8 - 0.6 * math.exp(-0.3 * l)
        lam = (jnp.exp(jnp.sum(lam_q1[l].astype(jnp.float32) * lam_k1[l].astype(jnp.float32)))
               - jnp.exp(jnp.sum(lam_q2[l].astype(jnp.float32) * lam_k2[l].astype(jnp.float32)))
               + lambda_init)
        o_diff = diff_attention(dq[:, :, :, 0], dq[:, :, :, 1], dk[:, :, :, 0], dk[:, :, :, 1], dv, lam, bias_diff)
        o_diff = (rms_norm(o_diff, g_subln[l]) * (1.0 - lambda_init)).reshape(B, S, DIFF_WIDTH)

        sq = sq.reshape(B, S, DSA_HEADS, DSA_LATENT)
        skv = rms_norm(skv, g_kv_norm[l])
        iq = iq.reshape(B, S, IDX_HEADS, IDX_DIM)
        iw = iw * (IDX_HEADS ** -0.5)
        o_dsa = dsa_attention(sq, skv, iq, ik, iw, w_uv[l], bias_dsa).reshape(B, S, DSA_WIDTH)

        mix = jnp.concatenate([o_diff, o_dsa], axis=-1) @ w_out[l]
        x = x + gate_a * mix

        h2 = rms_norm(x, g_norm_ffn[l]) * (1.0 + scale_f) + shift_f
        x = x + gate_f * peer(h2, w_peer_q[l], peer_keys[l], peer_u[l], peer_v[l])

    return rms_norm(x, g_final)
```

```python
import math
import os
import numpy as np
import concourse.bass as bass
import concourse.mybir as mybir
from concourse.bass_utils import run_bass_kernel_spmd

F32 = mybir.dt.float32
BF16 = mybir.dt.bfloat16
ALU = mybir.AluOpType
AF = mybir.ActivationFunctionType
AX = mybir.AxisListType

D = 1024
SEQ = 2048
NT = SEQ // 128
NCORES = 8
NIT = 22
NEG = -1.0e30
WIN_COLS = 2824


class Buf:
    __slots__ = ("name", "w", "r")

    def __init__(self, name=""):
        self.name = name
        self.w = None
        self.r = {}


class Sched:
    def __init__(self, nc):
        self.nc = nc
        self.eng = {"pe": nc.tensor, "act": nc.scalar, "dve": nc.vector,
                    "pool": nc.gpsimd, "sp": nc.sync}
        self.sem = {}
        self.cnt = {}
        for k in ("pe", "act", "dve", "pool", "d_sp", "d_pool", "d_act"):
            self.sem[k] = nc.alloc_semaphore("s_" + k)
            self.cnt[k] = 0
        self.seen = {k: {} for k in self.eng}
        self.n_ins = 0

    def _deps(self, reads, writes):
        deps = {}
        for b in reads:
            if b.w is not None:
                k, v = b.w
                if v > deps.get(k, 0):
                    deps[k] = v
        for b in writes:
            if b.w is not None:
                k, v = b.w
                if v > deps.get(k, 0):
                    deps[k] = v
            for k, v in b.r.items():
                if v > deps.get(k, 0):
                    deps[k] = v
        return deps

    def _wait(self, e, deps):
        eng = self.eng[e]
        seen = self.seen[e]
        for k, v in deps.items():
            if e == "pe" and k == "pe":
                continue
            if v > seen.get(k, 0):
                eng.wait_ge(self.sem[k], v)
                seen[k] = v
                self.n_ins += 1

    def _mark(self, tok, reads, writes):
        k, v = tok
        for b in writes:
            b.w = tok
            b.r = {}
        for b in reads:
            if b in writes:
                continue
            if v > b.r.get(k, 0):
                b.r[k] = v

    def op(self, e, fn, reads=(), writes=()):
        self._wait(e, self._deps(reads, writes))
        ins = fn(self.eng[e])
        self.cnt[e] += 1
        ins.then_inc(self.sem[e], 1)
        self.n_ins += 1
        self._mark((e, self.cnt[e]), reads, writes)
        return ins

    def dma(self, q, out, in_, reads=(), writes=(), key=None, **kw):
        self._wait(q, self._deps(reads, writes))
        ins = self.eng[q].dma_start(out=out, in_=in_, **kw)
        k = "d_" + q
        if key is not None:
            k = "dk_" + key
            if k not in self.sem:
                self.sem[k] = self.nc.alloc_semaphore("s_" + k)
                self.cnt[k] = 0
        self.cnt[k] += 16
        ins.then_inc(self.sem[k], 16)
        self.n_ins += 1
        self._mark((k, self.cnt[k]), reads, writes)
        return ins

    def barrier(self):
        allv = {k: v for k, v in self.cnt.items() if v > 0}
        for e in self.eng:
            self._wait(e, allv)


class Arena:
    def __init__(self, nc, S, words):
        self.t = nc.alloc_sbuf_tensor("arena", [128, words], F32)
        self.words = words
        self.top = 0
        self.S = S

    def push(self, shape, dtype, name=""):
        n = 1
        for s in shape[1:]:
            n *= s
        nbytes = n * (4 if dtype == F32 else 2)
        w = (nbytes + 3) // 4
        w = (w + 7) // 8 * 8
        assert self.top + w <= self.words, (name, self.top, w, self.words)
        v = self.t[:, self.top:self.top + w]
        if dtype != F32:
            v = v.bitcast(dtype)
        v = v[:, 0:n]
        if len(shape) == 3:
            v = v.rearrange("p (a b) -> p a b", a=shape[1])
        elif len(shape) == 4:
            v = v.rearrange("p (a b c) -> p a b c", a=shape[1], b=shape[2])
        self.top += w
        return v, Buf(name)

    def mark(self):
        return self.top

    def release(self, m):
        self.S.barrier()
        self.top = m


def rel_bucket_np(n):
    max_exact = 16
    nf = np.maximum(n, max_exact).astype(np.float32)
    large = max_exact + (np.log(nf / max_exact) / math.log(128 / max_exact) * 16).astype(np.int32)
    large = np.minimum(large, 31)
    return np.where(n < max_exact, n, large)


def const_bidx():
    t = np.arange(128)[:, None] + 128
    s = np.arange(256)[None, :]
    d = t - s
    b = rel_bucket_np(np.maximum(d, 0)).astype(np.float32)
    return np.where(d >= 0, b, -1.0).astype(np.float32)


class _Stop(Exception):
    pass


def build(nseq=4, dbg=False, stop=None):
    nc_S = []
    try:
        _build(nseq, stop, nc_S)
    except _Stop:
        pass
    nc, S = nc_S
    S.barrier()
    return nc, S


def _build(nseq, stop, nc_S):
    nc = bass.Bass("TRN2", target_bir_lowering=False)

    def din(name, shape):
        return nc.dram_tensor(name, list(shape), F32, kind="ExternalInput").ap()

    x_d = din("x", [nseq, SEQ, D])
    c_d = din("c", [nseq, D])
    w_ada_d = din("w_ada", [D, 6 * D])
    b_ada_d = din("b_ada", [6 * D])
    g_mix_d = din("g_norm_mix", [D])
    w_in_d = din("w_in", [D, 2760])
    lam_d = [din(n, [64]) for n in ("lam_q1", "lam_k1", "lam_q2", "lam_k2")]
    g_subln_d = din("g_subln", [128])
    g_kv_d = din("g_kv_norm", [128])
    w_uv_d = din("w_uv", [4, 128, 128])
    w_out_d = din("w_out", [D, D])
    g_ffn_d = din("g_norm_ffn", [D])
    w_pq_d = din("w_peer_q", [D, D])
    keys_d = din("peer_keys", [8, 2, 128, 64])
    pu_d = din("peer_u", [16384, D])
    pv_d = din("peer_v", [16384, D])
    relb_d = din("rel_bias", [32, 8])
    g_fin_d = din("g_final", [D])
    bidx_d = din("bidx", [128, 256])
    out_d = nc.dram_tensor("out", [nseq, SEQ, D], F32, kind="ExternalOutput").ap()

    def dscr(name, shape, dt):
        return nc.dram_tensor(name, list(shape), dt, kind="Internal").ap()

    w_in_bf = dscr("w_in_bf", [D, WIN_COLS], BF16)
    w_out_bf = dscr("w_out_bf", [D, D], BF16)
    w_pq_bf = dscr("w_pq_bf", [D, D], BF16)
    UT_d = dscr("UT_d", [128, 128, D], BF16)
    Vb_d = dscr("Vb_d", [16384, D], BF16)
    mod_d = dscr("mod_d", [nseq, 6 * D], F32)
    x1_d = dscr("x1_d", [SEQ, D], F32)
    b_w_in_bf, b_w_out_bf, b_w_pq_bf = Buf(), Buf(), Buf()
    b_UT_d, b_Vb_d, b_mod_d, b_x1_d = Buf(), Buf(), Buf(), Buf()

    S = Sched(nc)
    nc_S.extend([nc, S])
    A = nc.alloc_sbuf_tensor

    def cp(n):
        if stop == n:
            raise _Stop()

    def P_(name, shape, dt):
        return A(name, shape, dt), Buf(name)

    ident, b_ident = P_("ident", [128, 128], BF16)
    w_uvb, b_w_uvb = P_("w_uvb", [128, 4, 128], BF16)
    EB, b_EB = P_("EB", [128, 8, 256], F32)
    negm, b_negm = P_("negm", [128, 128], F32)
    gsub, b_gsub = P_("gsub", [128, 128], F32)
    gkv, b_gkv = P_("gkv", [128, 128], F32)
    keysBD, b_keysBD = P_("keysBD", [128, 8, 256], BF16)
    lamt, b_lamt = P_("lamt", [128, 8], F32)
    pow2, b_pow2 = P_("pow2", [128, NIT], F32)
    st, b_st = P_("st", [128, 64], F32)

    psS = nc.alloc_psum_tensor("psS", [128, 2048], F32)
    psX = nc.alloc_psum_tensor("psX", [128, 1024], F32)
    psO = nc.alloc_psum_tensor("psO", [128, 512], F32)
    psT = nc.alloc_psum_tensor("psT", [128, 1024], BF16)
    b_psS, b_psX, b_psO, b_psT = Buf("psS"), Buf("psX"), Buf("psO"), Buf("psT")

    ar = Arena(nc, S, 48600)

    def mm(out, lhsT, rhs, start, stop, reads, writes):
        S.op("pe", lambda e: e.matmul(out, lhsT=lhsT, rhs=rhs, start=start, stop=stop), reads, writes)

    def tr(out, in_, reads, writes):
        S.op("pe", lambda e: e.transpose(out, in_, ident[:in_.shape[0], :in_.shape[0]]),
             list(reads) + [b_ident], writes)

    def actf(out, in_, func, reads, writes, **kw):
        S.op("act", lambda e: e.activation(out=out, in_=in_, func=func, **kw), reads, writes)

    def acopy(out, in_, reads, writes):
        S.op("act", lambda e: e.copy(out=out, in_=in_), reads, writes)

    def vcopy(out, in_, reads, writes):
        S.op("dve", lambda e: e.tensor_copy(out, in_), reads, writes)

    def tt(out, in0, in1, op, reads, writes, eng="dve"):
        S.op(eng, lambda e: e.tensor_tensor(out=out, in0=in0, in1=in1, op=op), reads, writes)

    def ts(out, in0, s1, s2, op0, op1, reads, writes, accum=None):
        if op1 is None:
            S.op("dve", lambda e: e.tensor_scalar(out=out, in0=in0, scalar1=s1, scalar2=None, op0=op0),
                 reads, writes)
        elif accum is None:
            S.op("dve", lambda e: e.tensor_scalar(out=out, in0=in0, scalar1=s1, scalar2=s2, op0=op0, op1=op1),
                 reads, writes)
        else:
            S.op("dve", lambda e: e.tensor_scalar(out=out, in0=in0, scalar1=s1, scalar2=s2, op0=op0, op1=op1,
                                                  accum_out=accum), reads, writes)

    def stt(out, in0, scalar, in1, op0, op1, reads, writes, eng="dve"):
        S.op(eng, lambda e: e.scalar_tensor_tensor(out=out, in0=in0, scalar=scalar, in1=in1, op0=op0, op1=op1),
             reads, writes)

    def rsum(out, in_, reads, writes):
        S.op("dve", lambda e: e.reduce_sum(out=out, in_=in_, axis=AX.X), reads, writes)

    def rmax(out, in_, reads, writes):
        S.op("dve", lambda e: e.reduce_max(out=out, in_=in_, axis=AX.X), reads, writes)

    def rstd_from_ss(ss_col, tmp_col, out_col, n, bufs):
        ts(st[:, tmp_col:tmp_col + 1], st[:, ss_col:ss_col + 1], 1.0 / n, 1e-6, ALU.mult, ALU.add, bufs, bufs)
        S.op("act", lambda e: e.sqrt(out=st[:, tmp_col:tmp_col + 1], in_=st[:, tmp_col:tmp_col + 1]), bufs, bufs)
        S.op("dve", lambda e: e.reciprocal(out=st[:, out_col:out_col + 1], in_=st[:, tmp_col:tmp_col + 1]), bufs, bufs)

    def chunks(n, c=512):
        return [(a, min(n, a + c)) for a in range(0, n, c)]

    evac_flip = [0]

    def evac(out, in_, reads, writes):
        evac_flip[0] ^= 1
        if evac_flip[0]:
            acopy(out, in_, reads, writes)
        else:
            vcopy(out, in_, reads, writes)

    m0 = ar.mark()
    identf, b_identf = ar.push([128, 128], F32, "identf")
    S.op("pool", lambda e: e.memset(identf, 0.0), [], [b_identf])
    S.op("pool", lambda e: e.affine_select(out=identf, in_=identf, pattern=[[-1, 128]], compare_op=ALU.not_equal,
                                           fill=1.0, base=0, channel_multiplier=1), [b_identf], [b_identf])
    vcopy(ident[:], identf, [b_identf], [b_ident])
    S.op("pool", lambda e: e.memset(negm[:], 0.0), [], [b_negm])
    S.op("pool", lambda e: e.affine_select(out=negm[:], in_=negm[:], pattern=[[-1, 128]], compare_op=ALU.is_ge,
                                           fill=NEG, base=0, channel_multiplier=1), [b_negm], [b_negm])
    for k in range(NIT):
        S.op("pool", lambda e: e.memset(pow2[:, k:k + 1], 2.0 ** -(k + 1)), [], [b_pow2])

    cp(1)
    S.dma("sp", gsub[:], g_subln_d.partition_broadcast(128), [], [b_gsub])
    S.dma("sp", gkv[:], g_kv_d.partition_broadcast(128), [], [b_gkv])
    ts(gsub[:], gsub[:], 0.8, None, ALU.mult, None, [b_gsub], [b_gsub])
    lv, b_lv = ar.push([128, 4, 64], F32, "lv")
    for q in range(4):
        S.dma("sp", lv[:, q, :], lam_d[q].partition_broadcast(128), [], [b_lv])
    tt(lv[:, 0, :], lv[:, 0, :], lv[:, 1, :], ALU.mult, [b_lv], [b_lv])
    tt(lv[:, 2, :], lv[:, 2, :], lv[:, 3, :], ALU.mult, [b_lv], [b_lv])
    rsum(lamt[:, 0:1], lv[:, 0, :], [b_lv], [b_lamt])
    rsum(lamt[:, 1:2], lv[:, 2, :], [b_lv], [b_lamt])
    actf(lamt[:, 2:4], lamt[:, 0:2], AF.Exp, [b_lamt], [b_lamt])
    tt(lamt[:, 4:5], lamt[:, 2:3], lamt[:, 3:4], ALU.subtract, [b_lamt], [b_lamt])
    ts(lamt[:, 5:6], lamt[:, 4:5], -1.0, -0.2, ALU.mult, ALU.add, [b_lamt], [b_lamt])

    cp(2)
    tab, b_tab = ar.push([128, 32, 8], F32, "tab")
    ev, b_ev = ar.push([128, 32, 8], F32, "ev")
    bidx, b_bidx = ar.push([128, 256], F32, "bidx")
    tmpe, b_tmpe = ar.push([128, 256], F32, "tmpe")
    S.dma("sp", tab.rearrange("p a b -> p (a b)"), relb_d.rearrange("a b -> (a b)").partition_broadcast(128), [], [b_tab])
    S.dma("sp", bidx, bidx_d, [], [b_bidx])
    tt(ev, tab, tab[:, 31:32, :].to_broadcast([128, 32, 8]), ALU.subtract, [b_tab], [b_ev])
    actf(ev, ev, AF.Exp, [b_ev], [b_ev])
    S.op("pool", lambda e: e.memset(EB[:], 0.0), [], [b_EB])
    for h in range(8):
        for bk in range(32):
            stt(tmpe, bidx, float(bk), ev[:, bk, h:h + 1].to_broadcast([128, 256]), ALU.is_equal, ALU.mult,
                [b_bidx, b_ev], [b_tmpe])
            tt(EB[:, h, :], EB[:, h, :], tmpe, ALU.add, [b_EB, b_tmpe], [b_EB])

    cp(3)
    wuf, b_wuf = ar.push([128, 4, 128], F32, "wuf")
    S.dma("sp", wuf, w_uv_d.rearrange("h c d -> c h d"), [], [b_wuf])
    vcopy(w_uvb[:], wuf, [b_wuf], [b_w_uvb])

    kf, b_kf = ar.push([128, 16, 64], F32, "kf")
    kb, b_kb = ar.push([128, 16, 64], BF16, "kb")
    S.dma("sp", kf, keys_d.rearrange("h p n d -> n (h p) d"), [], [b_kf])
    vcopy(kb, kf, [b_kf], [b_kb])
    S.op("pool", lambda e: e.memset(keysBD[:], 0.0), [], [b_keysBD])
    for h in range(8):
        tr(psT[:, 0:128], kb[:, 2 * h:2 * h + 2, :].rearrange("p a b -> p (a b)"), [b_kb], [b_psT])
        acopy(keysBD[0:64, h, 0:128], psT[0:64, 0:128], [b_psT], [b_keysBD])
        acopy(keysBD[64:128, h, 128:256], psT[64:128, 0:128], [b_psT], [b_keysBD])

    cp(4)
    wst = [ar.push([128, 2760], F32, "wst%d" % i) for i in range(2)]
    wsb = [ar.push([128, 2760], BF16, "wsb%d" % i) for i in range(2)]
    cnt = 0
    for (src, dst, b_dst, ncol) in ((w_in_d, w_in_bf, b_w_in_bf, 2760), (w_out_d, w_out_bf, b_w_out_bf, D),
                                    (w_pq_d, w_pq_bf, b_w_pq_bf, D)):
        for k in range(8):
            (f, bf), (b, bb) = wst[cnt % 2], wsb[cnt % 2]
            cnt += 1
            S.dma("sp", f[:, 0:ncol], src[k * 128:(k + 1) * 128, :], [], [bf])
            evac(b[:, 0:ncol], f[:, 0:ncol], [bf], [bb])
            if ncol == 2760:
                S.dma("sp", dst[k * 128:(k + 1) * 128, 0:2752], b[:, 0:2752], [bb], [b_dst])
                S.dma("sp", dst[k * 128:(k + 1) * 128, 2752:2816], b[:, 2688:2752], [bb], [b_dst])
                S.dma("sp", dst[k * 128:(k + 1) * 128, 2816:2824], b[:, 2752:2760], [bb], [b_dst])
            else:
                S.dma("sp", dst[k * 128:(k + 1) * 128, :], b[:, 0:ncol], [bb], [b_dst])
    ar.release(m0)

    cp(5)
    m0 = ar.mark()
    cT, b_cT = ar.push([128, nseq, 8], F32, "cT")
    cTb, b_cTb = ar.push([128, 8, nseq], BF16, "cTb")
    bada, b_bada = ar.push([nseq, 6 * D], F32, "bada")
    modsb, b_modsb = ar.push([nseq, 6 * D], F32, "modsb")
    for b in range(nseq):
        S.dma("sp", cT[:, b, :], c_d[b].rearrange("(k p) -> p k", p=128), [], [b_cT], allow_slow_non_contiguous=True)
    S.dma("sp", bada[0:nseq, :], b_ada_d.partition_broadcast(nseq), [], [b_bada])
    actf(cT, cT, AF.Silu, [b_cT], [b_cT])
    vcopy(cTb.rearrange("p k b -> p b k"), cT, [b_cT], [b_cTb])
    waf = [ar.push([128, 8, 512], F32, "waf%d" % i) for i in range(2)]
    wab = [ar.push([128, 8, 512], BF16, "wab%d" % i) for i in range(2)]
    w_ada_v = w_ada_d.rearrange("(k p) n -> p k n", p=128)
    for n in range(12):
        (f, bf), (b, bb) = waf[n % 2], wab[n % 2]
        S.dma("sp", f, w_ada_v[:, :, n * 512:(n + 1) * 512], [], [bf])
        evac(b, f, [bf], [bb])
        for k in range(8):
            mm(psX[0:nseq, 0:512], cTb[:, k, :], b[:, k, :], k == 0, k == 7, [b_cTb, bb], [b_psX])
        tt(modsb[0:nseq, n * 512:(n + 1) * 512], psX[0:nseq, 0:512], bada[0:nseq, n * 512:(n + 1) * 512], ALU.add,
           [b_psX, b_bada], [b_modsb])
    S.dma("sp", mod_d, modsb[0:nseq, :], [b_modsb], [b_mod_d])
    ar.release(m0)

    cp(6)
    m0 = ar.mark()
    uf = [ar.push([128, D], F32, "uf%d" % i) for i in range(2)]
    vf = [ar.push([128, D], F32, "vf%d" % i) for i in range(2)]
    ub = [ar.push([128, D], BF16, "ub%d" % i) for i in range(2)]
    uts = [ar.push([128, D], BF16, "uts%d" % i) for i in range(2)]
    vb = [ar.push([128, D], BF16, "vb%d" % i) for i in range(2)]
    import os
    for blk in range(int(os.environ.get('NBLK', '128'))):
        s_ = blk % 2
        S.dma("sp", uf[s_][0], pu_d[blk * 128:(blk + 1) * 128, :], [], [uf[s_][1]])
        S.dma("sp", vf[s_][0], pv_d[blk * 128:(blk + 1) * 128, :], [], [vf[s_][1]])
        vcopy(ub[s_][0], uf[s_][0], [uf[s_][1]], [ub[s_][1]])
        for k in range(8):
            tr(psT[:, k * 128:(k + 1) * 128], ub[s_][0][:, k * 128:(k + 1) * 128], [ub[s_][1]], [b_psT])
        acopy(uts[s_][0], psT[:], [b_psT], [uts[s_][1]])
        S.dma("sp", UT_d[blk], uts[s_][0], [uts[s_][1]], [b_UT_d])
        acopy(vb[s_][0], vf[s_][0], [vf[s_][1]], [vb[s_][1]])
        S.dma("sp", Vb_d[blk * 128:(blk + 1) * 128, :], vb[s_][0], [vb[s_][1]], [b_Vb_d])
    ar.release(m0)

    cp(7)
    w_in_v = w_in_bf.rearrange("(k p) n -> p k n", p=128)
    w_out_v = w_out_bf.rearrange("(k p) n -> p k n", p=128)
    w_pq_v = w_pq_bf.rearrange("(k p) n -> p k n", p=128)

    def transposes_to(dst_fn, src, ntile, reads, writes):
        for g in range(0, ntile, 8):
            n = min(8, ntile - g)
            for q in range(n):
                jj = g + q
                tr(psT[:, q * 128:(q + 1) * 128], src[:, jj * 128:(jj + 1) * 128], reads, [b_psT])
            evac(dst_fn(g, n), psT[:, 0:n * 128], [b_psT], writes)

    for b in range(nseq):
        mseq = ar.mark()
        xin = [ar.push([128, D], F32, "xin%d" % i) for i in range(2)]
        junk, b_junk = ar.push([128, D], F32, "junk")
        mixT, b_mixT = ar.push([128, 8, SEQ], BF16, "mixT")
        hT, b_hT = ar.push([128, 8, SEQ], BF16, "hT")

        m1 = ar.mark()
        moda, b_moda = ar.push([128, 2, D], F32, "moda")
        gtmp, b_gtmp = ar.push([128, D], F32, "gtmp")
        xn, b_xn = ar.push([128, D], F32, "xn")
        hb, b_hb = ar.push([128, D], BF16, "hb")
        S.dma("sp", moda.rearrange("p a b -> p (a b)"), mod_d[b, 0:2 * D].partition_broadcast(128), [b_mod_d], [b_moda])
        S.dma("sp", gtmp, g_mix_d.partition_broadcast(128), [], [b_gtmp])
        stt(moda[:, 1, :], moda[:, 1, :], 1.0, gtmp, ALU.add, ALU.mult, [b_moda, b_gtmp], [b_moda])
        for j in range(NT):
            xt, b_xt = xin[j % 2]
            S.dma("sp", xt, x_d[b, j * 128:(j + 1) * 128, :], [], [b_xt], key="xin%d" % (j % 2))
            actf(junk, xt, AF.Square, [b_xt], [b_junk])
            rsum(st[:, 0:1], junk, [b_junk], [b_st])
            rstd_from_ss(0, 1, 2, D, [b_st])
            actf(xn, xt, AF.Copy, [b_xt, b_st], [b_xn], scale=st[:, 2:3])
            tt(xn, xn, moda[:, 1, :], ALU.mult, [b_xn, b_moda], [b_xn])
            tt(hb, xn, moda[:, 0, :], ALU.add, [b_xn, b_moda], [b_hb])
            for k in range(8):
                tr(psT[:, k * 128:(k + 1) * 128], hb[:, k * 128:(k + 1) * 128], [b_hb], [b_psT])
            evac(hT[:, :, j * 128:(j + 1) * 128], psT[:].rearrange("p (k t) -> p k t", k=8), [b_psT], [b_hT])
        ar.release(m1)

        cp(8)
        m2 = ar.mark()
        sqT, b_sqT = ar.push([128, 4, SEQ], BF16, "sqT")
        iqT, b_iqT = ar.push([128, 4, SEQ], BF16, "iqT")
        ik2, b_ik2 = ar.push([128, SEQ], BF16, "ik2")
        kvT, b_kvT = ar.push([128, SEQ], BF16, "kvT")
        kvA, b_kvA = ar.push([128, NT, 130], BF16, "kvA")
        iws, b_iws = ar.push([128, NT, 8], F32, "iws")
        m2b = ar.mark()
        wsl, b_wsl = ar.push([128, 8, 1288], BF16, "wsl")
        kf32, b_kf32 = ar.push([128, 128], F32, "kf32")
        S.dma("sp", wsl, w_in_v[:, :, 1536:2824], [b_w_in_bf], [b_wsl])
        S.op("pool", lambda e: e.memset(kvA[:, :, 128:130], 1.0), [], [b_kvA])
        for tc in range(0 if os.environ.get('SKIP_A') else 4):
            cs = slice(tc * 512, (tc + 1) * 512)
            for (dst, b_dst, off) in ([(sqT[:, h, cs], b_sqT, h * 128) for h in range(4)] +
                                      [(iqT[:, c, cs], b_iqT, 640 + c * 128) for c in range(4)] +
                                      [(ik2[:, cs], b_ik2, 1152)]):
                for k in range(8):
                    mm(psX[:, 0:512], wsl[:, k, off:off + 128], hT[:, k, cs], k == 0, k == 7, [b_wsl, b_hT], [b_psX])
                evac(dst, psX[:, 0:512], [b_psX], [b_dst])
        kfall, b_kfall = ar.push([128, NT, 128], F32, "kfall")
        for j in range(NT):
            js = slice(j * 128, (j + 1) * 128)
            for k in range(8):
                mm(psO[:, 0:128], hT[:, k, js], wsl[:, k, 512:640], k == 0, k == 7, [b_wsl, b_hT], [b_psO])
            for k in range(8):
                mm(psO[:, 128:264], hT[:, k, js], wsl[:, k, 1152:1288], k == 0, k == 7, [b_wsl, b_hT], [b_psO])
            ts(iws[:, j, :], psO[:, 256:264], (1.0 / 8.0) * 8.0 ** -0.5, None, ALU.mult, None, [b_psO], [b_iws])
            vcopy(kfall[:, j, :], psO[:, 0:128], [b_psO], [b_kfall])
        for hf in range(2):
            actf(junk.rearrange("p (a b) -> p a b", a=8), kfall[:, hf * 8:(hf + 1) * 8, :], AF.Square, [b_kfall], [b_junk])
            rsum(st[:, 40 + hf * 8:48 + hf * 8], junk.rearrange("p (a b) -> p a b", a=8), [b_junk], [b_st])
        ts(st[:, 40:56], st[:, 40:56], 1.0 / 128, 1e-6, ALU.mult, ALU.add, [b_st], [b_st])
        S.op("act", lambda e: e.sqrt(out=st[:, 40:56], in_=st[:, 40:56]), [b_st], [b_st])
        S.op("dve", lambda e: e.reciprocal(out=st[:, 40:56], in_=st[:, 40:56]), [b_st], [b_st])
        for j in range(0 if os.environ.get('SKIP_C') else NT):
            js = slice(j * 128, (j + 1) * 128)
            stt(kvA[:, j, 0:128], kfall[:, j, :], st[:, 40 + j:41 + j], gkv[:], ALU.mult, ALU.mult,
                [b_kfall, b_gkv, b_st], [b_kvA])
            tr(psT[:, 0:128], kvA[:, j, 0:128], [b_kvA], [b_psT])
            evac(kvT[:, js], psT[:, 0:128], [b_psT], [b_kvT])
        ar.release(m2b)
        Pm, b_Pm = ar.push([128, SEQ], F32, "Pm")
        acc, b_acc = ar.push([128, SEQ], F32, "acc")
        Rt, b_Rt = ar.push([128, SEQ], F32, "Rt")
        Mk, b_Mk = ar.push([128, SEQ], BF16, "Mk")
        Ab, b_Ab = ar.push([128, SEQ], BF16, "Ab")
        AT, b_AT = ar.push([128, NT, 128], BF16, "AT")
        osn, b_osn = ar.push([128, 128], BF16, "osn")
        osT, b_osT = ar.push([128, 128], BF16, "osT")
        steps, b_steps = ar.push([128, NIT], F32, "steps")
        sc_dsa = 128.0 ** -0.5
        import os
        for i in range(int(os.environ.get('NTQ', str(NT)))):
            Si = (i + 1) * 128
            isl = slice(i * 128, (i + 1) * 128)
            for ih in range(8):
                c2, hf = ih // 2, ih % 2
                ps_ = slice(hf * 64, (hf + 1) * 64)
                for (a0, a1) in chunks(Si):
                    mm(psS[:, a0:a1], iqT[ps_, c2, isl], ik2[ps_, a0:a1], True, True, [b_iqT, b_ik2], [b_psS])
                actf(Rt[:, 0:Si], psS[:, 0:Si], AF.Relu, [b_psS], [b_Rt])
                if ih == 0:
                    ts(acc[:, 0:Si], Rt[:, 0:Si], iws[:, i, 0:1], None, ALU.mult, None, [b_Rt, b_iws], [b_acc])
                else:
                    stt(acc[:, 0:Si], Rt[:, 0:Si], iws[:, i, ih:ih + 1], acc[:, 0:Si], ALU.mult, ALU.add,
                        [b_Rt, b_iws, b_acc], [b_acc])
            if i >= 2:
                S.op("dve", lambda e: e.tensor_reduce(out=st[:, 8:9], in_=acc[:, 0:Si], axis=AX.X, op=ALU.min),
                     [b_acc], [b_st])
                rmax(st[:, 9:10], acc[:, 0:Si], [b_acc], [b_st])
                tt(st[:, 10:11], st[:, 9:10], st[:, 8:9], ALU.subtract, [b_st], [b_st])
                ts(steps, pow2[:], st[:, 10:11], None, ALU.mult, None, [b_pow2, b_st], [b_steps])
            tt(acc[:, isl], acc[:, isl], negm[:], ALU.add, [b_acc, b_negm], [b_acc])
            if i >= 2:
                for k in range(NIT):
                    tt(st[:, 11:12], st[:, 8:9], steps[:, k:k + 1], ALU.add, [b_st, b_steps], [b_st])
                    S.op("dve", lambda e: e.memset(st[:, 12:13], 0.0), [], [b_st])
                    ts(Rt[:, 0:Si], acc[:, 0:Si], st[:, 11:12], 0.0, ALU.is_ge, ALU.add, [b_acc, b_st], [b_Rt, b_st],
                       accum=st[:, 12:13])
                    stt(st[:, 13:14], st[:, 12:13], 256.0, steps[:, k:k + 1], ALU.is_ge, ALU.mult,
                        [b_st, b_steps], [b_st])
                    tt(st[:, 8:9], st[:, 8:9], st[:, 13:14], ALU.add, [b_st], [b_st])
            else:
                S.op("dve", lambda e: e.memset(st[:, 8:9], -1.0e29), [], [b_st])
            ts(Mk[:, 0:Si], acc[:, 0:Si], st[:, 8:9], None, ALU.is_ge, None, [b_acc, b_st], [b_Mk])
            lo = max(0, (i - 1) * 128)
            for h in range(4):
                for (a0, a1) in chunks(Si):
                    mm(psS[:, a0:a1], sqT[:, h, isl], kvT[:, a0:a1], True, True, [b_sqT, b_kvT], [b_psS])
                rmax(st[:, 16:17], psS[:, 0:Si], [b_psS], [b_st])
                ts(st[:, 17:18], st[:, 16:17], -sc_dsa, None, ALU.mult, None, [b_st], [b_st])
                actf(Pm[:, 0:Si], psS[:, 0:Si], AF.Exp, [b_psS, b_st], [b_Pm], bias=st[:, 17:18], scale=sc_dsa)
                ebv = EB[:, 4 + h, 128:256] if i == 0 else EB[:, 4 + h, 0:256]
                tt(Pm[:, lo:Si], Pm[:, lo:Si], ebv, ALU.mult, [b_Pm, b_EB], [b_Pm])
                tt(Ab[:, 0:Si], Pm[:, 0:Si], Mk[:, 0:Si], ALU.mult, [b_Pm, b_Mk], [b_Ab])
                transposes_to(lambda g, n: AT[:, g:g + n, :].rearrange("p a b -> p (a b)"), Ab, i + 1, [b_Ab], [b_AT])
                for jj in range(i + 1):
                    mm(psO[:, 0:129], AT[:, jj, :], kvA[:, jj, 0:129], jj == 0, jj == i, [b_AT, b_kvA], [b_psO])
                S.op("dve", lambda e: e.reciprocal(out=st[:, 18:19], in_=psO[:, 128:129]), [b_psO], [b_st])
                ts(osn, psO[:, 0:128], st[:, 18:19], None, ALU.mult, None, [b_psO, b_st], [b_osn])
                tr(psT[:, 0:128], osn, [b_osn], [b_psT])
                evac(osT, psT[:, 0:128], [b_psT], [b_osT])
                mm(psX[:, 0:128], w_uvb[:, h, :], osT, True, True, [b_w_uvb, b_osT], [b_psX])
                evac(mixT[:, 4 + h, isl], psX[:, 0:128], [b_psX], [b_mixT])
        ar.release(m2)

        cp(9)
        sc_diff = 64.0 ** -0.5
        for h in range(4):
            m3 = ar.mark()
            wsl, b_wsl = ar.push([128, 8, 384], BF16, "wsl3")
            qT, b_qT = ar.push([128, SEQ], BF16, "qT")
            kT, b_kT = ar.push([128, SEQ], BF16, "kT")
            Vd, b_Vd = ar.push([128, NT, 128], BF16, "Vd")
            Pm0, b_Pm0 = ar.push([128, SEQ], F32, "Pm0")
            Pm1, b_Pm1 = ar.push([128, SEQ], F32, "Pm1")
            Ab, b_Ab = ar.push([128, SEQ], BF16, "Ab3")
            AT, b_AT = ar.push([128, NT, 128], BF16, "AT3")
            of32, b_of32 = ar.push([128, 128], F32, "of32")
            odb, b_odb = ar.push([128, 128], BF16, "odb")
            for q in range(3):
                S.dma("sp", wsl[:, :, q * 128:(q + 1) * 128], w_in_v[:, :, q * 512 + h * 128:q * 512 + (h + 1) * 128],
                      [b_w_in_bf], [b_wsl])
            for tc in range(4):
                cs = slice(tc * 512, (tc + 1) * 512)
                for (dst, b_dst, off) in ((qT[:, cs], b_qT, 0), (kT[:, cs], b_kT, 128)):
                    for k in range(8):
                        mm(psX[:, 0:512], wsl[:, k, off:off + 128], hT[:, k, cs], k == 0, k == 7, [b_wsl, b_hT], [b_psX])
                    evac(dst, psX[:, 0:512], [b_psX], [b_dst])
            for j in range(NT):
                js = slice(j * 128, (j + 1) * 128)
                for k in range(8):
                    mm(psO[:, 0:128], hT[:, k, js], wsl[:, k, 256:384], k == 0, k == 7, [b_wsl, b_hT], [b_psO])
                vcopy(Vd[:, j, :], psO[:, 0:128], [b_psO], [b_Vd])
            Pms = ((Pm0, b_Pm0), (Pm1, b_Pm1))
            for i in range(NT):
                Si = (i + 1) * 128
                isl = slice(i * 128, (i + 1) * 128)
                lo = max(0, (i - 1) * 128)
                ebv = EB[:, h, 128:256] if i == 0 else EB[:, h, 0:256]
                for m in range(2):
                    Pmm, b_Pmm = Pms[m]
                    ps_ = slice(m * 64, (m + 1) * 64)
                    for (a0, a1) in chunks(Si):
                        mm(psS[:, a0:a1], qT[ps_, isl], kT[ps_, a0:a1], True, True, [b_qT, b_kT], [b_psS])
                    rmax(st[:, 20 + m:21 + m], psS[:, 0:Si], [b_psS], [b_st])
                    ts(st[:, 22 + m:23 + m], st[:, 20 + m:21 + m], -sc_diff, None, ALU.mult, None, [b_st], [b_st])
                    actf(Pmm[:, 0:Si], psS[:, 0:Si], AF.Exp, [b_psS, b_st], [b_Pmm], bias=st[:, 22 + m:23 + m],
                         scale=sc_diff)
                    tt(Pmm[:, lo:Si], Pmm[:, lo:Si], ebv, ALU.mult, [b_Pmm, b_EB], [b_Pmm])
                    rsum(st[:, 24 + m:25 + m], Pmm[:, 0:Si], [b_Pmm], [b_st])
                    S.op("dve", lambda e: e.reciprocal(out=st[:, 26 + m:27 + m], in_=st[:, 24 + m:25 + m]), [b_st], [b_st])
                tt(st[:, 28:29], st[:, 27:28], lamt[:, 5:6], ALU.mult, [b_st, b_lamt], [b_st])
                actf(Pm0[:, 0:Si], Pm0[:, 0:Si], AF.Copy, [b_Pm0, b_st], [b_Pm0], scale=st[:, 26:27])
                stt(Ab[:, 0:Si], Pm1[:, 0:Si], st[:, 28:29], Pm0[:, 0:Si], ALU.mult, ALU.add, [b_Pm0, b_Pm1, b_st], [b_Ab])
                transposes_to(lambda g, n: AT[:, g:g + n, :].rearrange("p a b -> p (a b)"), Ab, i + 1, [b_Ab], [b_AT])
                for jj in range(i + 1):
                    mm(psO[:, 0:128], AT[:, jj, :], Vd[:, jj, :], jj == 0, jj == i, [b_AT, b_Vd], [b_psO])
                vcopy(of32, psO[:, 0:128], [b_psO], [b_of32])
                actf(junk[:, 0:128], of32, AF.Square, [b_of32], [b_junk])
                rsum(st[:, 30:31], junk[:, 0:128], [b_junk], [b_st])
                rstd_from_ss(30, 31, 32, 128, [b_st])
                stt(odb, of32, st[:, 32:33], gsub[:], ALU.mult, ALU.mult, [b_of32, b_gsub, b_st], [b_odb])
                tr(psT[:, 0:128], odb, [b_odb], [b_psT])
                evac(mixT[:, h, isl], psT[:, 0:128], [b_psT], [b_mixT])
            ar.release(m3)

        cp(10)
        m4 = ar.mark()
        w_outb, b_w_outb = ar.push([128, 8, D], BF16, "w_outb")
        gate_a, b_gate_a = ar.push([128, D], F32, "gate_a")
        x1t = [ar.push([128, D], F32, "x1t%d" % i) for i in range(2)]
        S.dma("sp", w_outb, w_out_v, [b_w_out_bf], [b_w_outb])
        S.dma("sp", gate_a, mod_d[b, 2 * D:3 * D].partition_broadcast(128), [b_mod_d], [b_gate_a])
        for j in range(NT):
            js = slice(j * 128, (j + 1) * 128)
            xt, b_xt = xin[j % 2]
            x1, b_x1 = x1t[j % 2]
            S.dma("sp", xt, x_d[b, js, :], [], [b_xt], key="xin%d" % (j % 2))
            for hf in range(2):
                for ch in range(8):
                    mm(psX[:, hf * 512:(hf + 1) * 512], mixT[:, ch, js], w_outb[:, ch, hf * 512:(hf + 1) * 512],
                       ch == 0, ch == 7, [b_mixT, b_w_outb], [b_psX])
            tt(x1, psX[:], gate_a, ALU.mult, [b_psX, b_gate_a], [b_x1])
            tt(x1, x1, xt, ALU.add, [b_x1, b_xt], [b_x1])
            S.dma("sp", x1_d[js, :], x1, [b_x1], [b_x1_d])
        ar.release(mseq)

        cp(11)
        m5 = ar.mark()
        w_pqb, b_w_pqb = ar.push([128, 8, D], BF16, "w_pqb")
        modf, b_modf = ar.push([128, 3, D], F32, "modf")
        gfin, b_gfin = ar.push([128, D], F32, "gfin")
        junk, b_junk = ar.push([128, D], F32, "junk5")
        x1g = [ar.push([128, D], F32, "x1g%d" % i) for i in range(2)]
        xn, b_xn = ar.push([128, D], F32, "xn5")
        hb, b_hb = ar.push([128, D], BF16, "hb5")
        h2T, b_h2T = ar.push([128, 8, 256], BF16, "h2T")
        pqT, b_pqT = ar.push([128, 8, 256], BF16, "pqT")
        scs, b_scs = ar.push([128, 2048], F32, "scs")
        cc, b_cc = ar.push([128, 8, 128], F32, "cc")
        STAGE = os.environ.get("STAGE", "1") == "1"
        LOOKAHEAD = os.environ.get("LOOKAHEAD", "1") == "1"
        NB = 3
        zc, _ = ar.push([128, NB * 1024], F32, "zc")
        b_zt = [Buf("zt%d" % i) for i in range(NB)]
        zts = [zc[:, i * 1024:(i + 1) * 1024].rearrange("p (a b) -> p a b", a=8) for i in range(NB)]
        ets = [ar.push([128, 8, 128], F32, "et%d" % i) for i in range(NB)]
        Wh = [ar.push([128, 8, 128], BF16, "Wh%d" % i) for i in range(NB)]
        sm, b_sm = ar.push([128, 16], F32, "sm")
        Wcs = [ar.push([128, 8, 128], BF16, "Wc%d" % i) for i in range(2)]
        WT, b_WT = ar.push([128, 128, 256], BF16, "WT")
        UTb = [ar.push([128, 8, 128], BF16, "UTb%d" % i) for i in range(3)]
        Vbb = [ar.push([128, D], BF16, "Vbb%d" % i) for i in range(3)]
        Gt = [ar.push([128, 256], F32, "Gt%d" % i) for i in range(2)]
        GW = [ar.push([128, 256], BF16, "GW%d" % i) for i in range(2)]
        s16, b_s16 = ar.push([128, 16, 16], F32, "s16")
        cand = zc[:, 0:2048].rearrange("p (h c) -> p h c", h=8)
        b_candl = [b_zt[0], b_zt[1]]
        wk, b_wk = ar.push([128, 256], F32, "wk")
        topv, b_topv = ar.push([128, 8, 16], F32, "topv")
        dv_, b_dv = ar.push([128, 8, 16], F32, "dv")
        zz, b_zz = ar.push([128, 16], F32, "zz")
        S.dma("sp", w_pqb, w_pq_v, [b_w_pq_bf], [b_w_pqb])
        S.dma("sp", modf.rearrange("p a b -> p (a b)"), mod_d[b, 3 * D:6 * D].partition_broadcast(128), [b_mod_d], [b_modf])
        S.dma("sp", gfin, g_ffn_d.partition_broadcast(128), [], [b_gfin])
        stt(modf[:, 1, :], modf[:, 1, :], 1.0, gfin, ALU.add, ALU.mult, [b_modf, b_gfin], [b_modf])
        S.dma("sp", gfin, g_fin_d.partition_broadcast(128), [], [b_gfin])
        sc4 = scs.rearrange("p (h q n) -> p h q n", h=8, q=2)
        s4 = s16.rearrange("p (h q) r -> p h q r", q=2)
        for g in range(SEQ // 256):
            for jj in range(2):
                j = 2 * g + jj
                x1, b_x1 = x1g[jj]
                S.dma("sp", x1, x1_d[j * 128:(j + 1) * 128, :], [b_x1_d], [b_x1], key="x1g%d" % jj)
                actf(junk, x1, AF.Square, [b_x1], [b_junk])
                rsum(st[:, 0:1], junk, [b_junk], [b_st])
                rstd_from_ss(0, 1, 2, D, [b_st])
                actf(xn, x1, AF.Copy, [b_x1, b_st], [b_xn], scale=st[:, 2:3])
                tt(xn, xn, modf[:, 1, :], ALU.mult, [b_xn, b_modf], [b_xn])
                tt(hb, xn, modf[:, 0, :], ALU.add, [b_xn, b_modf], [b_hb])
                for k in range(8):
                    tr(psT[:, k * 128:(k + 1) * 128], hb[:, k * 128:(k + 1) * 128], [b_hb], [b_psT])
                evac(h2T[:, :, jj * 128:(jj + 1) * 128], psT[:].rearrange("p (k t) -> p k t", k=8), [b_psT], [b_h2T])
            for hh in range(8):
                for k in range(8):
                    mm(psX[:, 0:256], w_pqb[:, k, hh * 128:(hh + 1) * 128], h2T[:, k, :], k == 0, k == 7,
                       [b_w_pqb, b_h2T], [b_psX])
                evac(pqT[:, hh, :], psX[:, 0:256], [b_psX], [b_pqT])
            for jj in range(2):
                tsl = slice(jj * 128, (jj + 1) * 128)
                for hh in range(8):
                    mm(psS[:, hh * 256:(hh + 1) * 256], pqT[:, hh, tsl], keysBD[:, hh, :], True, True,
                       [b_pqT, b_keysBD], [b_psS])
                acopy(scs[:, 0:1024], psS[:, 0:1024], [b_psS], [b_scs])
                vcopy(scs[:, 1024:2048], psS[:, 1024:2048], [b_psS], [b_scs])
                for hp in range(16):
                    src = scs[:, hp * 128:(hp + 1) * 128]
                    S.op("dve", lambda e: e.max(out=s16[:, hp, 0:8], in_=src), [b_scs], [b_s16])
                    S.op("dve", lambda e: e.match_replace(out=wk[:, 0:128], in_to_replace=s16[:, hp, 0:8],
                                                          in_values=src, imm_value=NEG), [b_scs, b_s16], [b_wk])
                    S.op("dve", lambda e: e.max(out=s16[:, hp, 8:16], in_=wk[:, 0:128]), [b_wk], [b_s16])
                tt(cand.rearrange("p h (i j) -> p h i j", i=16),
                   s4[:, :, 0, :].unsqueeze(3).to_broadcast([128, 8, 16, 16]),
                   s4[:, :, 1, :].unsqueeze(2).to_broadcast([128, 8, 16, 16]), ALU.add, [b_s16], b_candl)
                for hh in range(8):
                    S.op("dve", lambda e: e.max(out=topv[:, hh, 0:8], in_=cand[:, hh, :]), b_candl, [b_topv])
                    S.op("dve", lambda e: e.match_replace(out=wk[:, 0:256], in_to_replace=topv[:, hh, 0:8],
                                                          in_values=cand[:, hh, :], imm_value=NEG),
                         b_candl + [b_topv], [b_wk])
                    S.op("dve", lambda e: e.max(out=topv[:, hh, 8:16], in_=wk[:, 0:256]), [b_wk], [b_topv])
                vcopy(zz[:, 0:8], topv[:, :, 15], [b_topv], [b_zz])
                tt(dv_, topv, zz[:, 0:8].unsqueeze(2).to_broadcast([128, 8, 16]), ALU.subtract, [b_topv, b_zz], [b_dv])
                actf(dv_, dv_, AF.Exp, [b_dv], [b_dv])
                rsum(zz[:, 8:16], dv_, [b_dv], [b_zz])
                S.op("dve", lambda e: e.reciprocal(out=sm[:, 8:16], in_=zz[:, 8:16]), [b_zz], [b_sm])
                ts(sm[:, 8:16], sm[:, 8:16], 1.0 - 1.0e-4, None, ALU.mult, None, [b_sm], [b_sm])
                actf(zz[:, 8:16], zz[:, 8:16], AF.Ln, [b_zz], [b_zz])
                ts(zz[:, 8:16], zz[:, 8:16], -1.0, None, ALU.mult, None, [b_zz], [b_zz])
                ts(sm[:, 0:8], zz[:, 8:16], -1.0e-5, None, ALU.add, None, [b_zz], [b_sm])
                tt(cc, sc4[:, :, 0, :], zz[:, 0:8].unsqueeze(2).to_broadcast([128, 8, 128]), ALU.subtract,
                   [b_scs, b_zz], [b_cc])
                tt(cc, cc, zz[:, 8:16].unsqueeze(2).to_broadcast([128, 8, 128]), ALU.add, [b_cc, b_zz], [b_cc])
                steps = [(ci, hh) for ci in range(16) for hh in range(8)]
                NSTEP = len(steps)

                def fz(g):
                    ci, hh = steps[g]
                    k = g % NB
                    if hh % 2 == 0:
                        tt(zts[k], sc4[:, hh, 1, :].unsqueeze(1).to_broadcast([128, 8, 128]),
                           cc[:, hh, ci * 8:(ci + 1) * 8].unsqueeze(2).to_broadcast([128, 8, 128]), ALU.add,
                           [b_scs, b_cc], [b_zt[k]])

                def fe(g):
                    ci, hh = steps[g]
                    k = g % NB
                    et, b_et = ets[k]
                    if hh % 2 == 0:
                        actf(et, zts[k], AF.Exp, [b_zt[k]], [b_et])
                    else:
                        for r in range(8):
                            n1 = ci * 8 + r
                            actf(et[:, r, :], sc4[:, hh, 1, :], AF.Exp, [b_scs, b_cc], [b_et], bias=cc[:, hh, n1:n1 + 1])

                def fs(g):
                    ci, hh = steps[g]
                    k = g % NB
                    et, b_et = ets[k]
                    if hh % 2 == 0:
                        stt(Wh[k][0], zts[k], sm[:, hh:hh + 1], et, ALU.is_ge, ALU.mult, [b_zt[k], b_et, b_sm], [Wh[k][1]])
                    else:
                        stt(Wh[k][0], et, sm[:, 8 + hh:9 + hh], et, ALU.is_ge, ALU.mult, [b_et, b_sm], [Wh[k][1]])
                    Whf = Wh[k][0].rearrange("p a b -> p (a b)")
                    for q in range(2):
                        mm(psS[:, q * 512:(q + 1) * 512], ident[:], Whf[:, q * 512:(q + 1) * 512], hh == 0, hh == 7,
                           [b_ident, Wh[k][1]], [b_psS])
                    if hh == 7:
                        Wc, b_Wc = Wcs[ci % 2]
                        acopy(Wc.rearrange("p a b -> p (a b)"), psS[:, 0:1024], [b_psS], [b_Wc])
                        for q in range(8):
                            tr(psT[:, q * 128:(q + 1) * 128], Wc[:, q, :], [b_Wc], [b_psT])
                        acopy(WT[:, ci * 8:(ci + 1) * 8, tsl], psT[:].rearrange("p (a b) -> p a b", a=8), [b_psT], [b_WT])
                for sidx in range(NSTEP + 2):
                    if sidx < NSTEP:
                        fz(sidx)
                    if 0 <= sidx - 1 < NSTEP:
                        fe(sidx - 1)
                    if 0 <= sidx - 2 < NSTEP:
                        fs(sidx - 2)
            def mA(n1):
                (ut, b_ut), (vt, b_vt) = UTb[n1 % 3], Vbb[n1 % 3]
                S.dma("sp", ut.rearrange("p a b -> p (a b)"), UT_d[n1], [b_UT_d], [b_ut], key="ut%d" % (n1 % 3))
                S.dma("sp", vt, Vb_d[n1 * 128:(n1 + 1) * 128, :], [b_Vb_d], [b_vt], key="vt%d" % (n1 % 3))
                pa = psX[:, (n1 % 2) * 512:(n1 % 2) * 512 + 256]
                for k in range(8):
                    mm(pa, ut[:, k, :], h2T[:, k, :], k == 0, k == 7, [b_ut, b_h2T], [b_psX])
                Gn, b_Gn = Gt[n1 % 2]
                actf(Gn, pa, AF.Gelu, [b_psX], [b_Gn])

            def mB(n1):
                (vt, b_vt) = Vbb[n1 % 3]
                Gn, b_Gn = Gt[n1 % 2]
                GWn, b_GWn = GW[n1 % 2]
                tt(GWn, Gn, WT[:, n1, :], ALU.mult, [b_Gn, b_WT], [b_GWn])
                for jj in range(2):
                    for hf in range(2):
                        mm(psS[:, jj * 1024 + hf * 512:jj * 1024 + (hf + 1) * 512], GWn[:, jj * 128:(jj + 1) * 128],
                           vt[:, hf * 512:(hf + 1) * 512], n1 == 0, n1 == 127, [b_GWn, b_vt], [b_psS])
            if LOOKAHEAD:
                mA(0)
                for n1 in range(128):
                    if n1 + 1 < 128:
                        mA(n1 + 1)
                    mB(n1)
            else:
                for n1 in range(128):
                    mA(n1)
                    mB(n1)
            for jj in range(2):
                j = 2 * g + jj
                x1, b_x1 = x1g[jj]
                tt(xn, psS[:, jj * 1024:(jj + 1) * 1024], modf[:, 2, :], ALU.mult, [b_psS, b_modf], [b_xn])
                tt(xn, xn, x1, ALU.add, [b_xn, b_x1], [b_xn])
                actf(junk, xn, AF.Square, [b_xn], [b_junk])
                rsum(st[:, 0:1], junk, [b_junk], [b_st])
                rstd_from_ss(0, 1, 2, D, [b_st])
                actf(junk, xn, AF.Copy, [b_xn, b_st], [b_junk], scale=st[:, 2:3])
                tt(junk, junk, gfin, ALU.mult, [b_junk, b_gfin], [b_junk])
                S.dma("sp", out_d[b, j * 128:(j + 1) * 128, :], junk, [b_junk], [])
        ar.release(m5)


_CACHE = {}


def kernel(**inputs):
    nseq = 4
    if "nc" not in _CACHE:
        _CACHE["nc"] = build(nseq)[0]
    nc = _CACHE["nc"]
    f = lambda a: np.ascontiguousarray(np.asarray(a, dtype=np.float32))
    x = f(inputs["x"])
    c = f(inputs["c"])
    shared = {
        "w_ada": f(inputs["w_ada"])[0], "b_ada": f(inputs["b_ada"])[0], "g_norm_mix": f(inputs["g_norm_mix"])[0],
        "w_in": f(inputs["w_in"])[0], "lam_q1": f(inputs["lam_q1"])[0], "lam_k1": f(inputs["lam_k1"])[0],
        "lam_q2": f(inputs["lam_q2"])[0], "lam_k2": f(inputs["lam_k2"])[0], "g_subln": f(inputs["g_subln"])[0],
        "g_kv_norm": f(inputs["g_kv_norm"])[0], "w_uv": f(inputs["w_uv"])[0], "w_out": f(inputs["w_out"])[0],
        "g_norm_ffn": f(inputs["g_norm_ffn"])[0], "w_peer_q": f(inputs["w_peer_q"])[0],
        "peer_keys": f(inputs["peer_keys"])[0], "peer_u": f(inputs["peer_u"])[0], "peer_v": f(inputs["peer_v"])[0],
        "rel_bias": f(inputs["rel_bias"]), "g_final": f(inputs["g_final"]), "bidx": const_bidx(),
    }
    in_maps = []
    for i in range(NCORES):
        m = dict(shared)
        m["x"] = x[i * nseq:(i + 1) * nseq]
        m["c"] = c[i * nseq:(i + 1) * nseq]
        in_maps.append(m)
    res = run_bass_kernel_spmd(nc, in_maps, core_ids=list(range(NCORES)))
    return np.concatenate([r["out"] for r in res.results], axis=0).astype(np.float32)
```

```python
import math
import os
import numpy as np
import concourse.bass as bass
import concourse.mybir as mybir
from concourse.bass_utils import run_bass_kernel_spmd

F32 = mybir.dt.float32
BF16 = mybir.dt.bfloat16
ALU = mybir.AluOpType
AF = mybir.ActivationFunctionType
AX = mybir.AxisListType

D = 1024
SEQ = 2048
NT = SEQ // 128
NCORES = 8
NIT = 22
NEG = -1.0e30
WIN_COLS = 2824


class Buf:
    __slots__ = ("name", "w", "r")

    def __init__(self, name=""):
        self.name = name
        self.w = None
        self.r = {}


class Sched:
    def __init__(self, nc):
        self.nc = nc
        self.eng = {"pe": nc.tensor, "act": nc.scalar, "dve": nc.vector,
                    "pool": nc.gpsimd, "sp": nc.sync}
        self.sem = {}
        self.cnt = {}
        for k in ("pe", "act", "dve", "pool", "d_sp", "d_pool", "d_act"):
            self.sem[k] = nc.alloc_semaphore("s_" + k)
            self.cnt[k] = 0
        self.seen = {k: {} for k in self.eng}
        self.n_ins = 0

    def _deps(self, reads, writes):
        deps = {}
        for b in reads:
            if b.w is not None:
                k, v = b.w
                if v > deps.get(k, 0):
                    deps[k] = v
        for b in writes:
            if b.w is not None:
                k, v = b.w
                if v > deps.get(k, 0):
                    deps[k] = v
            for k, v in b.r.items():
                if v > deps.get(k, 0):
                    deps[k] = v
        return deps

    def _wait(self, e, deps):
        eng = self.eng[e]
        seen = self.seen[e]
        for k, v in deps.items():
            if e == "pe" and k == "pe":
                continue
            if v > seen.get(k, 0):
                eng.wait_ge(self.sem[k], v)
                seen[k] = v
                self.n_ins += 1

    def _mark(self, tok, reads, writes):
        k, v = tok
        for b in writes:
            b.w = tok
            b.r = {}
        for b in reads:
            if b in writes:
                continue
            if v > b.r.get(k, 0):
                b.r[k] = v

    def op(self, e, fn, reads=(), writes=()):
        self._wait(e, self._deps(reads, writes))
        ins = fn(self.eng[e])
        self.cnt[e] += 1
        ins.then_inc(self.sem[e], 1)
        self.n_ins += 1
        self._mark((e, self.cnt[e]), reads, writes)
        return ins

    def dma(self, q, out, in_, reads=(), writes=(), key=None, **kw):
        self._wait(q, self._deps(reads, writes))
        ins = self.eng[q].dma_start(out=out, in_=in_, **kw)
        k = "d_" + q
        if key is not None:
            k = "dk_" + key
            if k not in self.sem:
                self.sem[k] = self.nc.alloc_semaphore("s_" + k)
                self.cnt[k] = 0
        self.cnt[k] += 16
        ins.then_inc(self.sem[k], 16)
        self.n_ins += 1
        self._mark((k, self.cnt[k]), reads, writes)
        return ins

    def barrier(self):
        allv = {k: v for k, v in self.cnt.items() if v > 0}
        for e in self.eng:
            self._wait(e, allv)


class Arena:
    def __init__(self, nc, S, words):
        self.t = nc.alloc_sbuf_tensor("arena", [128, words], F32)
        self.words = words
        self.top = 0
        self.S = S

    def push(self, shape, dtype, name=""):
        n = 1
        for s in shape[1:]:
            n *= s
        nbytes = n * (4 if dtype == F32 else 2)
        w = (nbytes + 3) // 4
        w = (w + 7) // 8 * 8
        assert self.top + w <= self.words, (name, self.top, w, self.words)
        v = self.t[:, self.top:self.top + w]
        if dtype != F32:
            v = v.bitcast(dtype)
        v = v[:, 0:n]
        if len(shape) == 3:
            v = v.rearrange("p (a b) -> p a b", a=shape[1])
        elif len(shape) == 4:
            v = v.rearrange("p (a b c) -> p a b c", a=shape[1], b=shape[2])
        self.top += w
        return v, Buf(name)

    def mark(self):
        return self.top

    def release(self, m):
        self.S.barrier()
        self.top = m


def rel_bucket_np(n):
    max_exact = 16
    nf = np.maximum(n, max_exact).astype(np.float32)
    large = max_exact + (np.log(nf / max_exact) / math.log(128 / max_exact) * 16).astype(np.int32)
    large = np.minimum(large, 31)
    return np.where(n < max_exact, n, large)


def const_bidx():
    t = np.arange(128)[:, None] + 128
    s = np.arange(256)[None, :]
    d = t - s
    b = rel_bucket_np(np.maximum(d, 0)).astype(np.float32)
    return np.where(d >= 0, b, -1.0).astype(np.float32)


class _Stop(Exception):
    pass


def build(nseq=4, dbg=False, stop=None):
    nc_S = []
    try:
        _build(nseq, stop, nc_S)
    except _Stop:
        pass
    nc, S = nc_S
    S.barrier()
    return nc, S


def _build(nseq, stop, nc_S):
    nc = bass.Bass("TRN2", target_bir_lowering=False)

    def din(name, shape):
        return nc.dram_tensor(name, list(shape), F32, kind="ExternalInput").ap()

    x_d = din("x", [nseq, SEQ, D])
    c_d = din("c", [nseq, D])
    w_ada_d = din("w_ada", [D, 6 * D])
    b_ada_d = din("b_ada", [6 * D])
    g_mix_d = din("g_norm_mix", [D])
    w_in_d = din("w_in", [D, 2760])
    lam_d = [din(n, [64]) for n in ("lam_q1", "lam_k1", "lam_q2", "lam_k2")]
    g_subln_d = din("g_subln", [128])
    g_kv_d = din("g_kv_norm", [128])
    w_uv_d = din("w_uv", [4, 128, 128])
    w_out_d = din("w_out", [D, D])
    g_ffn_d = din("g_norm_ffn", [D])
    w_pq_d = din("w_peer_q", [D, D])
    keys_d = din("peer_keys", [8, 2, 128, 64])
    pu_d = din("peer_u", [16384, D])
    pv_d = din("peer_v", [16384, D])
    relb_d = din("rel_bias", [32, 8])
    g_fin_d = din("g_final", [D])
    bidx_d = din("bidx", [128, 256])
    out_d = nc.dram_tensor("out", [nseq, SEQ, D], F32, kind="ExternalOutput").ap()

    def dscr(name, shape, dt):
        return nc.dram_tensor(name, list(shape), dt, kind="Internal").ap()

    w_in_bf = dscr("w_in_bf", [D, WIN_COLS], BF16)
    w_out_bf = dscr("w_out_bf", [D, D], BF16)
    w_pq_bf = dscr("w_pq_bf", [D, D], BF16)
    UT_d = dscr("UT_d", [128, 128, D], BF16)
    Vb_d = dscr("Vb_d", [16384, D], BF16)
    mod_d = dscr("mod_d", [nseq, 6 * D], F32)
    x1_d = dscr("x1_d", [SEQ, D], F32)
    b_w_in_bf, b_w_out_bf, b_w_pq_bf = Buf(), Buf(), Buf()
    b_UT_d, b_Vb_d, b_mod_d, b_x1_d = Buf(), Buf(), Buf(), Buf()

    S = Sched(nc)
    nc_S.extend([nc, S])
    A = nc.alloc_sbuf_tensor

    def cp(n):
        if stop == n:
            raise _Stop()

    def P_(name, shape, dt):
        return A(name, shape, dt), Buf(name)

    ident, b_ident = P_("ident", [128, 128], BF16)
    w_uvb, b_w_uvb = P_("w_uvb", [128, 4, 128], BF16)
    EB, b_EB = P_("EB", [128, 8, 256], F32)
    negm, b_negm = P_("negm", [128, 128], F32)
    gsub, b_gsub = P_("gsub", [128, 128], F32)
    gkv, b_gkv = P_("gkv", [128, 128], F32)
    keysBD, b_keysBD = P_("keysBD", [128, 8, 256], BF16)
    lamt, b_lamt = P_("lamt", [128, 8], F32)
    pow2, b_pow2 = P_("pow2", [128, NIT], F32)
    st, b_st = P_("st", [128, 64], F32)

    psS = nc.alloc_psum_tensor("psS", [128, 2048], F32)
    psX = nc.alloc_psum_tensor("psX", [128, 1024], F32)
    psO = nc.alloc_psum_tensor("psO", [128, 512], F32)
    psT = nc.alloc_psum_tensor("psT", [128, 1024], BF16)
    b_psS, b_psX, b_psO, b_psT = Buf("psS"), Buf("psX"), Buf("psO"), Buf("psT")

    ar = Arena(nc, S, 48600)

    def mm(out, lhsT, rhs, start, stop, reads, writes):
        S.op("pe", lambda e: e.matmul(out, lhsT=lhsT, rhs=rhs, start=start, stop=stop), reads, writes)

    def tr(out, in_, reads, writes):
        S.op("pe", lambda e: e.transpose(out, in_, ident[:in_.shape[0], :in_.shape[0]]),
             list(reads) + [b_ident], writes)

    def actf(out, in_, func, reads, writes, **kw):
        S.op("act", lambda e: e.activation(out=out, in_=in_, func=func, **kw), reads, writes)

    def acopy(out, in_, reads, writes):
        S.op("act", lambda e: e.copy(out=out, in_=in_), reads, writes)

    def vcopy(out, in_, reads, writes):
        S.op("dve", lambda e: e.tensor_copy(out, in_), reads, writes)

    def tt(out, in0, in1, op, reads, writes, eng="dve"):
        S.op(eng, lambda e: e.tensor_tensor(out=out, in0=in0, in1=in1, op=op), reads, writes)

    def ts(out, in0, s1, s2, op0, op1, reads, writes, accum=None):
        if op1 is None:
            S.op("dve", lambda e: e.tensor_scalar(out=out, in0=in0, scalar1=s1, scalar2=None, op0=op0),
                 reads, writes)
        elif accum is None:
            S.op("dve", lambda e: e.tensor_scalar(out=out, in0=in0, scalar1=s1, scalar2=s2, op0=op0, op1=op1),
                 reads, writes)
        else:
            S.op("dve", lambda e: e.tensor_scalar(out=out, in0=in0, scalar1=s1, scalar2=s2, op0=op0, op1=op1,
                                                  accum_out=accum), reads, writes)

    def stt(out, in0, scalar, in1, op0, op1, reads, writes, eng="dve"):
        S.op(eng, lambda e: e.scalar_tensor_tensor(out=out, in0=in0, scalar=scalar, in1=in1, op0=op0, op1=op1),
             reads, writes)

    def rsum(out, in_, reads, writes):
        S.op("dve", lambda e: e.reduce_sum(out=out, in_=in_, axis=AX.X), reads, writes)

    def rmax(out, in_, reads, writes):
        S.op("dve", lambda e: e.reduce_max(out=out, in_=in_, axis=AX.X), reads, writes)

    def rstd_from_ss(ss_col, tmp_col, out_col, n, bufs):
        ts(st[:, tmp_col:tmp_col + 1], st[:, ss_col:ss_col + 1], 1.0 / n, 1e-6, ALU.mult, ALU.add, bufs, bufs)
        S.op("act", lambda e: e.sqrt(out=st[:, tmp_col:tmp_col + 1], in_=st[:, tmp_col:tmp_col + 1]), bufs, bufs)
        S.op("dve", lambda e: e.reciprocal(out=st[:, out_col:out_col + 1], in_=st[:, tmp_col:tmp_col + 1]), bufs, bufs)

    def chunks(n, c=512):
        return [(a, min(n, a + c)) for a in range(0, n, c)]

    evac_flip = [0]

    def evac(out, in_, reads, writes):
        evac_flip[0] ^= 1
        if evac_flip[0]:
            acopy(out, in_, reads, writes)
        else:
            vcopy(out, in_, reads, writes)

    m0 = ar.mark()
    identf, b_identf = ar.push([128, 128], F32, "identf")
    S.op("pool", lambda e: e.memset(identf, 0.0), [], [b_identf])
    S.op("pool", lambda e: e.affine_select(out=identf, in_=identf, pattern=[[-1, 128]], compare_op=ALU.not_equal,
                                           fill=1.0, base=0, channel_multiplier=1), [b_identf], [b_identf])
    vcopy(ident[:], identf, [b_identf], [b_ident])
    S.op("pool", lambda e: e.memset(negm[:], 0.0), [], [b_negm])
    S.op("pool", lambda e: e.affine_select(out=negm[:], in_=negm[:], pattern=[[-1, 128]], compare_op=ALU.is_ge,
                                           fill=NEG, base=0, channel_multiplier=1), [b_negm], [b_negm])
    for k in range(NIT):
        S.op("pool", lambda e: e.memset(pow2[:, k:k + 1], 2.0 ** -(k + 1)), [], [b_pow2])

    cp(1)
    S.dma("sp", gsub[:], g_subln_d.partition_broadcast(128), [], [b_gsub])
    S.dma("sp", gkv[:], g_kv_d.partition_broadcast(128), [], [b_gkv])
    ts(gsub[:], gsub[:], 0.8, None, ALU.mult, None, [b_gsub], [b_gsub])
    lv, b_lv = ar.push([128, 4, 64], F32, "lv")
    for q in range(4):
        S.dma("sp", lv[:, q, :], lam_d[q].partition_broadcast(128), [], [b_lv])
    tt(lv[:, 0, :], lv[:, 0, :], lv[:, 1, :], ALU.mult, [b_lv], [b_lv])
    tt(lv[:, 2, :], lv[:, 2, :], lv[:, 3, :], ALU.mult, [b_lv], [b_lv])
    rsum(lamt[:, 0:1], lv[:, 0, :], [b_lv], [b_lamt])
    rsum(lamt[:, 1:2], lv[:, 2, :], [b_lv], [b_lamt])
    actf(lamt[:, 2:4], lamt[:, 0:2], AF.Exp, [b_lamt], [b_lamt])
    tt(lamt[:, 4:5], lamt[:, 2:3], lamt[:, 3:4], ALU.subtract, [b_lamt], [b_lamt])
    ts(lamt[:, 5:6], lamt[:, 4:5], -1.0, -0.2, ALU.mult, ALU.add, [b_lamt], [b_lamt])

    cp(2)
    tab, b_tab = ar.push([128, 32, 8], F32, "tab")
    ev, b_ev = ar.push([128, 32, 8], F32, "ev")
    bidx, b_bidx = ar.push([128, 256], F32, "bidx")
    tmpe, b_tmpe = ar.push([128, 256], F32, "tmpe")
    S.dma("sp", tab.rearrange("p a b -> p (a b)"), relb_d.rearrange("a b -> (a b)").partition_broadcast(128), [], [b_tab])
    S.dma("sp", bidx, bidx_d, [], [b_bidx])
    tt(ev, tab, tab[:, 31:32, :].to_broadcast([128, 32, 8]), ALU.subtract, [b_tab], [b_ev])
    actf(ev, ev, AF.Exp, [b_ev], [b_ev])
    S.op("pool", lambda e: e.memset(EB[:], 0.0), [], [b_EB])
    for h in range(8):
        for bk in range(32):
            stt(tmpe, bidx, float(bk), ev[:, bk, h:h + 1].to_broadcast([128, 256]), ALU.is_equal, ALU.mult,
                [b_bidx, b_ev], [b_tmpe])
            tt(EB[:, h, :], EB[:, h, :], tmpe, ALU.add, [b_EB, b_tmpe], [b_EB])

    cp(3)
    wuf, b_wuf = ar.push([128, 4, 128], F32, "wuf")
    S.dma("sp", wuf, w_uv_d.rearrange("h c d -> c h d"), [], [b_wuf])
    vcopy(w_uvb[:], wuf, [b_wuf], [b_w_uvb])

    kf, b_kf = ar.push([128, 16, 64], F32, "kf")
    kb, b_kb = ar.push([128, 16, 64], BF16, "kb")
    S.dma("sp", kf, keys_d.rearrange("h p n d -> n (h p) d"), [], [b_kf])
    vcopy(kb, kf, [b_kf], [b_kb])
    S.op("pool", lambda e: e.memset(keysBD[:], 0.0), [], [b_keysBD])
    for h in range(8):
        tr(psT[:, 0:128], kb[:, 2 * h:2 * h + 2, :].rearrange("p a b -> p (a b)"), [b_kb], [b_psT])
        acopy(keysBD[0:64, h, 0:128], psT[0:64, 0:128], [b_psT], [b_keysBD])
        acopy(keysBD[64:128, h, 128:256], psT[64:128, 0:128], [b_psT], [b_keysBD])

    cp(4)
    wst = [ar.push([128, 2760], F32, "wst%d" % i) for i in range(2)]
    wsb = [ar.push([128, 2760], BF16, "wsb%d" % i) for i in range(2)]
    cnt = 0
    for (src, dst, b_dst, ncol) in ((w_in_d, w_in_bf, b_w_in_bf, 2760), (w_out_d, w_out_bf, b_w_out_bf, D),
                                    (w_pq_d, w_pq_bf, b_w_pq_bf, D)):
        for k in range(8):
            (f, bf), (b, bb) = wst[cnt % 2], wsb[cnt % 2]
            cnt += 1
            S.dma("sp", f[:, 0:ncol], src[k * 128:(k + 1) * 128, :], [], [bf])
            evac(b[:, 0:ncol], f[:, 0:ncol], [bf], [bb])
            if ncol == 2760:
                S.dma("sp", dst[k * 128:(k + 1) * 128, 0:2752], b[:, 0:2752], [bb], [b_dst])
                S.dma("sp", dst[k * 128:(k + 1) * 128, 2752:2816], b[:, 2688:2752], [bb], [b_dst])
                S.dma("sp", dst[k * 128:(k + 1) * 128, 2816:2824], b[:, 2752:2760], [bb], [b_dst])
            else:
                S.dma("sp", dst[k * 128:(k + 1) * 128, :], b[:, 0:ncol], [bb], [b_dst])
    ar.release(m0)

    cp(5)
    m0 = ar.mark()
    cT, b_cT = ar.push([128, nseq, 8], F32, "cT")
    cTb, b_cTb = ar.push([128, 8, nseq], BF16, "cTb")
    bada, b_bada = ar.push([nseq, 6 * D], F32, "bada")
    modsb, b_modsb = ar.push([nseq, 6 * D], F32, "modsb")
    for b in range(nseq):
        S.dma("sp", cT[:, b, :], c_d[b].rearrange("(k p) -> p k", p=128), [], [b_cT], allow_slow_non_contiguous=True)
    S.dma("sp", bada[0:nseq, :], b_ada_d.partition_broadcast(nseq), [], [b_bada])
    actf(cT, cT, AF.Silu, [b_cT], [b_cT])
    vcopy(cTb.rearrange("p k b -> p b k"), cT, [b_cT], [b_cTb])
    waf = [ar.push([128, 8, 512], F32, "waf%d" % i) for i in range(2)]
    wab = [ar.push([128, 8, 512], BF16, "wab%d" % i) for i in range(2)]
    w_ada_v = w_ada_d.rearrange("(k p) n -> p k n", p=128)
    for n in range(12):
        (f, bf), (b, bb) = waf[n % 2], wab[n % 2]
        S.dma("sp", f, w_ada_v[:, :, n * 512:(n + 1) * 512], [], [bf])
        evac(b, f, [bf], [bb])
        for k in range(8):
            mm(psX[0:nseq, 0:512], cTb[:, k, :], b[:, k, :], k == 0, k == 7, [b_cTb, bb], [b_psX])
        tt(modsb[0:nseq, n * 512:(n + 1) * 512], psX[0:nseq, 0:512], bada[0:nseq, n * 512:(n + 1) * 512], ALU.add,
           [b_psX, b_bada], [b_modsb])
    S.dma("sp", mod_d, modsb[0:nseq, :], [b_modsb], [b_mod_d])
    ar.release(m0)

    cp(6)
    m0 = ar.mark()
    uf = [ar.push([128, D], F32, "uf%d" % i) for i in range(2)]
    vf = [ar.push([128, D], F32, "vf%d" % i) for i in range(2)]
    ub = [ar.push([128, D], BF16, "ub%d" % i) for i in range(2)]
    uts = [ar.push([128, D], BF16, "uts%d" % i) for i in range(2)]
    vb = [ar.push([128, D], BF16, "vb%d" % i) for i in range(2)]
    import os
    for blk in range(int(os.environ.get('NBLK', '128'))):
        s_ = blk % 2
        S.dma("sp", uf[s_][0], pu_d[blk * 128:(blk + 1) * 128, :], [], [uf[s_][1]])
        S.dma("sp", vf[s_][0], pv_d[blk * 128:(blk + 1) * 128, :], [], [vf[s_][1]])
        vcopy(ub[s_][0], uf[s_][0], [uf[s_][1]], [ub[s_][1]])
        for k in range(8):
            tr(psT[:, k * 128:(k + 1) * 128], ub[s_][0][:, k * 128:(k + 1) * 128], [ub[s_][1]], [b_psT])
        acopy(uts[s_][0], psT[:], [b_psT], [uts[s_][1]])
        S.dma("sp", UT_d[blk], uts[s_][0], [uts[s_][1]], [b_UT_d])
        acopy(vb[s_][0], vf[s_][0], [vf[s_][1]], [vb[s_][1]])
        S.dma("sp", Vb_d[blk * 128:(blk + 1) * 128, :], vb[s_][0], [vb[s_][1]], [b_Vb_d])
    ar.release(m0)

    cp(7)
    w_in_v = w_in_bf.rearrange("(k p) n -> p k n", p=128)
    w_out_v = w_out_bf.rearrange("(k p) n -> p k n", p=128)
    w_pq_v = w_pq_bf.rearrange("(k p) n -> p k n", p=128)

    def transposes_to(dst_fn, src, ntile, reads, writes):
        for g in range(0, ntile, 8):
            n = min(8, ntile - g)
            for q in range(n):
                jj = g + q
                tr(psT[:, q * 128:(q + 1) * 128], src[:, jj * 128:(jj + 1) * 128], reads, [b_psT])
            evac(dst_fn(g, n), psT[:, 0:n * 128], [b_psT], writes)

    for b in range(nseq):
        mseq = ar.mark()
        xin = [ar.push([128, D], F32, "xin%d" % i) for i in range(2)]
        junk, b_junk = ar.push([128, D], F32, "junk")
        mixT, b_mixT = ar.push([128, 8, SEQ], BF16, "mixT")
        hT, b_hT = ar.push([128, 8, SEQ], BF16, "hT")

        m1 = ar.mark()
        moda, b_moda = ar.push([128, 2, D], F32, "moda")
        gtmp, b_gtmp = ar.push([128, D], F32, "gtmp")
        xn, b_xn = ar.push([128, D], F32, "xn")
        hb, b_hb = ar.push([128, D], BF16, "hb")
        S.dma("sp", moda.rearrange("p a b -> p (a b)"), mod_d[b, 0:2 * D].partition_broadcast(128), [b_mod_d], [b_moda])
        S.dma("sp", gtmp, g_mix_d.partition_broadcast(128), [], [b_gtmp])
        stt(moda[:, 1, :], moda[:, 1, :], 1.0, gtmp, ALU.add, ALU.mult, [b_moda, b_gtmp], [b_moda])
        for j in range(NT):
            xt, b_xt = xin[j % 2]
            S.dma("sp", xt, x_d[b, j * 128:(j + 1) * 128, :], [], [b_xt], key="xin%d" % (j % 2))
            actf(junk, xt, AF.Square, [b_xt], [b_junk])
            rsum(st[:, 0:1], junk, [b_junk], [b_st])
            rstd_from_ss(0, 1, 2, D, [b_st])
            actf(xn, xt, AF.Copy, [b_xt, b_st], [b_xn], scale=st[:, 2:3])
            tt(xn, xn, moda[:, 1, :], ALU.mult, [b_xn, b_moda], [b_xn])
            tt(hb, xn, moda[:, 0, :], ALU.add, [b_xn, b_moda], [b_hb])
            for k in range(8):
                tr(psT[:, k * 128:(k + 1) * 128], hb[:, k * 128:(k + 1) * 128], [b_hb], [b_psT])
            evac(hT[:, :, j * 128:(j + 1) * 128], psT[:].rearrange("p (k t) -> p k t", k=8), [b_psT], [b_hT])
        ar.release(m1)

        cp(8)
        m2 = ar.mark()
        sqT, b_sqT = ar.push([128, 4, SEQ], BF16, "sqT")
        iqT, b_iqT = ar.push([128, 4, SEQ], BF16, "iqT")
        ik2, b_ik2 = ar.push([128, SEQ], BF16, "ik2")
        kvT, b_kvT = ar.push([128, SEQ], BF16, "kvT")
        kvA, b_kvA = ar.push([128, NT, 130], BF16, "kvA")
        iws, b_iws = ar.push([128, NT, 8], F32, "iws")
        m2b = ar.mark()
        wsl, b_wsl = ar.push([128, 8, 1288], BF16, "wsl")
        kf32, b_kf32 = ar.push([128, 128], F32, "kf32")
        S.dma("sp", wsl, w_in_v[:, :, 1536:2824], [b_w_in_bf], [b_wsl])
        S.op("pool", lambda e: e.memset(kvA[:, :, 128:130], 1.0), [], [b_kvA])
        for tc in range(0 if os.environ.get('SKIP_A') else 4):
            cs = slice(tc * 512, (tc + 1) * 512)
            for (dst, b_dst, off) in ([(sqT[:, h, cs], b_sqT, h * 128) for h in range(4)] +
                                      [(iqT[:, c, cs], b_iqT, 640 + c * 128) for c in range(4)] +
                                      [(ik2[:, cs], b_ik2, 1152)]):
                for k in range(8):
                    mm(psX[:, 0:512], wsl[:, k, off:off + 128], hT[:, k, cs], k == 0, k == 7, [b_wsl, b_hT], [b_psX])
                evac(dst, psX[:, 0:512], [b_psX], [b_dst])
        kfall, b_kfall = ar.push([128, NT, 128], F32, "kfall")
        for j in range(NT):
            js = slice(j * 128, (j + 1) * 128)
            for k in range(8):
                mm(psO[:, 0:128], hT[:, k, js], wsl[:, k, 512:640], k == 0, k == 7, [b_wsl, b_hT], [b_psO])
            for k in range(8):
                mm(psO[:, 128:264], hT[:, k, js], wsl[:, k, 1152:1288], k == 0, k == 7, [b_wsl, b_hT], [b_psO])
            ts(iws[:, j, :], psO[:, 256:264], (1.0 / 8.0) * 8.0 ** -0.5, None, ALU.mult, None, [b_psO], [b_iws])
            vcopy(kfall[:, j, :], psO[:, 0:128], [b_psO], [b_kfall])
        for hf in range(2):
            actf(junk.rearrange("p (a b) -> p a b", a=8), kfall[:, hf * 8:(hf + 1) * 8, :], AF.Square, [b_kfall], [b_junk])
            rsum(st[:, 40 + hf * 8:48 + hf * 8], junk.rearrange("p (a b) -> p a b", a=8), [b_junk], [b_st])
        ts(st[:, 40:56], st[:, 40:56], 1.0 / 128, 1e-6, ALU.mult, ALU.add, [b_st], [b_st])
        S.op("act", lambda e: e.sqrt(out=st[:, 40:56], in_=st[:, 40:56]), [b_st], [b_st])
        S.op("dve", lambda e: e.reciprocal(out=st[:, 40:56], in_=st[:, 40:56]), [b_st], [b_st])
        for j in range(0 if os.environ.get('SKIP_C') else NT):
            js = slice(j * 128, (j + 1) * 128)
            stt(kvA[:, j, 0:128], kfall[:, j, :], st[:, 40 + j:41 + j], gkv[:], ALU.mult, ALU.mult,
                [b_kfall, b_gkv, b_st], [b_kvA])
            tr(psT[:, 0:128], kvA[:, j, 0:128], [b_kvA], [b_psT])
            evac(kvT[:, js], psT[:, 0:128], [b_psT], [b_kvT])
        ar.release(m2b)
        Pm, b_Pm = ar.push([128, SEQ], F32, "Pm")
        acc, b_acc = ar.push([128, SEQ], F32, "acc")
        Rt, b_Rt = ar.push([128, SEQ], F32, "Rt")
        Mk, b_Mk = ar.push([128, SEQ], BF16, "Mk")
        Ab, b_Ab = ar.push([128, SEQ], BF16, "Ab")
        AT, b_AT = ar.push([128, NT, 128], BF16, "AT")
        osn, b_osn = ar.push([128, 128], BF16, "osn")
        osT, b_osT = ar.push([128, 128], BF16, "osT")
        steps, b_steps = ar.push([128, NIT], F32, "steps")
        sc_dsa = 128.0 ** -0.5
        import os
        for i in range(int(os.environ.get('NTQ', str(NT)))):
            Si = (i + 1) * 128
            isl = slice(i * 128, (i + 1) * 128)
            for ih in range(8):
                c2, hf = ih // 2, ih % 2
                ps_ = slice(hf * 64, (hf + 1) * 64)
                for (a0, a1) in chunks(Si):
                    mm(psS[:, a0:a1], iqT[ps_, c2, isl], ik2[ps_, a0:a1], True, True, [b_iqT, b_ik2], [b_psS])
                actf(Rt[:, 0:Si], psS[:, 0:Si], AF.Relu, [b_psS], [b_Rt])
                if ih == 0:
                    ts(acc[:, 0:Si], Rt[:, 0:Si], iws[:, i, 0:1], None, ALU.mult, None, [b_Rt, b_iws], [b_acc])
                else:
                    stt(acc[:, 0:Si], Rt[:, 0:Si], iws[:, i, ih:ih + 1], acc[:, 0:Si], ALU.mult, ALU.add,
                        [b_Rt, b_iws, b_acc], [b_acc])
            if i >= 2:
                S.op("dve", lambda e: e.tensor_reduce(out=st[:, 8:9], in_=acc[:, 0:Si], axis=AX.X, op=ALU.min),
                     [b_acc], [b_st])
                rmax(st[:, 9:10], acc[:, 0:Si], [b_acc], [b_st])
                tt(st[:, 10:11], st[:, 9:10], st[:, 8:9], ALU.subtract, [b_st], [b_st])
                ts(steps, pow2[:], st[:, 10:11], None, ALU.mult, None, [b_pow2, b_st], [b_steps])
            tt(acc[:, isl], acc[:, isl], negm[:], ALU.add, [b_acc, b_negm], [b_acc])
            if i >= 2:
                for k in range(NIT):
                    tt(st[:, 11:12], st[:, 8:9], steps[:, k:k + 1], ALU.add, [b_st, b_steps], [b_st])
                    S.op("dve", lambda e: e.memset(st[:, 12:13], 0.0), [], [b_st])
                    ts(Rt[:, 0:Si], acc[:, 0:Si], st[:, 11:12], 0.0, ALU.is_ge, ALU.add, [b_acc, b_st], [b_Rt, b_st],
                       accum=st[:, 12:13])
                    stt(st[:, 13:14], st[:, 12:13], 256.0, steps[:, k:k + 1], ALU.is_ge, ALU.mult,
                        [b_st, b_steps], [b_st])
                    tt(st[:, 8:9], st[:, 8:9], st[:, 13:14], ALU.add, [b_st], [b_st])
            else:
                S.op("dve", lambda e: e.memset(st[:, 8:9], -1.0e29), [], [b_st])
            ts(Mk[:, 0:Si], acc[:, 0:Si], st[:, 8:9], None, ALU.is_ge, None, [b_acc, b_st], [b_Mk])
            lo = max(0, (i - 1) * 128)
            for h in range(4):
                for (a0, a1) in chunks(Si):
                    mm(psS[:, a0:a1], sqT[:, h, isl], kvT[:, a0:a1], True, True, [b_sqT, b_kvT], [b_psS])
                rmax(st[:, 16:17], psS[:, 0:Si], [b_psS], [b_st])
                ts(st[:, 17:18], st[:, 16:17], -sc_dsa, None, ALU.mult, None, [b_st], [b_st])
                actf(Pm[:, 0:Si], psS[:, 0:Si], AF.Exp, [b_psS, b_st], [b_Pm], bias=st[:, 17:18], scale=sc_dsa)
                ebv = EB[:, 4 + h, 128:256] if i == 0 else EB[:, 4 + h, 0:256]
                tt(Pm[:, lo:Si], Pm[:, lo:Si], ebv, ALU.mult, [b_Pm, b_EB], [b_Pm])
                tt(Ab[:, 0:Si], Pm[:, 0:Si], Mk[:, 0:Si], ALU.mult, [b_Pm, b_Mk], [b_Ab])
                transposes_to(lambda g, n: AT[:, g:g + n, :].rearrange("p a b -> p (a b)"), Ab, i + 1, [b_Ab], [b_AT])
                for jj in range(i + 1):
                    mm(psO[:, 0:129], AT[:, jj, :], kvA[:, jj, 0:129], jj == 0, jj == i, [b_AT, b_kvA], [b_psO])
                S.op("dve", lambda e: e.reciprocal(out=st[:, 18:19], in_=psO[:, 128:129]), [b_psO], [b_st])
                ts(osn, psO[:, 0:128], st[:, 18:19], None, ALU.mult, None, [b_psO, b_st], [b_osn])
                tr(psT[:, 0:128], osn, [b_osn], [b_psT])
                evac(osT, psT[:, 0:128], [b_psT], [b_osT])
                mm(psX[:, 0:128], w_uvb[:, h, :], osT, True, True, [b_w_uvb, b_osT], [b_psX])
                evac(mixT[:, 4 + h, isl], psX[:, 0:128], [b_psX], [b_mixT])
        ar.release(m2)

        cp(9)
        sc_diff = 64.0 ** -0.5
        for h in range(4):
            m3 = ar.mark()
            wsl, b_wsl = ar.push([128, 8, 384], BF16, "wsl3")
            qT, b_qT = ar.push([128, SEQ], BF16, "qT")
            kT, b_kT = ar.push([128, SEQ], BF16, "kT")
            Vd, b_Vd = ar.push([128, NT, 128], BF16, "Vd")
            Pm0, b_Pm0 = ar.push([128, SEQ], F32, "Pm0")
            Pm1, b_Pm1 = ar.push([128, SEQ], F32, "Pm1")
            Ab, b_Ab = ar.push([128, SEQ], BF16, "Ab3")
            AT, b_AT = ar.push([128, NT, 128], BF16, "AT3")
            of32, b_of32 = ar.push([128, 128], F32, "of32")
            odb, b_odb = ar.push([128, 128], BF16, "odb")
            for q in range(3):
                S.dma("sp", wsl[:, :, q * 128:(q + 1) * 128], w_in_v[:, :, q * 512 + h * 128:q * 512 + (h + 1) * 128],
                      [b_w_in_bf], [b_wsl])
            for tc in range(4):
                cs = slice(tc * 512, (tc + 1) * 512)
                for (dst, b_dst, off) in ((qT[:, cs], b_qT, 0), (kT[:, cs], b_kT, 128)):
                    for k in range(8):
                        mm(psX[:, 0:512], wsl[:, k, off:off + 128], hT[:, k, cs], k == 0, k == 7, [b_wsl, b_hT], [b_psX])
                    evac(dst, psX[:, 0:512], [b_psX], [b_dst])
            for j in range(NT):
                js = slice(j * 128, (j + 1) * 128)
                for k in range(8):
                    mm(psO[:, 0:128], hT[:, k, js], wsl[:, k, 256:384], k == 0, k == 7, [b_wsl, b_hT], [b_psO])
                vcopy(Vd[:, j, :], psO[:, 0:128], [b_psO], [b_Vd])
            Pms = ((Pm0, b_Pm0), (Pm1, b_Pm1))
            for i in range(NT):
                Si = (i + 1) * 128
                isl = slice(i * 128, (i + 1) * 128)
                lo = max(0, (i - 1) * 128)
                ebv = EB[:, h, 128:256] if i == 0 else EB[:, h, 0:256]
                for m in range(2):
                    Pmm, b_Pmm = Pms[m]
                    ps_ = slice(m * 64, (m + 1) * 64)
                    for (a0, a1) in chunks(Si):
                        mm(psS[:, a0:a1], qT[ps_, isl], kT[ps_, a0:a1], True, True, [b_qT, b_kT], [b_psS])
                    rmax(st[:, 20 + m:21 + m], psS[:, 0:Si], [b_psS], [b_st])
                    ts(st[:, 22 + m:23 + m], st[:, 20 + m:21 + m], -sc_diff, None, ALU.mult, None, [b_st], [b_st])
                    actf(Pmm[:, 0:Si], psS[:, 0:Si], AF.Exp, [b_psS, b_st], [b_Pmm], bias=st[:, 22 + m:23 + m],
                         scale=sc_diff)
                    tt(Pmm[:, lo:Si], Pmm[:, lo:Si], ebv, ALU.mult, [b_Pmm, b_EB], [b_Pmm])
                    rsum(st[:, 24 + m:25 + m], Pmm[:, 0:Si], [b_Pmm], [b_st])
                    S.op("dve", lambda e: e.reciprocal(out=st[:, 26 + m:27 + m], in_=st[:, 24 + m:25 + m]), [b_st], [b_st])
                tt(st[:, 28:29], st[:, 27:28], lamt[:, 5:6], ALU.mult, [b_st, b_lamt], [b_st])
                actf(Pm0[:, 0:Si], Pm0[:, 0:Si], AF.Copy, [b_Pm0, b_st], [b_Pm0], scale=st[:, 26:27])
                stt(Ab[:, 0:Si], Pm1[:, 0:Si], st[:, 28:29], Pm0[:, 0:Si], ALU.mult, ALU.add, [b_Pm0, b_Pm1, b_st], [b_Ab])
                transposes_to(lambda g, n: AT[:, g:g + n, :].rearrange("p a b -> p (a b)"), Ab, i + 1, [b_Ab], [b_AT])
                for jj in range(i + 1):
                    mm(psO[:, 0:128], AT[:, jj, :], Vd[:, jj, :], jj == 0, jj == i, [b_AT, b_Vd], [b_psO])
                vcopy(of32, psO[:, 0:128], [b_psO], [b_of32])
                actf(junk[:, 0:128], of32, AF.Square, [b_of32], [b_junk])
                rsum(st[:, 30:31], junk[:, 0:128], [b_junk], [b_st])
                rstd_from_ss(30, 31, 32, 128, [b_st])
                stt(odb, of32, st[:, 32:33], gsub[:], ALU.mult, ALU.mult, [b_of32, b_gsub, b_st], [b_odb])
                tr(psT[:, 0:128], odb, [b_odb], [b_psT])
                evac(mixT[:, h, isl], psT[:, 0:128], [b_psT], [b_mixT])
            ar.release(m3)

        cp(10)
        m4 = ar.mark()
        w_outb, b_w_outb = ar.push([128, 8, D], BF16, "w_outb")
        gate_a, b_gate_a = ar.push([128, D], F32, "gate_a")
        x1t = [ar.push([128, D], F32, "x1t%d" % i) for i in range(2)]
        S.dma("sp", w_outb, w_out_v, [b_w_out_bf], [b_w_outb])
        S.dma("sp", gate_a, mod_d[b, 2 * D:3 * D].partition_broadcast(128), [b_mod_d], [b_gate_a])
        for j in range(NT):
            js = slice(j * 128, (j + 1) * 128)
            xt, b_xt = xin[j % 2]
            x1, b_x1 = x1t[j % 2]
            S.dma("sp", xt, x_d[b, js, :], [], [b_xt], key="xin%d" % (j % 2))
            for hf in range(2):
                for ch in range(8):
                    mm(psX[:, hf * 512:(hf + 1) * 512], mixT[:, ch, js], w_outb[:, ch, hf * 512:(hf + 1) * 512],
                       ch == 0, ch == 7, [b_mixT, b_w_outb], [b_psX])
            tt(x1, psX[:], gate_a, ALU.mult, [b_psX, b_gate_a], [b_x1])
            tt(x1, x1, xt, ALU.add, [b_x1, b_xt], [b_x1])
            S.dma("sp", x1_d[js, :], x1, [b_x1], [b_x1_d])
        ar.release(mseq)

        cp(11)
        m5 = ar.mark()
        w_pqb, b_w_pqb = ar.push([128, 8, D], BF16, "w_pqb")
        modf, b_modf = ar.push([128, 3, D], F32, "modf")
        gfin, b_gfin = ar.push([128, D], F32, "gfin")
        junk, b_junk = ar.push([128, D], F32, "junk5")
        x1g = [ar.push([128, D], F32, "x1g%d" % i) for i in range(2)]
        xn, b_xn = ar.push([128, D], F32, "xn5")
        hb, b_hb = ar.push([128, D], BF16, "hb5")
        h2T, b_h2T = ar.push([128, 8, 256], BF16, "h2T")
        pqT, b_pqT = ar.push([128, 8, 256], BF16, "pqT")
        scs, b_scs = ar.push([128, 2048], F32, "scs")
        LOOKAHEAD = os.environ.get("LOOKAHEAD", "1") == "1"
        NB = 3
        ets = [ar.push([128, 8, 128], F32, "et%d" % i) for i in range(NB)]
        Wh = [ar.push([128, 8, 128], BF16, "Wh%d" % i) for i in range(NB)]
        Wcs = [ar.push([128, 8, 128], BF16, "Wc%d" % i) for i in range(2)]
        sms = [ar.push([128, 16], F32, "sm%d" % i) for i in range(2)]
        b_psZ = [Buf("psZ0"), Buf("psZ1")]
        WT, b_WT = ar.push([128, 128, 256], BF16, "WT")
        UTb = [ar.push([128, 8, 128], BF16, "UTb%d" % i) for i in range(3)]
        Vbb = [ar.push([128, D], BF16, "Vbb%d" % i) for i in range(3)]
        Gt = [ar.push([128, 256], F32, "Gt%d" % i) for i in range(2)]
        GW = [ar.push([128, 256], BF16, "GW%d" % i) for i in range(2)]
        s16, b_s16 = ar.push([128, 16, 16], F32, "s16")
        cand, b_cand = ar.push([128, 8, 256], F32, "cand")
        wk, b_wk = ar.push([128, 256], F32, "wk")
        topv, b_topv = ar.push([128, 8, 16], F32, "topv")
        dv_, b_dv = ar.push([128, 8, 16], F32, "dv")
        zz, b_zz = ar.push([128, 16], F32, "zz")
        S.dma("sp", w_pqb, w_pq_v, [b_w_pq_bf], [b_w_pqb])
        S.dma("sp", modf.rearrange("p a b -> p (a b)"), mod_d[b, 3 * D:6 * D].partition_broadcast(128), [b_mod_d], [b_modf])
        S.dma("sp", gfin, g_ffn_d.partition_broadcast(128), [], [b_gfin])
        stt(modf[:, 1, :], modf[:, 1, :], 1.0, gfin, ALU.add, ALU.mult, [b_modf, b_gfin], [b_modf])
        S.dma("sp", gfin, g_fin_d.partition_broadcast(128), [], [b_gfin])
        sc4 = scs.rearrange("p (h q n) -> p h q n", h=8, q=2)
        s4 = s16.rearrange("p (h q) r -> p h q r", q=2)
        for g in range(SEQ // 256):
            for jj in range(2):
                j = 2 * g + jj
                x1, b_x1 = x1g[jj]
                S.dma("sp", x1, x1_d[j * 128:(j + 1) * 128, :], [b_x1_d], [b_x1], key="x1g%d" % jj)
                actf(junk, x1, AF.Square, [b_x1], [b_junk])
                rsum(st[:, 0:1], junk, [b_junk], [b_st])
                rstd_from_ss(0, 1, 2, D, [b_st])
                actf(xn, x1, AF.Copy, [b_x1, b_st], [b_xn], scale=st[:, 2:3])
                tt(xn, xn, modf[:, 1, :], ALU.mult, [b_xn, b_modf], [b_xn])
                tt(hb, xn, modf[:, 0, :], ALU.add, [b_xn, b_modf], [b_hb])
                for k in range(8):
                    tr(psT[:, k * 128:(k + 1) * 128], hb[:, k * 128:(k + 1) * 128], [b_hb], [b_psT])
                evac(h2T[:, :, jj * 128:(jj + 1) * 128], psT[:].rearrange("p (k t) -> p k t", k=8), [b_psT], [b_h2T])
            for hh in range(8):
                for k in range(8):
                    mm(psX[:, 0:256], w_pqb[:, k, hh * 128:(hh + 1) * 128], h2T[:, k, :], k == 0, k == 7,
                       [b_w_pqb, b_h2T], [b_psX])
                evac(pqT[:, hh, :], psX[:, 0:256], [b_psX], [b_pqT])
            def prep_items(jj, via_psO):
                tsl = slice(jj * 128, (jj + 1) * 128)
                sm, b_sm = sms[jj]
                items = []
                if via_psO:
                    def f_sc(qd):
                        for u in range(2):
                            hh = 2 * qd + u
                            mm(psO[:, u * 256:(u + 1) * 256], pqT[:, hh, tsl], keysBD[:, hh, :], True, True,
                               [b_pqT, b_keysBD], [b_psO])
                        vcopy(scs[:, qd * 512:(qd + 1) * 512], psO[:], [b_psO], [b_scs])
                    items += [(lambda qd=qd: f_sc(qd)) for qd in range(4)]
                else:
                    def f_sc_all():
                        for hh in range(8):
                            mm(psS[:, hh * 256:(hh + 1) * 256], pqT[:, hh, tsl], keysBD[:, hh, :], True, True,
                               [b_pqT, b_keysBD], [b_psS] + b_psZ)
                        acopy(scs[:, 0:1024], psS[:, 0:1024], [b_psS], [b_scs])
                        vcopy(scs[:, 1024:2048], psS[:, 1024:2048], [b_psS], [b_scs])
                    items.append(f_sc_all)

                def f_hp(hp):
                    src = scs[:, hp * 128:(hp + 1) * 128]
                    S.op("dve", lambda e: e.max(out=s16[:, hp, 0:8], in_=src), [b_scs], [b_s16])
                    S.op("dve", lambda e: e.match_replace(out=wk[:, 0:128], in_to_replace=s16[:, hp, 0:8],
                                                          in_values=src, imm_value=NEG), [b_scs, b_s16], [b_wk])
                    S.op("dve", lambda e: e.max(out=s16[:, hp, 8:16], in_=wk[:, 0:128]), [b_wk], [b_s16])
                items += [(lambda hp=hp: f_hp(hp)) for hp in range(16)]

                def f_cand():
                    tt(cand.rearrange("p h (i j) -> p h i j", i=16),
                       s4[:, :, 0, :].unsqueeze(3).to_broadcast([128, 8, 16, 16]),
                       s4[:, :, 1, :].unsqueeze(2).to_broadcast([128, 8, 16, 16]), ALU.add, [b_s16], [b_cand])
                items.append(f_cand)

                def f_top(hh):
                    S.op("dve", lambda e: e.max(out=topv[:, hh, 0:8], in_=cand[:, hh, :]), [b_cand], [b_topv])
                    S.op("dve", lambda e: e.match_replace(out=wk[:, 0:256], in_to_replace=topv[:, hh, 0:8],
                                                          in_values=cand[:, hh, :], imm_value=NEG),
                         [b_cand, b_topv], [b_wk])
                    S.op("dve", lambda e: e.max(out=topv[:, hh, 8:16], in_=wk[:, 0:256]), [b_wk], [b_topv])
                items += [(lambda hh=hh: f_top(hh)) for hh in range(8)]

                def f_fin():
                    vcopy(zz[:, 0:8], topv[:, :, 15], [b_topv], [b_zz])
                    tt(dv_, topv, zz[:, 0:8].unsqueeze(2).to_broadcast([128, 8, 16]), ALU.subtract, [b_topv, b_zz], [b_dv])
                    actf(dv_, dv_, AF.Exp, [b_dv], [b_dv])
                    rsum(zz[:, 8:16], dv_, [b_dv], [b_zz])
                    S.op("dve", lambda e: e.reciprocal(out=sm[:, 8:16], in_=zz[:, 8:16]), [b_zz], [b_sm])
                    ts(sm[:, 8:16], sm[:, 8:16], 1.0 - 1.0e-4, None, ALU.mult, None, [b_sm], [b_sm])
                    actf(zz[:, 8:16], zz[:, 8:16], AF.Ln, [b_zz], [b_zz])
                    stt(sm[:, 0:8], zz[:, 8:16], -1.0, zz[:, 0:8], ALU.mult, ALU.subtract, [b_zz], [b_sm])
                items.append(f_fin)
                return items

            def wbuild(jj, queue):
                tsl = slice(jj * 128, (jj + 1) * 128)
                sm, b_sm = sms[jj]
                steps = [(ci, hh) for ci in range(16) for hh in range(8)]
                NSTEP = len(steps)

                def fzp(gi):
                    ci, hh = steps[gi]
                    k2 = gi % 2
                    wr = [b_psZ[k2]] + ([b_psS] if gi < 2 else [])
                    for q in range(2):
                        o_ = psS[:, k2 * 1024 + q * 512:k2 * 1024 + (q + 1) * 512]
                        n0 = ci * 8 + q * 4
                        mm(o_, pqT[:, hh, tsl], keysBD[:, hh, 128:256].unsqueeze(1).to_broadcast([128, 4, 128]), True, False,
                           [b_pqT, b_keysBD], wr)
                        mm(o_, pqT[:, hh, tsl], keysBD[:, hh, n0:n0 + 4].unsqueeze(2).to_broadcast([128, 4, 128]), False, True,
                           [b_pqT, b_keysBD], wr)

                def fe(gi):
                    ci, hh = steps[gi]
                    k2 = gi % 2
                    et, b_et = ets[gi % NB]
                    actf(et.rearrange("p a b -> p (a b)"), psS[:, k2 * 1024:(k2 + 1) * 1024], AF.Exp, [b_psZ[k2], b_sm], [b_et],
                         bias=sm[:, hh:hh + 1])

                def fs(gi):
                    ci, hh = steps[gi]
                    et, b_et = ets[gi % NB]
                    Whh, b_Whh = Wh[gi % NB]
                    stt(Whh, et, sm[:, 8 + hh:9 + hh], et, ALU.is_ge, ALU.mult, [b_et, b_sm], [b_Whh])
                    Whf = Whh.rearrange("p a b -> p (a b)")
                    for q in range(2):
                        mm(psX[:, q * 512:(q + 1) * 512], ident[:], Whf[:, q * 512:(q + 1) * 512], hh == 0, hh == 7,
                           [b_ident, b_Whh], [b_psX])
                    if hh == 7:
                        Wc, b_Wc = Wcs[ci % 2]
                        acopy(Wc.rearrange("p a b -> p (a b)"), psX[:], [b_psX], [b_Wc])
                        for q in range(8):
                            tr(psT[:, q * 128:(q + 1) * 128], Wc[:, q, :], [b_Wc], [b_psT])
                        acopy(WT[:, ci * 8:(ci + 1) * 8, tsl], psT[:].rearrange("p (a b) -> p a b", a=8), [b_psT], [b_WT])
                for sidx in range(NSTEP + 2):
                    if sidx < NSTEP:
                        fzp(sidx)
                    if 0 <= sidx - 1 < NSTEP:
                        fe(sidx - 1)
                    if 0 <= sidx - 2 < NSTEP:
                        fs(sidx - 2)
                    if queue and sidx % 3 == 2:
                        queue.pop(0)()
                while queue:
                    queue.pop(0)()

            for f in prep_items(0, False):
                f()
            wbuild(0, prep_items(1, True) if not os.environ.get("NO_PREP_OVERLAP") else [])
            if os.environ.get("NO_PREP_OVERLAP"):
                for f in prep_items(1, True):
                    f()
            wbuild(1, [])
            def mA(n1):
                (ut, b_ut), (vt, b_vt) = UTb[n1 % 3], Vbb[n1 % 3]
                S.dma("sp", ut.rearrange("p a b -> p (a b)"), UT_d[n1], [b_UT_d], [b_ut], key="ut%d" % (n1 % 3))
                S.dma("sp", vt, Vb_d[n1 * 128:(n1 + 1) * 128, :], [b_Vb_d], [b_vt], key="vt%d" % (n1 % 3))
                pa = psX[:, (n1 % 2) * 512:(n1 % 2) * 512 + 256]
                for k in range(8):
                    mm(pa, ut[:, k, :], h2T[:, k, :], k == 0, k == 7, [b_ut, b_h2T], [b_psX])
                Gn, b_Gn = Gt[n1 % 2]
                actf(Gn, pa, AF.Gelu, [b_psX], [b_Gn])

            def mB(n1):
                (vt, b_vt) = Vbb[n1 % 3]
                Gn, b_Gn = Gt[n1 % 2]
                GWn, b_GWn = GW[n1 % 2]
                tt(GWn, Gn, WT[:, n1, :], ALU.mult, [b_Gn, b_WT], [b_GWn])
                for jj in range(2):
                    for hf in range(2):
                        mm(psS[:, jj * 1024 + hf * 512:jj * 1024 + (hf + 1) * 512], GWn[:, jj * 128:(jj + 1) * 128],
                           vt[:, hf * 512:(hf + 1) * 512], n1 == 0, n1 == 127, [b_GWn, b_vt], [b_psS] + b_psZ)
            if LOOKAHEAD:
                mA(0)
                for n1 in range(128):
                    if n1 + 1 < 128:
                        mA(n1 + 1)
                    mB(n1)
            else:
                for n1 in range(128):
                    mA(n1)
                    mB(n1)
            for jj in range(2):
                j = 2 * g + jj
                x1, b_x1 = x1g[jj]
                tt(xn, psS[:, jj * 1024:(jj + 1) * 1024], modf[:, 2, :], ALU.mult, [b_psS, b_modf], [b_xn])
                tt(xn, xn, x1, ALU.add, [b_xn, b_x1], [b_xn])
                actf(junk, xn, AF.Square, [b_xn], [b_junk])
                rsum(st[:, 0:1], junk, [b_junk], [b_st])
                rstd_from_ss(0, 1, 2, D, [b_st])
                actf(junk, xn, AF.Copy, [b_xn, b_st], [b_junk], scale=st[:, 2:3])
                tt(junk, junk, gfin, ALU.mult, [b_junk, b_gfin], [b_junk])
                S.dma("sp", out_d[b, j * 128:(j + 1) * 128, :], junk, [b_junk], [])
        ar.release(m5)


_CACHE = {}


def kernel(**inputs):
    nseq = 4
    if "nc" not in _CACHE:
        _CACHE["nc"] = build(nseq)[0]
    nc = _CACHE["nc"]
    f = lambda a: np.ascontiguousarray(np.asarray(a, dtype=np.float32))
    x = f(inputs["x"])
    c = f(inputs["c"])
    shared = {
        "w_ada": f(inputs["w_ada"])[0], "b_ada": f(inputs["b_ada"])[0], "g_norm_mix": f(inputs["g_norm_mix"])[0],
        "w_in": f(inputs["w_in"])[0], "lam_q1": f(inputs["lam_q1"])[0], "lam_k1": f(inputs["lam_k1"])[0],
        "lam_q2": f(inputs["lam_q2"])[0], "lam_k2": f(inputs["lam_k2"])[0], "g_subln": f(inputs["g_subln"])[0],
        "g_kv_norm": f(inputs["g_kv_norm"])[0], "w_uv": f(inputs["w_uv"])[0], "w_out": f(inputs["w_out"])[0],
        "g_norm_ffn": f(inputs["g_norm_ffn"])[0], "w_peer_q": f(inputs["w_peer_q"])[0],
        "peer_keys": f(inputs["peer_keys"])[0], "peer_u": f(inputs["peer_u"])[0], "peer_v": f(inputs["peer_v"])[0],
        "rel_bias": f(inputs["rel_bias"]), "g_final": f(inputs["g_final"]), "bidx": const_bidx(),
    }
    in_maps = []
    for i in range(NCORES):
        m = dict(shared)
        m["x"] = x[i * nseq:(i + 1) * nseq]
        m["c"] = c[i * nseq:(i + 1) * nseq]
        in_maps.append(m)
    res = run_bass_kernel_spmd(nc, in_maps, core_ids=list(range(NCORES)))
    return np.concatenate([r["out"] for r in res.results], axis=0).astype(np.float32)
```

```python
import math
import os
import numpy as np
import concourse.bass as bass
import concourse.mybir as mybir
from concourse.bass_utils import run_bass_kernel_spmd

F32 = mybir.dt.float32
BF16 = mybir.dt.bfloat16
ALU = mybir.AluOpType
AF = mybir.ActivationFunctionType
AX = mybir.AxisListType

D = 1024
SEQ = 2048
NT = SEQ // 128
NCORES = 8
NIT = 22
NEG = -1.0e30
WIN_COLS = 2824


class Buf:
    __slots__ = ("name", "w", "r")

    def __init__(self, name=""):
        self.name = name
        self.w = None
        self.r = {}


class Sched:
    def __init__(self, nc):
        self.nc = nc
        self.eng = {"pe": nc.tensor, "act": nc.scalar, "dve": nc.vector,
                    "pool": nc.gpsimd, "sp": nc.sync}
        self.sem = {}
        self.cnt = {}
        for k in ("pe", "act", "dve", "pool", "d_sp", "d_pool", "d_act"):
            self.sem[k] = nc.alloc_semaphore("s_" + k)
            self.cnt[k] = 0
        self.seen = {k: {} for k in self.eng}
        self.n_ins = 0

    def _deps(self, reads, writes):
        deps = {}
        for b in reads:
            if b.w is not None:
                k, v = b.w
                if v > deps.get(k, 0):
                    deps[k] = v
        for b in writes:
            if b.w is not None:
                k, v = b.w
                if v > deps.get(k, 0):
                    deps[k] = v
            for k, v in b.r.items():
                if v > deps.get(k, 0):
                    deps[k] = v
        return deps

    def _wait(self, e, deps):
        eng = self.eng[e]
        seen = self.seen[e]
        for k, v in deps.items():
            if e == "pe" and k == "pe":
                continue
            if v > seen.get(k, 0):
                eng.wait_ge(self.sem[k], v)
                seen[k] = v
                self.n_ins += 1

    def _mark(self, tok, reads, writes):
        k, v = tok
        for b in writes:
            b.w = tok
            b.r = {}
        for b in reads:
            if b in writes:
                continue
            if v > b.r.get(k, 0):
                b.r[k] = v

    def op(self, e, fn, reads=(), writes=()):
        self._wait(e, self._deps(reads, writes))
        ins = fn(self.eng[e])
        self.cnt[e] += 1
        ins.then_inc(self.sem[e], 1)
        self.n_ins += 1
        self._mark((e, self.cnt[e]), reads, writes)
        return ins

    def dma(self, q, out, in_, reads=(), writes=(), key=None, **kw):
        self._wait(q, self._deps(reads, writes))
        ins = self.eng[q].dma_start(out=out, in_=in_, **kw)
        k = "d_" + q
        if key is not None:
            k = "dk_" + key
            if k not in self.sem:
                self.sem[k] = self.nc.alloc_semaphore("s_" + k)
                self.cnt[k] = 0
        self.cnt[k] += 16
        ins.then_inc(self.sem[k], 16)
        self.n_ins += 1
        self._mark((k, self.cnt[k]), reads, writes)
        return ins

    def barrier(self):
        allv = {k: v for k, v in self.cnt.items() if v > 0}
        for e in self.eng:
            self._wait(e, allv)


class Arena:
    def __init__(self, nc, S, words):
        self.t = nc.alloc_sbuf_tensor("arena", [128, words], F32)
        self.words = words
        self.top = 0
        self.S = S

    def push(self, shape, dtype, name=""):
        n = 1
        for s in shape[1:]:
            n *= s
        nbytes = n * (4 if dtype == F32 else 2)
        w = (nbytes + 3) // 4
        w = (w + 7) // 8 * 8
        assert self.top + w <= self.words, (name, self.top, w, self.words)
        v = self.t[:, self.top:self.top + w]
        if dtype != F32:
            v = v.bitcast(dtype)
        v = v[:, 0:n]
        if len(shape) == 3:
            v = v.rearrange("p (a b) -> p a b", a=shape[1])
        elif len(shape) == 4:
            v = v.rearrange("p (a b c) -> p a b c", a=shape[1], b=shape[2])
        self.top += w
        return v, Buf(name)

    def mark(self):
        return self.top

    def release(self, m):
        self.S.barrier()
        self.top = m


def rel_bucket_np(n):
    max_exact = 16
    nf = np.maximum(n, max_exact).astype(np.float32)
    large = max_exact + (np.log(nf / max_exact) / math.log(128 / max_exact) * 16).astype(np.int32)
    large = np.minimum(large, 31)
    return np.where(n < max_exact, n, large)


def const_bidx():
    t = np.arange(128)[:, None] + 128
    s = np.arange(256)[None, :]
    d = t - s
    b = rel_bucket_np(np.maximum(d, 0)).astype(np.float32)
    return np.where(d >= 0, b, -1.0).astype(np.float32)


class _Stop(Exception):
    pass


def build(nseq=4, dbg=False, stop=None):
    nc_S = []
    try:
        _build(nseq, stop, nc_S)
    except _Stop:
        pass
    nc, S = nc_S
    S.barrier()
    return nc, S


def _build(nseq, stop, nc_S):
    nc = bass.Bass("TRN2", target_bir_lowering=False)

    def din(name, shape):
        return nc.dram_tensor(name, list(shape), F32, kind="ExternalInput").ap()

    x_d = din("x", [nseq, SEQ, D])
    c_d = din("c", [nseq, D])
    w_ada_d = din("w_ada", [D, 6 * D])
    b_ada_d = din("b_ada", [6 * D])
    g_mix_d = din("g_norm_mix", [D])
    w_in_d = din("w_in", [D, 2760])
    lam_d = [din(n, [64]) for n in ("lam_q1", "lam_k1", "lam_q2", "lam_k2")]
    g_subln_d = din("g_subln", [128])
    g_kv_d = din("g_kv_norm", [128])
    w_uv_d = din("w_uv", [4, 128, 128])
    w_out_d = din("w_out", [D, D])
    g_ffn_d = din("g_norm_ffn", [D])
    w_pq_d = din("w_peer_q", [D, D])
    keys_d = din("peer_keys", [8, 2, 128, 64])
    pu_d = din("peer_u", [16384, D])
    pv_d = din("peer_v", [16384, D])
    relb_d = din("rel_bias", [32, 8])
    g_fin_d = din("g_final", [D])
    bidx_d = din("bidx", [128, 256])
    out_d = nc.dram_tensor("out", [nseq, SEQ, D], F32, kind="ExternalOutput").ap()

    def dscr(name, shape, dt):
        return nc.dram_tensor(name, list(shape), dt, kind="Internal").ap()

    w_in_bf = dscr("w_in_bf", [D, WIN_COLS], BF16)
    w_out_bf = dscr("w_out_bf", [D, D], BF16)
    w_pq_bf = dscr("w_pq_bf", [D, D], BF16)
    UT_d = dscr("UT_d", [128, 128, D], BF16)
    Vb_d = dscr("Vb_d", [16384, D], BF16)
    mod_d = dscr("mod_d", [nseq, 6 * D], F32)
    x1_d = dscr("x1_d", [SEQ, D], F32)
    b_w_in_bf, b_w_out_bf, b_w_pq_bf = Buf(), Buf(), Buf()
    b_UT_d, b_Vb_d, b_mod_d, b_x1_d = Buf(), Buf(), Buf(), Buf()

    S = Sched(nc)
    nc_S.extend([nc, S])
    A = nc.alloc_sbuf_tensor

    def cp(n):
        if stop == n:
            raise _Stop()

    def P_(name, shape, dt):
        return A(name, shape, dt), Buf(name)

    ident, b_ident = P_("ident", [128, 128], BF16)
    w_uvb, b_w_uvb = P_("w_uvb", [128, 4, 128], BF16)
    EB, b_EB = P_("EB", [128, 8, 256], F32)
    negm, b_negm = P_("negm", [128, 128], F32)
    gsub, b_gsub = P_("gsub", [128, 128], F32)
    gkv, b_gkv = P_("gkv", [128, 128], F32)
    keysBD, b_keysBD = P_("keysBD", [128, 8, 256], BF16)
    lamt, b_lamt = P_("lamt", [128, 8], F32)
    pow2, b_pow2 = P_("pow2", [128, NIT + 1], F32)
    st, b_st = P_("st", [128, 64], F32)

    psS = nc.alloc_psum_tensor("psS", [128, 2048], F32)
    psX = nc.alloc_psum_tensor("psX", [128, 1024], F32)
    psO = nc.alloc_psum_tensor("psO", [128, 512], F32)
    psT = nc.alloc_psum_tensor("psT", [128, 1024], BF16)
    b_psS, b_psX, b_psO, b_psT = Buf("psS"), Buf("psX"), Buf("psO"), Buf("psT")

    ar = Arena(nc, S, 48600)

    def mm(out, lhsT, rhs, start, stop, reads, writes):
        S.op("pe", lambda e: e.matmul(out, lhsT=lhsT, rhs=rhs, start=start, stop=stop), reads, writes)

    def tr(out, in_, reads, writes):
        S.op("pe", lambda e: e.transpose(out, in_, ident[:in_.shape[0], :in_.shape[0]]),
             list(reads) + [b_ident], writes)

    def actf(out, in_, func, reads, writes, **kw):
        S.op("act", lambda e: e.activation(out=out, in_=in_, func=func, **kw), reads, writes)

    def acopy(out, in_, reads, writes):
        S.op("act", lambda e: e.copy(out=out, in_=in_), reads, writes)

    def vcopy(out, in_, reads, writes):
        S.op("dve", lambda e: e.tensor_copy(out, in_), reads, writes)

    def tt(out, in0, in1, op, reads, writes, eng="dve"):
        S.op(eng, lambda e: e.tensor_tensor(out=out, in0=in0, in1=in1, op=op), reads, writes)

    def ts(out, in0, s1, s2, op0, op1, reads, writes, accum=None):
        if op1 is None:
            S.op("dve", lambda e: e.tensor_scalar(out=out, in0=in0, scalar1=s1, scalar2=None, op0=op0),
                 reads, writes)
        elif accum is None:
            S.op("dve", lambda e: e.tensor_scalar(out=out, in0=in0, scalar1=s1, scalar2=s2, op0=op0, op1=op1),
                 reads, writes)
        else:
            S.op("dve", lambda e: e.tensor_scalar(out=out, in0=in0, scalar1=s1, scalar2=s2, op0=op0, op1=op1,
                                                  accum_out=accum), reads, writes)

    def stt(out, in0, scalar, in1, op0, op1, reads, writes, eng="dve"):
        S.op(eng, lambda e: e.scalar_tensor_tensor(out=out, in0=in0, scalar=scalar, in1=in1, op0=op0, op1=op1),
             reads, writes)

    def rsum(out, in_, reads, writes):
        S.op("dve", lambda e: e.reduce_sum(out=out, in_=in_, axis=AX.X), reads, writes)

    def rmax(out, in_, reads, writes):
        S.op("dve", lambda e: e.reduce_max(out=out, in_=in_, axis=AX.X), reads, writes)

    def rstd_from_ss(ss_col, tmp_col, out_col, n, bufs):
        ts(st[:, tmp_col:tmp_col + 1], st[:, ss_col:ss_col + 1], 1.0 / n, 1e-6, ALU.mult, ALU.add, bufs, bufs)
        S.op("act", lambda e: e.sqrt(out=st[:, tmp_col:tmp_col + 1], in_=st[:, tmp_col:tmp_col + 1]), bufs, bufs)
        S.op("dve", lambda e: e.reciprocal(out=st[:, out_col:out_col + 1], in_=st[:, tmp_col:tmp_col + 1]), bufs, bufs)

    def chunks(n, c=512):
        return [(a, min(n, a + c)) for a in range(0, n, c)]

    evac_flip = [0]

    def evac(out, in_, reads, writes):
        evac_flip[0] ^= 1
        if evac_flip[0]:
            acopy(out, in_, reads, writes)
        else:
            vcopy(out, in_, reads, writes)

    m0 = ar.mark()
    identf, b_identf = ar.push([128, 128], F32, "identf")
    S.op("pool", lambda e: e.memset(identf, 0.0), [], [b_identf])
    S.op("pool", lambda e: e.affine_select(out=identf, in_=identf, pattern=[[-1, 128]], compare_op=ALU.not_equal,
                                           fill=1.0, base=0, channel_multiplier=1), [b_identf], [b_identf])
    vcopy(ident[:], identf, [b_identf], [b_ident])
    S.op("pool", lambda e: e.memset(negm[:], 0.0), [], [b_negm])
    S.op("pool", lambda e: e.affine_select(out=negm[:], in_=negm[:], pattern=[[-1, 128]], compare_op=ALU.is_ge,
                                           fill=NEG, base=0, channel_multiplier=1), [b_negm], [b_negm])
    for k in range(NIT + 1):
        S.op("pool", lambda e: e.memset(pow2[:, k:k + 1], 2.0 ** -(k + 1)), [], [b_pow2])

    cp(1)
    S.dma("sp", gsub[:], g_subln_d.partition_broadcast(128), [], [b_gsub])
    S.dma("sp", gkv[:], g_kv_d.partition_broadcast(128), [], [b_gkv])
    ts(gsub[:], gsub[:], 0.8, None, ALU.mult, None, [b_gsub], [b_gsub])
    lv, b_lv = ar.push([128, 4, 64], F32, "lv")
    for q in range(4):
        S.dma("sp", lv[:, q, :], lam_d[q].partition_broadcast(128), [], [b_lv])
    tt(lv[:, 0, :], lv[:, 0, :], lv[:, 1, :], ALU.mult, [b_lv], [b_lv])
    tt(lv[:, 2, :], lv[:, 2, :], lv[:, 3, :], ALU.mult, [b_lv], [b_lv])
    rsum(lamt[:, 0:1], lv[:, 0, :], [b_lv], [b_lamt])
    rsum(lamt[:, 1:2], lv[:, 2, :], [b_lv], [b_lamt])
    actf(lamt[:, 2:4], lamt[:, 0:2], AF.Exp, [b_lamt], [b_lamt])
    tt(lamt[:, 4:5], lamt[:, 2:3], lamt[:, 3:4], ALU.subtract, [b_lamt], [b_lamt])
    ts(lamt[:, 5:6], lamt[:, 4:5], -1.0, -0.2, ALU.mult, ALU.add, [b_lamt], [b_lamt])

    cp(2)
    tab, b_tab = ar.push([128, 32, 8], F32, "tab")
    ev, b_ev = ar.push([128, 32, 8], F32, "ev")
    bidx, b_bidx = ar.push([128, 256], F32, "bidx")
    tmpe, b_tmpe = ar.push([128, 256], F32, "tmpe")
    S.dma("sp", tab.rearrange("p a b -> p (a b)"), relb_d.rearrange("a b -> (a b)").partition_broadcast(128), [], [b_tab])
    S.dma("sp", bidx, bidx_d, [], [b_bidx])
    tt(ev, tab, tab[:, 31:32, :].to_broadcast([128, 32, 8]), ALU.subtract, [b_tab], [b_ev])
    actf(ev, ev, AF.Exp, [b_ev], [b_ev])
    S.op("pool", lambda e: e.memset(EB[:], 0.0), [], [b_EB])
    for h in range(8):
        for bk in range(32):
            stt(tmpe, bidx, float(bk), ev[:, bk, h:h + 1].to_broadcast([128, 256]), ALU.is_equal, ALU.mult,
                [b_bidx, b_ev], [b_tmpe])
            tt(EB[:, h, :], EB[:, h, :], tmpe, ALU.add, [b_EB, b_tmpe], [b_EB])

    cp(3)
    wuf, b_wuf = ar.push([128, 4, 128], F32, "wuf")
    S.dma("sp", wuf, w_uv_d.rearrange("h c d -> c h d"), [], [b_wuf])
    vcopy(w_uvb[:], wuf, [b_wuf], [b_w_uvb])

    kf, b_kf = ar.push([128, 16, 64], F32, "kf")
    kb, b_kb = ar.push([128, 16, 64], BF16, "kb")
    S.dma("sp", kf, keys_d.rearrange("h p n d -> n (h p) d"), [], [b_kf])
    vcopy(kb, kf, [b_kf], [b_kb])
    S.op("pool", lambda e: e.memset(keysBD[:], 0.0), [], [b_keysBD])
    for h in range(8):
        tr(psT[:, 0:128], kb[:, 2 * h:2 * h + 2, :].rearrange("p a b -> p (a b)"), [b_kb], [b_psT])
        acopy(keysBD[0:64, h, 0:128], psT[0:64, 0:128], [b_psT], [b_keysBD])
        acopy(keysBD[64:128, h, 128:256], psT[64:128, 0:128], [b_psT], [b_keysBD])

    cp(4)
    wst = [ar.push([128, 2760], F32, "wst%d" % i) for i in range(2)]
    wsb = [ar.push([128, 2760], BF16, "wsb%d" % i) for i in range(2)]
    cnt = 0
    for (src, dst, b_dst, ncol) in ((w_in_d, w_in_bf, b_w_in_bf, 2760), (w_out_d, w_out_bf, b_w_out_bf, D),
                                    (w_pq_d, w_pq_bf, b_w_pq_bf, D)):
        for k in range(8):
            (f, bf), (b, bb) = wst[cnt % 2], wsb[cnt % 2]
            cnt += 1
            S.dma("sp", f[:, 0:ncol], src[k * 128:(k + 1) * 128, :], [], [bf])
            evac(b[:, 0:ncol], f[:, 0:ncol], [bf], [bb])
            if ncol == 2760:
                S.dma("sp", dst[k * 128:(k + 1) * 128, 0:2752], b[:, 0:2752], [bb], [b_dst])
                S.dma("sp", dst[k * 128:(k + 1) * 128, 2752:2816], b[:, 2688:2752], [bb], [b_dst])
                S.dma("sp", dst[k * 128:(k + 1) * 128, 2816:2824], b[:, 2752:2760], [bb], [b_dst])
            else:
                S.dma("sp", dst[k * 128:(k + 1) * 128, :], b[:, 0:ncol], [bb], [b_dst])
    ar.release(m0)

    cp(5)
    m0 = ar.mark()
    cT, b_cT = ar.push([128, nseq, 8], F32, "cT")
    cTb, b_cTb = ar.push([128, 8, nseq], BF16, "cTb")
    bada, b_bada = ar.push([nseq, 6 * D], F32, "bada")
    modsb, b_modsb = ar.push([nseq, 6 * D], F32, "modsb")
    for b in range(nseq):
        S.dma("sp", cT[:, b, :], c_d[b].rearrange("(k p) -> p k", p=128), [], [b_cT], allow_slow_non_contiguous=True)
    S.dma("sp", bada[0:nseq, :], b_ada_d.partition_broadcast(nseq), [], [b_bada])
    actf(cT, cT, AF.Silu, [b_cT], [b_cT])
    vcopy(cTb.rearrange("p k b -> p b k"), cT, [b_cT], [b_cTb])
    waf = [ar.push([128, 8, 512], F32, "waf%d" % i) for i in range(2)]
    wab = [ar.push([128, 8, 512], BF16, "wab%d" % i) for i in range(2)]
    w_ada_v = w_ada_d.rearrange("(k p) n -> p k n", p=128)
    for n in range(12):
        (f, bf), (b, bb) = waf[n % 2], wab[n % 2]
        S.dma("sp", f, w_ada_v[:, :, n * 512:(n + 1) * 512], [], [bf])
        evac(b, f, [bf], [bb])
        for k in range(8):
            mm(psX[0:nseq, 0:512], cTb[:, k, :], b[:, k, :], k == 0, k == 7, [b_cTb, bb], [b_psX])
        tt(modsb[0:nseq, n * 512:(n + 1) * 512], psX[0:nseq, 0:512], bada[0:nseq, n * 512:(n + 1) * 512], ALU.add,
           [b_psX, b_bada], [b_modsb])
    S.dma("sp", mod_d, modsb[0:nseq, :], [b_modsb], [b_mod_d])
    ar.release(m0)

    cp(6)
    m0 = ar.mark()
    uf = [ar.push([128, D], F32, "uf%d" % i) for i in range(2)]
    vf = [ar.push([128, D], F32, "vf%d" % i) for i in range(2)]
    ub = [ar.push([128, D], BF16, "ub%d" % i) for i in range(2)]
    uts = [ar.push([128, D], BF16, "uts%d" % i) for i in range(2)]
    vb = [ar.push([128, D], BF16, "vb%d" % i) for i in range(2)]
    import os
    for blk in range(int(os.environ.get('NBLK', '128'))):
        s_ = blk % 2
        S.dma("sp", uf[s_][0], pu_d[blk * 128:(blk + 1) * 128, :], [], [uf[s_][1]])
        S.dma("sp", vf[s_][0], pv_d[blk * 128:(blk + 1) * 128, :], [], [vf[s_][1]])
        vcopy(ub[s_][0], uf[s_][0], [uf[s_][1]], [ub[s_][1]])
        for k in range(8):
            tr(psT[:, k * 128:(k + 1) * 128], ub[s_][0][:, k * 128:(k + 1) * 128], [ub[s_][1]], [b_psT])
        acopy(uts[s_][0], psT[:], [b_psT], [uts[s_][1]])
        S.dma("sp", UT_d[blk], uts[s_][0], [uts[s_][1]], [b_UT_d])
        acopy(vb[s_][0], vf[s_][0], [vf[s_][1]], [vb[s_][1]])
        S.dma("sp", Vb_d[blk * 128:(blk + 1) * 128, :], vb[s_][0], [vb[s_][1]], [b_Vb_d])
    ar.release(m0)

    cp(7)
    w_in_v = w_in_bf.rearrange("(k p) n -> p k n", p=128)
    w_out_v = w_out_bf.rearrange("(k p) n -> p k n", p=128)
    w_pq_v = w_pq_bf.rearrange("(k p) n -> p k n", p=128)

    def transposes_to(dst_fn, src, ntile, reads, writes):
        for g in range(0, ntile, 8):
            n = min(8, ntile - g)
            for q in range(n):
                jj = g + q
                tr(psT[:, q * 128:(q + 1) * 128], src[:, jj * 128:(jj + 1) * 128], reads, [b_psT])
            evac(dst_fn(g, n), psT[:, 0:n * 128], [b_psT], writes)

    for b in range(nseq):
        mseq = ar.mark()
        xin = [ar.push([128, D], F32, "xin%d" % i) for i in range(2)]
        junk, b_junk = ar.push([128, D], F32, "junk")
        mixT, b_mixT = ar.push([128, 8, SEQ], BF16, "mixT")
        hT, b_hT = ar.push([128, 8, SEQ], BF16, "hT")

        m1 = ar.mark()
        moda, b_moda = ar.push([128, 2, D], F32, "moda")
        gtmp, b_gtmp = ar.push([128, D], F32, "gtmp")
        xn, b_xn = ar.push([128, D], F32, "xn")
        hb, b_hb = ar.push([128, D], BF16, "hb")
        S.dma("sp", moda.rearrange("p a b -> p (a b)"), mod_d[b, 0:2 * D].partition_broadcast(128), [b_mod_d], [b_moda])
        S.dma("sp", gtmp, g_mix_d.partition_broadcast(128), [], [b_gtmp])
        stt(moda[:, 1, :], moda[:, 1, :], 1.0, gtmp, ALU.add, ALU.mult, [b_moda, b_gtmp], [b_moda])
        for j in range(NT):
            xt, b_xt = xin[j % 2]
            S.dma("sp", xt, x_d[b, j * 128:(j + 1) * 128, :], [], [b_xt], key="xin%d" % (j % 2))
            actf(junk, xt, AF.Square, [b_xt], [b_junk])
            rsum(st[:, 0:1], junk, [b_junk], [b_st])
            rstd_from_ss(0, 1, 2, D, [b_st])
            actf(xn, xt, AF.Copy, [b_xt, b_st], [b_xn], scale=st[:, 2:3])
            tt(xn, xn, moda[:, 1, :], ALU.mult, [b_xn, b_moda], [b_xn])
            tt(hb, xn, moda[:, 0, :], ALU.add, [b_xn, b_moda], [b_hb])
            for k in range(8):
                tr(psT[:, k * 128:(k + 1) * 128], hb[:, k * 128:(k + 1) * 128], [b_hb], [b_psT])
            evac(hT[:, :, j * 128:(j + 1) * 128], psT[:].rearrange("p (k t) -> p k t", k=8), [b_psT], [b_hT])
        ar.release(m1)

        cp(8)
        m2 = ar.mark()
        sqT, b_sqT = ar.push([128, 4, SEQ], BF16, "sqT")
        iqT, b_iqT = ar.push([128, 4, SEQ], BF16, "iqT")
        ik2, b_ik2 = ar.push([128, SEQ], BF16, "ik2")
        kvT, b_kvT = ar.push([128, SEQ], BF16, "kvT")
        kvA, b_kvA = ar.push([128, NT, 130], BF16, "kvA")
        iws, b_iws = ar.push([128, NT, 8], F32, "iws")
        m2b = ar.mark()
        wsl, b_wsl = ar.push([128, 8, 1288], BF16, "wsl")
        kf32, b_kf32 = ar.push([128, 128], F32, "kf32")
        S.dma("sp", wsl, w_in_v[:, :, 1536:2824], [b_w_in_bf], [b_wsl])
        S.op("pool", lambda e: e.memset(kvA[:, :, 128:130], 1.0), [], [b_kvA])
        for tc in range(0 if os.environ.get('SKIP_A') else 4):
            cs = slice(tc * 512, (tc + 1) * 512)
            for (dst, b_dst, off) in ([(sqT[:, h, cs], b_sqT, h * 128) for h in range(4)] +
                                      [(iqT[:, c, cs], b_iqT, 640 + c * 128) for c in range(4)] +
                                      [(ik2[:, cs], b_ik2, 1152)]):
                for k in range(8):
                    mm(psX[:, 0:512], wsl[:, k, off:off + 128], hT[:, k, cs], k == 0, k == 7, [b_wsl, b_hT], [b_psX])
                evac(dst, psX[:, 0:512], [b_psX], [b_dst])
        kfall, b_kfall = ar.push([128, NT, 128], F32, "kfall")
        for j in range(NT):
            js = slice(j * 128, (j + 1) * 128)
            for k in range(8):
                mm(psO[:, 0:128], hT[:, k, js], wsl[:, k, 512:640], k == 0, k == 7, [b_wsl, b_hT], [b_psO])
            for k in range(8):
                mm(psO[:, 128:264], hT[:, k, js], wsl[:, k, 1152:1288], k == 0, k == 7, [b_wsl, b_hT], [b_psO])
            ts(iws[:, j, :], psO[:, 256:264], (1.0 / 8.0) * 8.0 ** -0.5, None, ALU.mult, None, [b_psO], [b_iws])
            vcopy(kfall[:, j, :], psO[:, 0:128], [b_psO], [b_kfall])
        for hf in range(2):
            actf(junk.rearrange("p (a b) -> p a b", a=8), kfall[:, hf * 8:(hf + 1) * 8, :], AF.Square, [b_kfall], [b_junk])
            rsum(st[:, 40 + hf * 8:48 + hf * 8], junk.rearrange("p (a b) -> p a b", a=8), [b_junk], [b_st])
        ts(st[:, 40:56], st[:, 40:56], 1.0 / 128, 1e-6, ALU.mult, ALU.add, [b_st], [b_st])
        S.op("act", lambda e: e.sqrt(out=st[:, 40:56], in_=st[:, 40:56]), [b_st], [b_st])
        S.op("dve", lambda e: e.reciprocal(out=st[:, 40:56], in_=st[:, 40:56]), [b_st], [b_st])
        for j in range(0 if os.environ.get('SKIP_C') else NT):
            js = slice(j * 128, (j + 1) * 128)
            stt(kvA[:, j, 0:128], kfall[:, j, :], st[:, 40 + j:41 + j], gkv[:], ALU.mult, ALU.mult,
                [b_kfall, b_gkv, b_st], [b_kvA])
            tr(psT[:, 0:128], kvA[:, j, 0:128], [b_kvA], [b_psT])
            evac(kvT[:, js], psT[:, 0:128], [b_psT], [b_kvT])
        ar.release(m2b)
        Pm, b_Pm = ar.push([128, SEQ], F32, "Pm")
        acc, b_acc = ar.push([128, SEQ], F32, "acc")
        Rt, b_Rt = ar.push([128, SEQ], F32, "Rt")
        Rt2, b_Rt2 = ar.push([128, SEQ], F32, "Rt2")
        cntk, b_cntk = ar.push([128, NIT], F32, "cntk")
        Mk, b_Mk = ar.push([128, SEQ], BF16, "Mk")
        Ab, b_Ab = ar.push([128, SEQ], BF16, "Ab")
        AT, b_AT = ar.push([128, NT, 128], BF16, "AT")
        osn, b_osn = ar.push([128, 128], BF16, "osn")
        osT, b_osT = ar.push([128, 128], BF16, "osT")
        steps, b_steps = ar.push([128, NIT + 1], F32, "steps")
        sc_dsa = 128.0 ** -0.5
        import os
        for i in range(int(os.environ.get('NTQ', str(NT)))):
            Si = (i + 1) * 128
            isl = slice(i * 128, (i + 1) * 128)
            for ih in range(8):
                c2, hf = ih // 2, ih % 2
                ps_ = slice(hf * 64, (hf + 1) * 64)
                for (a0, a1) in chunks(Si):
                    mm(psS[:, a0:a1], iqT[ps_, c2, isl], ik2[ps_, a0:a1], True, True, [b_iqT, b_ik2], [b_psS])
                Rq, b_Rq = (Rt, b_Rt) if ih % 2 == 0 else (Rt2, b_Rt2)
                actf(Rq[:, 0:Si], psS[:, 0:Si], AF.Relu, [b_psS], [b_Rq])
                if ih == 0:
                    ts(acc[:, 0:Si], Rq[:, 0:Si], iws[:, i, 0:1], None, ALU.mult, None, [b_Rq, b_iws], [b_acc])
                else:
                    stt(acc[:, 0:Si], Rq[:, 0:Si], iws[:, i, ih:ih + 1], acc[:, 0:Si], ALU.mult, ALU.add,
                        [b_Rq, b_iws, b_acc], [b_acc])
            if i >= 2:
                S.op("dve", lambda e: e.tensor_reduce(out=st[:, 8:9], in_=acc[:, 0:Si], axis=AX.X, op=ALU.min),
                     [b_acc], [b_st])
                rmax(st[:, 9:10], acc[:, 0:Si], [b_acc], [b_st])
                tt(st[:, 10:11], st[:, 9:10], st[:, 8:9], ALU.subtract, [b_st], [b_st])
                ts(steps, pow2[:], st[:, 10:11], None, ALU.mult, None, [b_pow2, b_st], [b_steps])
            tt(acc[:, isl], acc[:, isl], negm[:], ALU.add, [b_acc, b_negm], [b_acc])
            if i >= 2:
                S.op("dve", lambda e: e.memset(cntk, 0.0), [], [b_cntk])
                tt(st[:, 11:12], st[:, 8:9], steps[:, 0:1], ALU.add, [b_st, b_steps], [b_st])
                for k in range(NIT):
                    ts(Rt[:, 0:Si], acc[:, 0:Si], st[:, 11:12], 0.0, ALU.is_ge, ALU.add, [b_acc, b_st], [b_Rt, b_cntk],
                       accum=cntk[:, k:k + 1])
                    stt(st[:, 13:14], cntk[:, k:k + 1], 256.0, steps[:, k:k + 1], ALU.is_ge, ALU.mult,
                        [b_cntk, b_steps], [b_st])
                    stt(st[:, 11:12], st[:, 11:12], steps[:, k + 1:k + 2], st[:, 13:14], ALU.subtract, ALU.add,
                        [b_st, b_steps], [b_st])
                tt(st[:, 8:9], st[:, 11:12], steps[:, NIT:NIT + 1], ALU.subtract, [b_st, b_steps], [b_st])
            else:
                S.op("dve", lambda e: e.memset(st[:, 8:9], -1.0e29), [], [b_st])
            ts(Mk[:, 0:Si], acc[:, 0:Si], st[:, 8:9], None, ALU.is_ge, None, [b_acc, b_st], [b_Mk])
            lo = max(0, (i - 1) * 128)
            for h in range(4):
                for (a0, a1) in chunks(Si):
                    mm(psS[:, a0:a1], sqT[:, h, isl], kvT[:, a0:a1], True, True, [b_sqT, b_kvT], [b_psS])
                rmax(st[:, 16:17], psS[:, 0:Si], [b_psS], [b_st])
                ts(st[:, 17:18], st[:, 16:17], -sc_dsa, None, ALU.mult, None, [b_st], [b_st])
                actf(Pm[:, 0:Si], psS[:, 0:Si], AF.Exp, [b_psS, b_st], [b_Pm], bias=st[:, 17:18], scale=sc_dsa)
                ebv = EB[:, 4 + h, 128:256] if i == 0 else EB[:, 4 + h, 0:256]
                tt(Pm[:, lo:Si], Pm[:, lo:Si], ebv, ALU.mult, [b_Pm, b_EB], [b_Pm])
                tt(Ab[:, 0:Si], Pm[:, 0:Si], Mk[:, 0:Si], ALU.mult, [b_Pm, b_Mk], [b_Ab])
                transposes_to(lambda g, n: AT[:, g:g + n, :].rearrange("p a b -> p (a b)"), Ab, i + 1, [b_Ab], [b_AT])
                for jj in range(i + 1):
                    mm(psO[:, 0:129], AT[:, jj, :], kvA[:, jj, 0:129], jj == 0, jj == i, [b_AT, b_kvA], [b_psO])
                S.op("dve", lambda e: e.reciprocal(out=st[:, 18:19], in_=psO[:, 128:129]), [b_psO], [b_st])
                ts(osn, psO[:, 0:128], st[:, 18:19], None, ALU.mult, None, [b_psO, b_st], [b_osn])
                tr(psT[:, 0:128], osn, [b_osn], [b_psT])
                evac(osT, psT[:, 0:128], [b_psT], [b_osT])
                mm(psX[:, 0:128], w_uvb[:, h, :], osT, True, True, [b_w_uvb, b_osT], [b_psX])
                evac(mixT[:, 4 + h, isl], psX[:, 0:128], [b_psX], [b_mixT])
        ar.release(m2)

        cp(9)
        sc_diff = 64.0 ** -0.5
        for h in range(4):
            m3 = ar.mark()
            wsl, b_wsl = ar.push([128, 8, 384], BF16, "wsl3")
            qT, b_qT = ar.push([128, SEQ], BF16, "qT")
            kT, b_kT = ar.push([128, SEQ], BF16, "kT")
            Vd, b_Vd = ar.push([128, NT, 128], BF16, "Vd")
            Pm0, b_Pm0 = ar.push([128, SEQ], F32, "Pm0")
            Pm1, b_Pm1 = ar.push([128, SEQ], F32, "Pm1")
            Ab, b_Ab = ar.push([128, SEQ], BF16, "Ab3")
            AT, b_AT = ar.push([128, NT, 128], BF16, "AT3")
            of32, b_of32 = ar.push([128, 128], F32, "of32")
            odb, b_odb = ar.push([128, 128], BF16, "odb")
            for q in range(3):
                S.dma("sp", wsl[:, :, q * 128:(q + 1) * 128], w_in_v[:, :, q * 512 + h * 128:q * 512 + (h + 1) * 128],
                      [b_w_in_bf], [b_wsl])
            for tc in range(4):
                cs = slice(tc * 512, (tc + 1) * 512)
                for (dst, b_dst, off) in ((qT[:, cs], b_qT, 0), (kT[:, cs], b_kT, 128)):
                    for k in range(8):
                        mm(psX[:, 0:512], wsl[:, k, off:off + 128], hT[:, k, cs], k == 0, k == 7, [b_wsl, b_hT], [b_psX])
                    evac(dst, psX[:, 0:512], [b_psX], [b_dst])
            for j in range(NT):
                js = slice(j * 128, (j + 1) * 128)
                for k in range(8):
                    mm(psO[:, 0:128], hT[:, k, js], wsl[:, k, 256:384], k == 0, k == 7, [b_wsl, b_hT], [b_psO])
                vcopy(Vd[:, j, :], psO[:, 0:128], [b_psO], [b_Vd])
            Pms = ((Pm0, b_Pm0), (Pm1, b_Pm1))
            for i in range(NT):
                Si = (i + 1) * 128
                isl = slice(i * 128, (i + 1) * 128)
                lo = max(0, (i - 1) * 128)
                ebv = EB[:, h, 128:256] if i == 0 else EB[:, h, 0:256]
                for m in range(2):
                    Pmm, b_Pmm = Pms[m]
                    ps_ = slice(m * 64, (m + 1) * 64)
                    for (a0, a1) in chunks(Si):
                        mm(psS[:, a0:a1], qT[ps_, isl], kT[ps_, a0:a1], True, True, [b_qT, b_kT], [b_psS])
                    rmax(st[:, 20 + m:21 + m], psS[:, 0:Si], [b_psS], [b_st])
                    ts(st[:, 22 + m:23 + m], st[:, 20 + m:21 + m], -sc_diff, None, ALU.mult, None, [b_st], [b_st])
                    actf(Pmm[:, 0:Si], psS[:, 0:Si], AF.Exp, [b_psS, b_st], [b_Pmm], bias=st[:, 22 + m:23 + m],
                         scale=sc_diff)
                    tt(Pmm[:, lo:Si], Pmm[:, lo:Si], ebv, ALU.mult, [b_Pmm, b_EB], [b_Pmm])
                    rsum(st[:, 24 + m:25 + m], Pmm[:, 0:Si], [b_Pmm], [b_st])
                    S.op("dve", lambda e: e.reciprocal(out=st[:, 26 + m:27 + m], in_=st[:, 24 + m:25 + m]), [b_st], [b_st])
                tt(st[:, 28:29], st[:, 27:28], lamt[:, 5:6], ALU.mult, [b_st, b_lamt], [b_st])
                actf(Pm0[:, 0:Si], Pm0[:, 0:Si], AF.Copy, [b_Pm0, b_st], [b_Pm0], scale=st[:, 26:27])
                stt(Ab[:, 0:Si], Pm1[:, 0:Si], st[:, 28:29], Pm0[:, 0:Si], ALU.mult, ALU.add, [b_Pm0, b_Pm1, b_st], [b_Ab])
                transposes_to(lambda g, n: AT[:, g:g + n, :].rearrange("p a b -> p (a b)"), Ab, i + 1, [b_Ab], [b_AT])
                for jj in range(i + 1):
                    mm(psO[:, 0:128], AT[:, jj, :], Vd[:, jj, :], jj == 0, jj == i, [b_AT, b_Vd], [b_psO])
                vcopy(of32, psO[:, 0:128], [b_psO], [b_of32])
                actf(junk[:, 0:128], of32, AF.Square, [b_of32], [b_junk])
                rsum(st[:, 30:31], junk[:, 0:128], [b_junk], [b_st])
                rstd_from_ss(30, 31, 32, 128, [b_st])
                stt(odb, of32, st[:, 32:33], gsub[:], ALU.mult, ALU.mult, [b_of32, b_gsub, b_st], [b_odb])
                tr(psT[:, 0:128], odb, [b_odb], [b_psT])
                evac(mixT[:, h, isl], psT[:, 0:128], [b_psT], [b_mixT])
            ar.release(m3)

        cp(10)
        m4 = ar.mark()
        w_outb, b_w_outb = ar.push([128, 8, D], BF16, "w_outb")
        gate_a, b_gate_a = ar.push([128, D], F32, "gate_a")
        x1t = [ar.push([128, D], F32, "x1t%d" % i) for i in range(2)]
        S.dma("sp", w_outb, w_out_v, [b_w_out_bf], [b_w_outb])
        S.dma("sp", gate_a, mod_d[b, 2 * D:3 * D].partition_broadcast(128), [b_mod_d], [b_gate_a])
        for j in range(NT):
            js = slice(j * 128, (j + 1) * 128)
            xt, b_xt = xin[j % 2]
            x1, b_x1 = x1t[j % 2]
            S.dma("sp", xt, x_d[b, js, :], [], [b_xt], key="xin%d" % (j % 2))
            for hf in range(2):
                for ch in range(8):
                    mm(psX[:, hf * 512:(hf + 1) * 512], mixT[:, ch, js], w_outb[:, ch, hf * 512:(hf + 1) * 512],
                       ch == 0, ch == 7, [b_mixT, b_w_outb], [b_psX])
            tt(x1, psX[:], gate_a, ALU.mult, [b_psX, b_gate_a], [b_x1])
            tt(x1, x1, xt, ALU.add, [b_x1, b_xt], [b_x1])
            S.dma("sp", x1_d[js, :], x1, [b_x1], [b_x1_d])
        ar.release(mseq)

        cp(11)
        m5 = ar.mark()
        w_pqb, b_w_pqb = ar.push([128, 8, D], BF16, "w_pqb")
        modf, b_modf = ar.push([128, 3, D], F32, "modf")
        gfin, b_gfin = ar.push([128, D], F32, "gfin")
        junk, b_junk = ar.push([128, D], F32, "junk5")
        x1g = [ar.push([128, D], F32, "x1g%d" % i) for i in range(2)]
        xn, b_xn = ar.push([128, D], F32, "xn5")
        hb, b_hb = ar.push([128, D], BF16, "hb5")
        h2T, b_h2T = ar.push([128, 8, 256], BF16, "h2T")
        pqT, b_pqT = ar.push([128, 8, 256], BF16, "pqT")
        scs, b_scs = ar.push([128, 2048], F32, "scs")
        LOOKAHEAD = os.environ.get("LOOKAHEAD", "1") == "1"
        NB = 3
        ets = [ar.push([128, 8, 128], F32, "et%d" % i) for i in range(NB)]
        Wh = [ar.push([128, 8, 128], BF16, "Wh%d" % i) for i in range(NB)]
        Wcs = [ar.push([128, 8, 128], BF16, "Wc%d" % i) for i in range(2)]
        sms = [ar.push([128, 16], F32, "sm%d" % i) for i in range(2)]
        b_psZ = [Buf("psZ0"), Buf("psZ1")]
        WT, b_WT = ar.push([128, 128, 256], BF16, "WT")
        UTb = [ar.push([128, 8, 128], BF16, "UTb%d" % i) for i in range(3)]
        Vbb = [ar.push([128, D], BF16, "Vbb%d" % i) for i in range(3)]
        Gt = [ar.push([128, 256], F32, "Gt%d" % i) for i in range(2)]
        GW = [ar.push([128, 256], BF16, "GW%d" % i) for i in range(2)]
        s16, b_s16 = ar.push([128, 16, 16], F32, "s16")
        cand, b_cand = ar.push([128, 8, 256], F32, "cand")
        wk, b_wk = ar.push([128, 256], F32, "wk")
        topv, b_topv = ar.push([128, 8, 16], F32, "topv")
        dv_, b_dv = ar.push([128, 8, 16], F32, "dv")
        zz, b_zz = ar.push([128, 16], F32, "zz")
        S.dma("sp", w_pqb, w_pq_v, [b_w_pq_bf], [b_w_pqb])
        S.dma("sp", modf.rearrange("p a b -> p (a b)"), mod_d[b, 3 * D:6 * D].partition_broadcast(128), [b_mod_d], [b_modf])
        S.dma("sp", gfin, g_ffn_d.partition_broadcast(128), [], [b_gfin])
        stt(modf[:, 1, :], modf[:, 1, :], 1.0, gfin, ALU.add, ALU.mult, [b_modf, b_gfin], [b_modf])
        S.dma("sp", gfin, g_fin_d.partition_broadcast(128), [], [b_gfin])
        sc4 = scs.rearrange("p (h q n) -> p h q n", h=8, q=2)
        s4 = s16.rearrange("p (h q) r -> p h q r", q=2)
        for g in range(SEQ // 256):
            for jj in range(2):
                j = 2 * g + jj
                x1, b_x1 = x1g[jj]
                S.dma("sp", x1, x1_d[j * 128:(j + 1) * 128, :], [b_x1_d], [b_x1], key="x1g%d" % jj)
                actf(junk, x1, AF.Square, [b_x1], [b_junk])
                rsum(st[:, 0:1], junk, [b_junk], [b_st])
                rstd_from_ss(0, 1, 2, D, [b_st])
                actf(xn, x1, AF.Copy, [b_x1, b_st], [b_xn], scale=st[:, 2:3])
                tt(xn, xn, modf[:, 1, :], ALU.mult, [b_xn, b_modf], [b_xn])
                tt(hb, xn, modf[:, 0, :], ALU.add, [b_xn, b_modf], [b_hb])
                for k in range(8):
                    tr(psT[:, k * 128:(k + 1) * 128], hb[:, k * 128:(k + 1) * 128], [b_hb], [b_psT])
                evac(h2T[:, :, jj * 128:(jj + 1) * 128], psT[:].rearrange("p (k t) -> p k t", k=8), [b_psT], [b_h2T])
            for hh in range(8):
                for k in range(8):
                    mm(psX[:, 0:256], w_pqb[:, k, hh * 128:(hh + 1) * 128], h2T[:, k, :], k == 0, k == 7,
                       [b_w_pqb, b_h2T], [b_psX])
                evac(pqT[:, hh, :], psX[:, 0:256], [b_psX], [b_pqT])
            def prep_items(jj, via_psO):
                tsl = slice(jj * 128, (jj + 1) * 128)
                sm, b_sm = sms[jj]
                items = []
                if via_psO:
                    def f_sc(qd):
                        for u in range(2):
                            hh = 2 * qd + u
                            mm(psO[:, u * 256:(u + 1) * 256], pqT[:, hh, tsl], keysBD[:, hh, :], True, True,
                               [b_pqT, b_keysBD], [b_psO])
                        vcopy(scs[:, qd * 512:(qd + 1) * 512], psO[:], [b_psO], [b_scs])
                    items += [(lambda qd=qd: f_sc(qd)) for qd in range(4)]
                else:
                    def f_sc_all():
                        for hh in range(8):
                            mm(psS[:, hh * 256:(hh + 1) * 256], pqT[:, hh, tsl], keysBD[:, hh, :], True, True,
                               [b_pqT, b_keysBD], [b_psS] + b_psZ)
                        acopy(scs[:, 0:1024], psS[:, 0:1024], [b_psS], [b_scs])
                        vcopy(scs[:, 1024:2048], psS[:, 1024:2048], [b_psS], [b_scs])
                    items.append(f_sc_all)

                def f_hp(hp):
                    src = scs[:, hp * 128:(hp + 1) * 128]
                    S.op("dve", lambda e: e.max(out=s16[:, hp, 0:8], in_=src), [b_scs], [b_s16])
                    S.op("dve", lambda e: e.match_replace(out=wk[:, 0:128], in_to_replace=s16[:, hp, 0:8],
                                                          in_values=src, imm_value=NEG), [b_scs, b_s16], [b_wk])
                    S.op("dve", lambda e: e.max(out=s16[:, hp, 8:16], in_=wk[:, 0:128]), [b_wk], [b_s16])
                items += [(lambda hp=hp: f_hp(hp)) for hp in range(16)]

                def f_cand():
                    tt(cand.rearrange("p h (i j) -> p h i j", i=16),
                       s4[:, :, 0, :].unsqueeze(3).to_broadcast([128, 8, 16, 16]),
                       s4[:, :, 1, :].unsqueeze(2).to_broadcast([128, 8, 16, 16]), ALU.add, [b_s16], [b_cand])
                items.append(f_cand)

                def f_top(hh):
                    S.op("dve", lambda e: e.max(out=topv[:, hh, 0:8], in_=cand[:, hh, :]), [b_cand], [b_topv])
                    S.op("dve", lambda e: e.match_replace(out=wk[:, 0:256], in_to_replace=topv[:, hh, 0:8],
                                                          in_values=cand[:, hh, :], imm_value=NEG),
                         [b_cand, b_topv], [b_wk])
                    S.op("dve", lambda e: e.max(out=topv[:, hh, 8:16], in_=wk[:, 0:256]), [b_wk], [b_topv])
                items += [(lambda hh=hh: f_top(hh)) for hh in range(8)]

                def f_fin():
                    vcopy(zz[:, 0:8], topv[:, :, 15], [b_topv], [b_zz])
                    tt(dv_, topv, zz[:, 0:8].unsqueeze(2).to_broadcast([128, 8, 16]), ALU.subtract, [b_topv, b_zz], [b_dv])
                    actf(dv_, dv_, AF.Exp, [b_dv], [b_dv])
                    rsum(zz[:, 8:16], dv_, [b_dv], [b_zz])
                    S.op("dve", lambda e: e.reciprocal(out=sm[:, 8:16], in_=zz[:, 8:16]), [b_zz], [b_sm])
                    ts(sm[:, 8:16], sm[:, 8:16], 1.0 - 1.0e-4, None, ALU.mult, None, [b_sm], [b_sm])
                    actf(zz[:, 8:16], zz[:, 8:16], AF.Ln, [b_zz], [b_zz])
                    stt(sm[:, 0:8], zz[:, 8:16], -1.0, zz[:, 0:8], ALU.mult, ALU.subtract, [b_zz], [b_sm])
                items.append(f_fin)
                return items

            def wbuild(jj, queue):
                tsl = slice(jj * 128, (jj + 1) * 128)
                sm, b_sm = sms[jj]
                steps = [(ci, hh) for ci in range(16) for hh in range(8)]
                NSTEP = len(steps)

                def fzp(gi):
                    ci, hh = steps[gi]
                    k2 = gi % 2
                    wr = [b_psZ[k2]] + ([b_psS] if gi < 2 else [])
                    for q in range(2):
                        o_ = psS[:, k2 * 1024 + q * 512:k2 * 1024 + (q + 1) * 512]
                        n0 = ci * 8 + q * 4
                        mm(o_, pqT[:, hh, tsl], keysBD[:, hh, 128:256].unsqueeze(1).to_broadcast([128, 4, 128]), True, False,
                           [b_pqT, b_keysBD], wr)
                        mm(o_, pqT[:, hh, tsl], keysBD[:, hh, n0:n0 + 4].unsqueeze(2).to_broadcast([128, 4, 128]), False, True,
                           [b_pqT, b_keysBD], wr)

                def fe(gi):
                    ci, hh = steps[gi]
                    k2 = gi % 2
                    et, b_et = ets[gi % NB]
                    actf(et.rearrange("p a b -> p (a b)"), psS[:, k2 * 1024:(k2 + 1) * 1024], AF.Exp, [b_psZ[k2], b_sm], [b_et],
                         bias=sm[:, hh:hh + 1])

                def fs(gi):
                    ci, hh = steps[gi]
                    et, b_et = ets[gi % NB]
                    Whh, b_Whh = Wh[gi % NB]
                    stt(Whh, et, sm[:, 8 + hh:9 + hh], et, ALU.is_ge, ALU.mult, [b_et, b_sm], [b_Whh])
                    Whf = Whh.rearrange("p a b -> p (a b)")
                    for q in range(2):
                        mm(psX[:, q * 512:(q + 1) * 512], ident[:], Whf[:, q * 512:(q + 1) * 512], hh == 0, hh == 7,
                           [b_ident, b_Whh], [b_psX])
                    if hh == 7:
                        Wc, b_Wc = Wcs[ci % 2]
                        acopy(Wc.rearrange("p a b -> p (a b)"), psX[:], [b_psX], [b_Wc])
                        for q in range(8):
                            tr(psT[:, q * 128:(q + 1) * 128], Wc[:, q, :], [b_Wc], [b_psT])
                        acopy(WT[:, ci * 8:(ci + 1) * 8, tsl], psT[:].rearrange("p (a b) -> p a b", a=8), [b_psT], [b_WT])
                for sidx in range(NSTEP + 2):
                    if sidx < NSTEP:
                        fzp(sidx)
                    if 0 <= sidx - 1 < NSTEP:
                        fe(sidx - 1)
                    if 0 <= sidx - 2 < NSTEP:
                        fs(sidx - 2)
                    if queue and sidx % 3 == 2:
                        queue.pop(0)()
                while queue:
                    queue.pop(0)()

            for f in prep_items(0, False):
                f()
            wbuild(0, prep_items(1, True) if not os.environ.get("NO_PREP_OVERLAP") else [])
            if os.environ.get("NO_PREP_OVERLAP"):
                for f in prep_items(1, True):
                    f()
            wbuild(1, [])
            def mA(n1):
                (ut, b_ut), (vt, b_vt) = UTb[n1 % 3], Vbb[n1 % 3]
                S.dma("sp", ut.rearrange("p a b -> p (a b)"), UT_d[n1], [b_UT_d], [b_ut], key="ut%d" % (n1 % 3))
                S.dma("sp", vt, Vb_d[n1 * 128:(n1 + 1) * 128, :], [b_Vb_d], [b_vt], key="vt%d" % (n1 % 3))
                pa = psX[:, (n1 % 2) * 512:(n1 % 2) * 512 + 256]
                for k in range(8):
                    mm(pa, ut[:, k, :], h2T[:, k, :], k == 0, k == 7, [b_ut, b_h2T], [b_psX])
                Gn, b_Gn = Gt[n1 % 2]
                actf(Gn, pa, AF.Gelu, [b_psX], [b_Gn])

            def mB(n1):
                (vt, b_vt) = Vbb[n1 % 3]
                Gn, b_Gn = Gt[n1 % 2]
                GWn, b_GWn = GW[n1 % 2]
                tt(GWn, Gn, WT[:, n1, :], ALU.mult, [b_Gn, b_WT], [b_GWn])
                for jj in range(2):
                    for hf in range(2):
                        mm(psS[:, jj * 1024 + hf * 512:jj * 1024 + (hf + 1) * 512], GWn[:, jj * 128:(jj + 1) * 128],
                           vt[:, hf * 512:(hf + 1) * 512], n1 == 0, n1 == 127, [b_GWn, b_vt], [b_psS] + b_psZ)
            if LOOKAHEAD:
                mA(0)
                for n1 in range(128):
                    if n1 + 1 < 128:
                        mA(n1 + 1)
                    mB(n1)
            else:
                for n1 in range(128):
                    mA(n1)
                    mB(n1)
            for jj in range(2):
                j = 2 * g + jj
                x1, b_x1 = x1g[jj]
                tt(xn, psS[:, jj * 1024:(jj + 1) * 1024], modf[:, 2, :], ALU.mult, [b_psS, b_modf], [b_xn])
                tt(xn, xn, x1, ALU.add, [b_xn, b_x1], [b_xn])
                actf(junk, xn, AF.Square, [b_xn], [b_junk])
                rsum(st[:, 0:1], junk, [b_junk], [b_st])
                rstd_from_ss(0, 1, 2, D, [b_st])
                actf(junk, xn, AF.Copy, [b_xn, b_st], [b_junk], scale=st[:, 2:3])
                tt(junk, junk, gfin, ALU.mult, [b_junk, b_gfin], [b_junk])
                S.dma("sp", out_d[b, j * 128:(j + 1) * 128, :], junk, [b_junk], [])
        ar.release(m5)


_CACHE = {}


def kernel(**inputs):
    nseq = 4
    if "nc" not in _CACHE:
        _CACHE["nc"] = build(nseq)[0]
    nc = _CACHE["nc"]
    f = lambda a: np.ascontiguousarray(np.asarray(a, dtype=np.float32))
    x = f(inputs["x"])
    c = f(inputs["c"])
    shared = {
        "w_ada": f(inputs["w_ada"])[0], "b_ada": f(inputs["b_ada"])[0], "g_norm_mix": f(inputs["g_norm_mix"])[0],
        "w_in": f(inputs["w_in"])[0], "lam_q1": f(inputs["lam_q1"])[0], "lam_k1": f(inputs["lam_k1"])[0],
        "lam_q2": f(inputs["lam_q2"])[0], "lam_k2": f(inputs["lam_k2"])[0], "g_subln": f(inputs["g_subln"])[0],
        "g_kv_norm": f(inputs["g_kv_norm"])[0], "w_uv": f(inputs["w_uv"])[0], "w_out": f(inputs["w_out"])[0],
        "g_norm_ffn": f(inputs["g_norm_ffn"])[0], "w_peer_q": f(inputs["w_peer_q"])[0],
        "peer_keys": f(inputs["peer_keys"])[0], "peer_u": f(inputs["peer_u"])[0], "peer_v": f(inputs["peer_v"])[0],
        "rel_bias": f(inputs["rel_bias"]), "g_final": f(inputs["g_final"]), "bidx": const_bidx(),
    }
    in_maps = []
    for i in range(NCORES):
        m = dict(shared)
        m["x"] = x[i * nseq:(i + 1) * nseq]
        m["c"] = c[i * nseq:(i + 1) * nseq]
        in_maps.append(m)
    res = run_bass_kernel_spmd(nc, in_maps, core_ids=list(range(NCORES)))
    return np.concatenate([r["out"] for r in res.results], axis=0).astype(np.float32)
```

```python
import math
import os
import numpy as np
import concourse.bass as bass
import concourse.mybir as mybir
from concourse.bass_utils import run_bass_kernel_spmd

F32 = mybir.dt.float32
BF16 = mybir.dt.bfloat16
ALU = mybir.AluOpType
AF = mybir.ActivationFunctionType
AX = mybir.AxisListType

D = 1024
SEQ = 2048
NT = SEQ // 128
NCORES = 8
NIT = 22
NEG = -1.0e30
WIN_COLS = 2824


class Buf:
    __slots__ = ("name", "w", "r")

    def __init__(self, name=""):
        self.name = name
        self.w = None
        self.r = {}


class Sched:
    def __init__(self, nc):
        self.nc = nc
        self.eng = {"pe": nc.tensor, "act": nc.scalar, "dve": nc.vector,
                    "pool": nc.gpsimd, "sp": nc.sync}
        self.sem = {}
        self.cnt = {}
        for k in ("pe", "act", "dve", "pool", "d_sp", "d_pool", "d_act"):
            self.sem[k] = nc.alloc_semaphore("s_" + k)
            self.cnt[k] = 0
        self.seen = {k: {} for k in self.eng}
        self.n_ins = 0

    def _deps(self, reads, writes):
        deps = {}
        for b in reads:
            if b.w is not None:
                k, v = b.w
                if v > deps.get(k, 0):
                    deps[k] = v
        for b in writes:
            if b.w is not None:
                k, v = b.w
                if v > deps.get(k, 0):
                    deps[k] = v
            for k, v in b.r.items():
                if v > deps.get(k, 0):
                    deps[k] = v
        return deps

    def _wait(self, e, deps):
        eng = self.eng[e]
        seen = self.seen[e]
        for k, v in deps.items():
            if e == "pe" and k == "pe":
                continue
            if v > seen.get(k, 0):
                eng.wait_ge(self.sem[k], v)
                seen[k] = v
                self.n_ins += 1

    def _mark(self, tok, reads, writes):
        k, v = tok
        for b in writes:
            b.w = tok
            b.r = {}
        for b in reads:
            if b in writes:
                continue
            if v > b.r.get(k, 0):
                b.r[k] = v

    def op(self, e, fn, reads=(), writes=()):
        self._wait(e, self._deps(reads, writes))
        ins = fn(self.eng[e])
        self.cnt[e] += 1
        ins.then_inc(self.sem[e], 1)
        self.n_ins += 1
        self._mark((e, self.cnt[e]), reads, writes)
        return ins

    def dma(self, q, out, in_, reads=(), writes=(), key=None, **kw):
        self._wait(q, self._deps(reads, writes))
        ins = self.eng[q].dma_start(out=out, in_=in_, **kw)
        k = "d_" + q
        if key is not None:
            k = "dk_" + key
            if k not in self.sem:
                self.sem[k] = self.nc.alloc_semaphore("s_" + k)
                self.cnt[k] = 0
        self.cnt[k] += 16
        ins.then_inc(self.sem[k], 16)
        self.n_ins += 1
        self._mark((k, self.cnt[k]), reads, writes)
        return ins

    def barrier(self):
        allv = {k: v for k, v in self.cnt.items() if v > 0}
        for e in self.eng:
            self._wait(e, allv)


class Arena:
    def __init__(self, nc, S, words):
        self.t = nc.alloc_sbuf_tensor("arena", [128, words], F32)
        self.words = words
        self.top = 0
        self.S = S

    def push(self, shape, dtype, name=""):
        n = 1
        for s in shape[1:]:
            n *= s
        nbytes = n * (4 if dtype == F32 else 2)
        w = (nbytes + 3) // 4
        w = (w + 7) // 8 * 8
        assert self.top + w <= self.words, (name, self.top, w, self.words)
        v = self.t[:, self.top:self.top + w]
        if dtype != F32:
            v = v.bitcast(dtype)
        v = v[:, 0:n]
        if len(shape) == 3:
            v = v.rearrange("p (a b) -> p a b", a=shape[1])
        elif len(shape) == 4:
            v = v.rearrange("p (a b c) -> p a b c", a=shape[1], b=shape[2])
        self.top += w
        return v, Buf(name)

    def mark(self):
        return self.top

    def release(self, m):
        self.S.barrier()
        self.top = m


def rel_bucket_np(n):
    max_exact = 16
    nf = np.maximum(n, max_exact).astype(np.float32)
    large = max_exact + (np.log(nf / max_exact) / math.log(128 / max_exact) * 16).astype(np.int32)
    large = np.minimum(large, 31)
    return np.where(n < max_exact, n, large)


def const_bidx():
    t = np.arange(128)[:, None] + 128
    s = np.arange(256)[None, :]
    d = t - s
    b = rel_bucket_np(np.maximum(d, 0)).astype(np.float32)
    return np.where(d >= 0, b, -1.0).astype(np.float32)


class _Stop(Exception):
    pass


def build(nseq=4, dbg=False, stop=None):
    nc_S = []
    try:
        _build(nseq, stop, nc_S)
    except _Stop:
        pass
    nc, S = nc_S
    S.barrier()
    return nc, S


def _build(nseq, stop, nc_S):
    nc = bass.Bass("TRN2", target_bir_lowering=False)

    def din(name, shape):
        return nc.dram_tensor(name, list(shape), F32, kind="ExternalInput").ap()

    x_d = din("x", [nseq, SEQ, D])
    c_d = din("c", [nseq, D])
    w_ada_d = din("w_ada", [D, 6 * D])
    b_ada_d = din("b_ada", [6 * D])
    g_mix_d = din("g_norm_mix", [D])
    w_in_d = din("w_in", [D, 2760])
    lam_d = [din(n, [64]) for n in ("lam_q1", "lam_k1", "lam_q2", "lam_k2")]
    g_subln_d = din("g_subln", [128])
    g_kv_d = din("g_kv_norm", [128])
    w_uv_d = din("w_uv", [4, 128, 128])
    w_out_d = din("w_out", [D, D])
    g_ffn_d = din("g_norm_ffn", [D])
    w_pq_d = din("w_peer_q", [D, D])
    keys_d = din("peer_keys", [8, 2, 128, 64])
    pu_d = din("peer_u", [16384, D])
    pv_d = din("peer_v", [16384, D])
    relb_d = din("rel_bias", [32, 8])
    g_fin_d = din("g_final", [D])
    bidx_d = din("bidx", [128, 256])
    out_d = nc.dram_tensor("out", [nseq, SEQ, D], F32, kind="ExternalOutput").ap()

    def dscr(name, shape, dt):
        return nc.dram_tensor(name, list(shape), dt, kind="Internal").ap()

    w_in_bf = dscr("w_in_bf", [D, WIN_COLS], BF16)
    w_out_bf = dscr("w_out_bf", [D, D], BF16)
    w_pq_bf = dscr("w_pq_bf", [D, D], BF16)
    UT_d = dscr("UT_d", [128, 128, D], BF16)
    Vb_d = dscr("Vb_d", [16384, D], BF16)
    mod_d = dscr("mod_d", [nseq, 6 * D], F32)
    x1_d = dscr("x1_d", [SEQ, D], F32)
    b_w_in_bf, b_w_out_bf, b_w_pq_bf = Buf(), Buf(), Buf()
    b_UT_d, b_Vb_d, b_mod_d, b_x1_d = Buf(), Buf(), Buf(), Buf()

    S = Sched(nc)
    nc_S.extend([nc, S])
    A = nc.alloc_sbuf_tensor

    def cp(n):
        if stop == n:
            raise _Stop()

    def P_(name, shape, dt):
        return A(name, shape, dt), Buf(name)

    ident, b_ident = P_("ident", [128, 128], BF16)
    w_uvb, b_w_uvb = P_("w_uvb", [128, 4, 128], BF16)
    EB, b_EB = P_("EB", [128, 8, 256], F32)
    negm, b_negm = P_("negm", [128, 128], F32)
    gsub, b_gsub = P_("gsub", [128, 128], F32)
    gkv, b_gkv = P_("gkv", [128, 128], F32)
    keysBD, b_keysBD = P_("keysBD", [128, 8, 256], BF16)
    lamt, b_lamt = P_("lamt", [128, 8], F32)
    pow2, b_pow2 = P_("pow2", [128, NIT + 1], F32)
    st, b_st = P_("st", [128, 64], F32)

    psS = nc.alloc_psum_tensor("psS", [128, 2048], F32)
    psX = nc.alloc_psum_tensor("psX", [128, 1024], F32)
    psO = nc.alloc_psum_tensor("psO", [128, 512], F32)
    psT = nc.alloc_psum_tensor("psT", [128, 1024], BF16)
    b_psS, b_psX, b_psO, b_psT = Buf("psS"), Buf("psX"), Buf("psO"), Buf("psT")

    ar = Arena(nc, S, 48600)

    def mm(out, lhsT, rhs, start, stop, reads, writes):
        S.op("pe", lambda e: e.matmul(out, lhsT=lhsT, rhs=rhs, start=start, stop=stop), reads, writes)

    def tr(out, in_, reads, writes):
        S.op("pe", lambda e: e.transpose(out, in_, ident[:in_.shape[0], :in_.shape[0]]),
             list(reads) + [b_ident], writes)

    def actf(out, in_, func, reads, writes, **kw):
        S.op("act", lambda e: e.activation(out=out, in_=in_, func=func, **kw), reads, writes)

    def acopy(out, in_, reads, writes):
        S.op("act", lambda e: e.copy(out=out, in_=in_), reads, writes)

    def vcopy(out, in_, reads, writes):
        S.op("dve", lambda e: e.tensor_copy(out, in_), reads, writes)

    def tt(out, in0, in1, op, reads, writes, eng="dve"):
        S.op(eng, lambda e: e.tensor_tensor(out=out, in0=in0, in1=in1, op=op), reads, writes)

    def ts(out, in0, s1, s2, op0, op1, reads, writes, accum=None):
        if op1 is None:
            S.op("dve", lambda e: e.tensor_scalar(out=out, in0=in0, scalar1=s1, scalar2=None, op0=op0),
                 reads, writes)
        elif accum is None:
            S.op("dve", lambda e: e.tensor_scalar(out=out, in0=in0, scalar1=s1, scalar2=s2, op0=op0, op1=op1),
                 reads, writes)
        else:
            S.op("dve", lambda e: e.tensor_scalar(out=out, in0=in0, scalar1=s1, scalar2=s2, op0=op0, op1=op1,
                                                  accum_out=accum), reads, writes)

    def stt(out, in0, scalar, in1, op0, op1, reads, writes, eng="dve"):
        S.op(eng, lambda e: e.scalar_tensor_tensor(out=out, in0=in0, scalar=scalar, in1=in1, op0=op0, op1=op1),
             reads, writes)

    def rsum(out, in_, reads, writes):
        S.op("dve", lambda e: e.reduce_sum(out=out, in_=in_, axis=AX.X), reads, writes)

    def rmax(out, in_, reads, writes):
        S.op("dve", lambda e: e.reduce_max(out=out, in_=in_, axis=AX.X), reads, writes)

    def rstd_from_ss(ss_col, tmp_col, out_col, n, bufs):
        ts(st[:, tmp_col:tmp_col + 1], st[:, ss_col:ss_col + 1], 1.0 / n, 1e-6, ALU.mult, ALU.add, bufs, bufs)
        S.op("act", lambda e: e.sqrt(out=st[:, tmp_col:tmp_col + 1], in_=st[:, tmp_col:tmp_col + 1]), bufs, bufs)
        S.op("dve", lambda e: e.reciprocal(out=st[:, out_col:out_col + 1], in_=st[:, tmp_col:tmp_col + 1]), bufs, bufs)

    def chunks(n, c=512):
        return [(a, min(n, a + c)) for a in range(0, n, c)]

    evac_flip = [0]

    def evac(out, in_, reads, writes):
        evac_flip[0] ^= 1
        if evac_flip[0]:
            acopy(out, in_, reads, writes)
        else:
            vcopy(out, in_, reads, writes)

    m0 = ar.mark()
    identf, b_identf = ar.push([128, 128], F32, "identf")
    S.op("pool", lambda e: e.memset(identf, 0.0), [], [b_identf])
    S.op("pool", lambda e: e.affine_select(out=identf, in_=identf, pattern=[[-1, 128]], compare_op=ALU.not_equal,
                                           fill=1.0, base=0, channel_multiplier=1), [b_identf], [b_identf])
    vcopy(ident[:], identf, [b_identf], [b_ident])
    S.op("pool", lambda e: e.memset(negm[:], 0.0), [], [b_negm])
    S.op("pool", lambda e: e.affine_select(out=negm[:], in_=negm[:], pattern=[[-1, 128]], compare_op=ALU.is_ge,
                                           fill=NEG, base=0, channel_multiplier=1), [b_negm], [b_negm])
    for k in range(NIT + 1):
        S.op("pool", lambda e: e.memset(pow2[:, k:k + 1], 2.0 ** -(k + 1)), [], [b_pow2])

    cp(1)
    S.dma("sp", gsub[:], g_subln_d.partition_broadcast(128), [], [b_gsub])
    S.dma("sp", gkv[:], g_kv_d.partition_broadcast(128), [], [b_gkv])
    ts(gsub[:], gsub[:], 0.8, None, ALU.mult, None, [b_gsub], [b_gsub])
    lv, b_lv = ar.push([128, 4, 64], F32, "lv")
    for q in range(4):
        S.dma("sp", lv[:, q, :], lam_d[q].partition_broadcast(128), [], [b_lv])
    tt(lv[:, 0, :], lv[:, 0, :], lv[:, 1, :], ALU.mult, [b_lv], [b_lv])
    tt(lv[:, 2, :], lv[:, 2, :], lv[:, 3, :], ALU.mult, [b_lv], [b_lv])
    rsum(lamt[:, 0:1], lv[:, 0, :], [b_lv], [b_lamt])
    rsum(lamt[:, 1:2], lv[:, 2, :], [b_lv], [b_lamt])
    actf(lamt[:, 2:4], lamt[:, 0:2], AF.Exp, [b_lamt], [b_lamt])
    tt(lamt[:, 4:5], lamt[:, 2:3], lamt[:, 3:4], ALU.subtract, [b_lamt], [b_lamt])
    ts(lamt[:, 5:6], lamt[:, 4:5], -1.0, -0.2, ALU.mult, ALU.add, [b_lamt], [b_lamt])

    cp(2)
    tab, b_tab = ar.push([128, 32, 8], F32, "tab")
    ev, b_ev = ar.push([128, 32, 8], F32, "ev")
    bidx, b_bidx = ar.push([128, 256], F32, "bidx")
    tmpe, b_tmpe = ar.push([128, 256], F32, "tmpe")
    S.dma("sp", tab.rearrange("p a b -> p (a b)"), relb_d.rearrange("a b -> (a b)").partition_broadcast(128), [], [b_tab])
    S.dma("sp", bidx, bidx_d, [], [b_bidx])
    tt(ev, tab, tab[:, 31:32, :].to_broadcast([128, 32, 8]), ALU.subtract, [b_tab], [b_ev])
    actf(ev, ev, AF.Exp, [b_ev], [b_ev])
    S.op("pool", lambda e: e.memset(EB[:], 0.0), [], [b_EB])
    for h in range(8):
        for bk in range(32):
            stt(tmpe, bidx, float(bk), ev[:, bk, h:h + 1].to_broadcast([128, 256]), ALU.is_equal, ALU.mult,
                [b_bidx, b_ev], [b_tmpe])
            tt(EB[:, h, :], EB[:, h, :], tmpe, ALU.add, [b_EB, b_tmpe], [b_EB])

    cp(3)
    wuf, b_wuf = ar.push([128, 4, 128], F32, "wuf")
    S.dma("sp", wuf, w_uv_d.rearrange("h c d -> c h d"), [], [b_wuf])
    vcopy(w_uvb[:], wuf, [b_wuf], [b_w_uvb])

    kf, b_kf = ar.push([128, 16, 64], F32, "kf")
    kb, b_kb = ar.push([128, 16, 64], BF16, "kb")
    S.dma("sp", kf, keys_d.rearrange("h p n d -> n (h p) d"), [], [b_kf])
    vcopy(kb, kf, [b_kf], [b_kb])
    S.op("pool", lambda e: e.memset(keysBD[:], 0.0), [], [b_keysBD])
    for h in range(8):
        tr(psT[:, 0:128], kb[:, 2 * h:2 * h + 2, :].rearrange("p a b -> p (a b)"), [b_kb], [b_psT])
        acopy(keysBD[0:64, h, 0:128], psT[0:64, 0:128], [b_psT], [b_keysBD])
        acopy(keysBD[64:128, h, 128:256], psT[64:128, 0:128], [b_psT], [b_keysBD])

    cp(4)
    wst = [ar.push([128, 2760], F32, "wst%d" % i) for i in range(2)]
    wsb = [ar.push([128, 2760], BF16, "wsb%d" % i) for i in range(2)]
    cnt = 0
    for (src, dst, b_dst, ncol) in ((w_in_d, w_in_bf, b_w_in_bf, 2760), (w_out_d, w_out_bf, b_w_out_bf, D),
                                    (w_pq_d, w_pq_bf, b_w_pq_bf, D)):
        for k in range(8):
            (f, bf), (b, bb) = wst[cnt % 2], wsb[cnt % 2]
            cnt += 1
            S.dma("sp", f[:, 0:ncol], src[k * 128:(k + 1) * 128, :], [], [bf])
            evac(b[:, 0:ncol], f[:, 0:ncol], [bf], [bb])
            if ncol == 2760:
                S.dma("sp", dst[k * 128:(k + 1) * 128, 0:2752], b[:, 0:2752], [bb], [b_dst])
                S.dma("sp", dst[k * 128:(k + 1) * 128, 2752:2816], b[:, 2688:2752], [bb], [b_dst])
                S.dma("sp", dst[k * 128:(k + 1) * 128, 2816:2824], b[:, 2752:2760], [bb], [b_dst])
            else:
                S.dma("sp", dst[k * 128:(k + 1) * 128, :], b[:, 0:ncol], [bb], [b_dst])
    ar.release(m0)

    cp(5)
    m0 = ar.mark()
    cT, b_cT = ar.push([128, nseq, 8], F32, "cT")
    cTb, b_cTb = ar.push([128, 8, nseq], BF16, "cTb")
    bada, b_bada = ar.push([nseq, 6 * D], F32, "bada")
    modsb, b_modsb = ar.push([nseq, 6 * D], F32, "modsb")
    for b in range(nseq):
        S.dma("sp", cT[:, b, :], c_d[b].rearrange("(k p) -> p k", p=128), [], [b_cT], allow_slow_non_contiguous=True)
    S.dma("sp", bada[0:nseq, :], b_ada_d.partition_broadcast(nseq), [], [b_bada])
    actf(cT, cT, AF.Silu, [b_cT], [b_cT])
    vcopy(cTb.rearrange("p k b -> p b k"), cT, [b_cT], [b_cTb])
    waf = [ar.push([128, 8, 512], F32, "waf%d" % i) for i in range(2)]
    wab = [ar.push([128, 8, 512], BF16, "wab%d" % i) for i in range(2)]
    w_ada_v = w_ada_d.rearrange("(k p) n -> p k n", p=128)
    for n in range(12):
        (f, bf), (b, bb) = waf[n % 2], wab[n % 2]
        S.dma("sp", f, w_ada_v[:, :, n * 512:(n + 1) * 512], [], [bf])
        evac(b, f, [bf], [bb])
        for k in range(8):
            mm(psX[0:nseq, 0:512], cTb[:, k, :], b[:, k, :], k == 0, k == 7, [b_cTb, bb], [b_psX])
        tt(modsb[0:nseq, n * 512:(n + 1) * 512], psX[0:nseq, 0:512], bada[0:nseq, n * 512:(n + 1) * 512], ALU.add,
           [b_psX, b_bada], [b_modsb])
    S.dma("sp", mod_d, modsb[0:nseq, :], [b_modsb], [b_mod_d])
    ar.release(m0)

    cp(6)
    m0 = ar.mark()
    uf = [ar.push([128, D], F32, "uf%d" % i) for i in range(2)]
    vf = [ar.push([128, D], F32, "vf%d" % i) for i in range(2)]
    ub = [ar.push([128, D], BF16, "ub%d" % i) for i in range(2)]
    uts = [ar.push([128, D], BF16, "uts%d" % i) for i in range(2)]
    vb = [ar.push([128, D], BF16, "vb%d" % i) for i in range(2)]
    import os
    for blk in range(int(os.environ.get('NBLK', '128'))):
        s_ = blk % 2
        S.dma("sp", uf[s_][0], pu_d[blk * 128:(blk + 1) * 128, :], [], [uf[s_][1]])
        S.dma("sp", vf[s_][0], pv_d[blk * 128:(blk + 1) * 128, :], [], [vf[s_][1]])
        vcopy(ub[s_][0], uf[s_][0], [uf[s_][1]], [ub[s_][1]])
        for k in range(8):
            tr(psT[:, k * 128:(k + 1) * 128], ub[s_][0][:, k * 128:(k + 1) * 128], [ub[s_][1]], [b_psT])
        acopy(uts[s_][0], psT[:], [b_psT], [uts[s_][1]])
        S.dma("sp", UT_d[blk], uts[s_][0], [uts[s_][1]], [b_UT_d])
        acopy(vb[s_][0], vf[s_][0], [vf[s_][1]], [vb[s_][1]])
        S.dma("sp", Vb_d[blk * 128:(blk + 1) * 128, :], vb[s_][0], [vb[s_][1]], [b_Vb_d])
    ar.release(m0)

    cp(7)
    w_in_v = w_in_bf.rearrange("(k p) n -> p k n", p=128)
    w_out_v = w_out_bf.rearrange("(k p) n -> p k n", p=128)
    w_pq_v = w_pq_bf.rearrange("(k p) n -> p k n", p=128)

    def transposes_to(dst_fn, src, ntile, reads, writes):
        for g in range(0, ntile, 8):
            n = min(8, ntile - g)
            for q in range(n):
                jj = g + q
                tr(psT[:, q * 128:(q + 1) * 128], src[:, jj * 128:(jj + 1) * 128], reads, [b_psT])
            evac(dst_fn(g, n), psT[:, 0:n * 128], [b_psT], writes)

    for b in range(nseq):
        mseq = ar.mark()
        xin = [ar.push([128, D], F32, "xin%d" % i) for i in range(2)]
        junk, b_junk = ar.push([128, D], F32, "junk")
        mixT, b_mixT = ar.push([128, 8, SEQ], BF16, "mixT")
        hT, b_hT = ar.push([128, 8, SEQ], BF16, "hT")

        m1 = ar.mark()
        moda, b_moda = ar.push([128, 2, D], F32, "moda")
        gtmp, b_gtmp = ar.push([128, D], F32, "gtmp")
        xn, b_xn = ar.push([128, D], F32, "xn")
        hb, b_hb = ar.push([128, D], BF16, "hb")
        S.dma("sp", moda.rearrange("p a b -> p (a b)"), mod_d[b, 0:2 * D].partition_broadcast(128), [b_mod_d], [b_moda])
        S.dma("sp", gtmp, g_mix_d.partition_broadcast(128), [], [b_gtmp])
        stt(moda[:, 1, :], moda[:, 1, :], 1.0, gtmp, ALU.add, ALU.mult, [b_moda, b_gtmp], [b_moda])
        for j in range(NT):
            xt, b_xt = xin[j % 2]
            S.dma("sp", xt, x_d[b, j * 128:(j + 1) * 128, :], [], [b_xt], key="xin%d" % (j % 2))
            actf(junk, xt, AF.Square, [b_xt], [b_junk])
            rsum(st[:, 0:1], junk, [b_junk], [b_st])
            rstd_from_ss(0, 1, 2, D, [b_st])
            actf(xn, xt, AF.Copy, [b_xt, b_st], [b_xn], scale=st[:, 2:3])
            tt(xn, xn, moda[:, 1, :], ALU.mult, [b_xn, b_moda], [b_xn])
            tt(hb, xn, moda[:, 0, :], ALU.add, [b_xn, b_moda], [b_hb])
            for k in range(8):
                tr(psT[:, k * 128:(k + 1) * 128], hb[:, k * 128:(k + 1) * 128], [b_hb], [b_psT])
            evac(hT[:, :, j * 128:(j + 1) * 128], psT[:].rearrange("p (k t) -> p k t", k=8), [b_psT], [b_hT])
        ar.release(m1)

        cp(8)
        m2 = ar.mark()
        sqT, b_sqT = ar.push([128, 4, SEQ], BF16, "sqT")
        iqT, b_iqT = ar.push([128, 4, SEQ], BF16, "iqT")
        ik2, b_ik2 = ar.push([128, SEQ], BF16, "ik2")
        kvT, b_kvT = ar.push([128, SEQ], BF16, "kvT")
        kvA, b_kvA = ar.push([128, NT, 130], BF16, "kvA")
        iws, b_iws = ar.push([128, NT, 8], F32, "iws")
        m2b = ar.mark()
        wsl, b_wsl = ar.push([128, 8, 1288], BF16, "wsl")
        kf32, b_kf32 = ar.push([128, 128], F32, "kf32")
        S.dma("sp", wsl, w_in_v[:, :, 1536:2824], [b_w_in_bf], [b_wsl])
        S.op("pool", lambda e: e.memset(kvA[:, :, 128:130], 1.0), [], [b_kvA])
        for tc in range(0 if os.environ.get('SKIP_A') else 4):
            cs = slice(tc * 512, (tc + 1) * 512)
            for (dst, b_dst, off) in ([(sqT[:, h, cs], b_sqT, h * 128) for h in range(4)] +
                                      [(iqT[:, c, cs], b_iqT, 640 + c * 128) for c in range(4)] +
                                      [(ik2[:, cs], b_ik2, 1152)]):
                for k in range(8):
                    mm(psX[:, 0:512], wsl[:, k, off:off + 128], hT[:, k, cs], k == 0, k == 7, [b_wsl, b_hT], [b_psX])
                evac(dst, psX[:, 0:512], [b_psX], [b_dst])
        kfall, b_kfall = ar.push([128, NT, 128], F32, "kfall")
        for j in range(NT):
            js = slice(j * 128, (j + 1) * 128)
            for k in range(8):
                mm(psO[:, 0:128], hT[:, k, js], wsl[:, k, 512:640], k == 0, k == 7, [b_wsl, b_hT], [b_psO])
            for k in range(8):
                mm(psO[:, 128:264], hT[:, k, js], wsl[:, k, 1152:1288], k == 0, k == 7, [b_wsl, b_hT], [b_psO])
            ts(iws[:, j, :], psO[:, 256:264], (1.0 / 8.0) * 8.0 ** -0.5, None, ALU.mult, None, [b_psO], [b_iws])
            vcopy(kfall[:, j, :], psO[:, 0:128], [b_psO], [b_kfall])
        for hf in range(2):
            actf(junk.rearrange("p (a b) -> p a b", a=8), kfall[:, hf * 8:(hf + 1) * 8, :], AF.Square, [b_kfall], [b_junk])
            rsum(st[:, 40 + hf * 8:48 + hf * 8], junk.rearrange("p (a b) -> p a b", a=8), [b_junk], [b_st])
        ts(st[:, 40:56], st[:, 40:56], 1.0 / 128, 1e-6, ALU.mult, ALU.add, [b_st], [b_st])
        S.op("act", lambda e: e.sqrt(out=st[:, 40:56], in_=st[:, 40:56]), [b_st], [b_st])
        S.op("dve", lambda e: e.reciprocal(out=st[:, 40:56], in_=st[:, 40:56]), [b_st], [b_st])
        for j in range(0 if os.environ.get('SKIP_C') else NT):
            js = slice(j * 128, (j + 1) * 128)
            stt(kvA[:, j, 0:128], kfall[:, j, :], st[:, 40 + j:41 + j], gkv[:], ALU.mult, ALU.mult,
                [b_kfall, b_gkv, b_st], [b_kvA])
            tr(psT[:, 0:128], kvA[:, j, 0:128], [b_kvA], [b_psT])
            evac(kvT[:, js], psT[:, 0:128], [b_psT], [b_kvT])
        ar.release(m2b)
        Pm, b_Pm = ar.push([128, SEQ], F32, "Pm")
        acc, b_acc = ar.push([128, SEQ], F32, "acc")
        Rt, b_Rt = ar.push([128, SEQ], F32, "Rt")
        Rt2, b_Rt2 = ar.push([128, SEQ], F32, "Rt2")
        cntk, b_cntk = ar.push([128, NIT], F32, "cntk")
        Mk, b_Mk = ar.push([128, SEQ], BF16, "Mk")
        Ab, b_Ab = ar.push([128, SEQ], BF16, "Ab")
        AT, b_AT = ar.push([128, NT, 128], BF16, "AT")
        osn, b_osn = ar.push([128, 128], BF16, "osn")
        osT, b_osT = ar.push([128, 128], BF16, "osT")
        steps, b_steps = ar.push([128, NIT + 1], F32, "steps")
        sc_dsa = 128.0 ** -0.5
        import os
        for i in range(int(os.environ.get('NTQ', str(NT)))):
            Si = (i + 1) * 128
            isl = slice(i * 128, (i + 1) * 128)
            for ih in range(8):
                c2, hf = ih // 2, ih % 2
                ps_ = slice(hf * 64, (hf + 1) * 64)
                for (a0, a1) in chunks(Si):
                    mm(psS[:, a0:a1], iqT[ps_, c2, isl], ik2[ps_, a0:a1], True, True, [b_iqT, b_ik2], [b_psS])
                Rq, b_Rq = (Rt, b_Rt) if ih % 2 == 0 else (Rt2, b_Rt2)
                actf(Rq[:, 0:Si], psS[:, 0:Si], AF.Relu, [b_psS], [b_Rq])
                if ih == 0:
                    ts(acc[:, 0:Si], Rq[:, 0:Si], iws[:, i, 0:1], None, ALU.mult, None, [b_Rq, b_iws], [b_acc])
                else:
                    stt(acc[:, 0:Si], Rq[:, 0:Si], iws[:, i, ih:ih + 1], acc[:, 0:Si], ALU.mult, ALU.add,
                        [b_Rq, b_iws, b_acc], [b_acc])
            if i >= 2:
                S.op("dve", lambda e: e.tensor_reduce(out=st[:, 8:9], in_=acc[:, 0:Si], axis=AX.X, op=ALU.min),
                     [b_acc], [b_st])
                rmax(st[:, 9:10], acc[:, 0:Si], [b_acc], [b_st])
                tt(st[:, 10:11], st[:, 9:10], st[:, 8:9], ALU.subtract, [b_st], [b_st])
                ts(steps, pow2[:], st[:, 10:11], None, ALU.mult, None, [b_pow2, b_st], [b_steps])
            tt(acc[:, isl], acc[:, isl], negm[:], ALU.add, [b_acc, b_negm], [b_acc])
            if i >= 2:
                S.op("dve", lambda e: e.memset(cntk, 0.0), [], [b_cntk])
                tt(st[:, 11:12], st[:, 8:9], steps[:, 0:1], ALU.add, [b_st, b_steps], [b_st])
                for k in range(NIT):
                    ts(Rt[:, 0:Si], acc[:, 0:Si], st[:, 11:12], 0.0, ALU.is_ge, ALU.add, [b_acc, b_st], [b_Rt, b_cntk],
                       accum=cntk[:, k:k + 1])
                    stt(st[:, 13:14], cntk[:, k:k + 1], 256.0, steps[:, k:k + 1], ALU.is_ge, ALU.mult,
                        [b_cntk, b_steps], [b_st])
                    stt(st[:, 11:12], st[:, 11:12], steps[:, k + 1:k + 2], st[:, 13:14], ALU.subtract, ALU.add,
                        [b_st, b_steps], [b_st])
                tt(st[:, 8:9], st[:, 11:12], steps[:, NIT:NIT + 1], ALU.subtract, [b_st, b_steps], [b_st])
            else:
                S.op("dve", lambda e: e.memset(st[:, 8:9], -1.0e29), [], [b_st])
            ts(Mk[:, 0:Si], acc[:, 0:Si], st[:, 8:9], None, ALU.is_ge, None, [b_acc, b_st], [b_Mk])
            lo = max(0, (i - 1) * 128)
            for h in range(4):
                for (a0, a1) in chunks(Si):
                    mm(psS[:, a0:a1], sqT[:, h, isl], kvT[:, a0:a1], True, True, [b_sqT, b_kvT], [b_psS])
                rmax(st[:, 16:17], psS[:, 0:Si], [b_psS], [b_st])
                ts(st[:, 17:18], st[:, 16:17], -sc_dsa, None, ALU.mult, None, [b_st], [b_st])
                actf(Pm[:, 0:Si], psS[:, 0:Si], AF.Exp, [b_psS, b_st], [b_Pm], bias=st[:, 17:18], scale=sc_dsa)
                ebv = EB[:, 4 + h, 128:256] if i == 0 else EB[:, 4 + h, 0:256]
                tt(Pm[:, lo:Si], Pm[:, lo:Si], ebv, ALU.mult, [b_Pm, b_EB], [b_Pm])
                tt(Ab[:, 0:Si], Pm[:, 0:Si], Mk[:, 0:Si], ALU.mult, [b_Pm, b_Mk], [b_Ab])
                transposes_to(lambda g, n: AT[:, g:g + n, :].rearrange("p a b -> p (a b)"), Ab, i + 1, [b_Ab], [b_AT])
                for jj in range(i + 1):
                    mm(psO[:, 0:129], AT[:, jj, :], kvA[:, jj, 0:129], jj == 0, jj == i, [b_AT, b_kvA], [b_psO])
                S.op("dve", lambda e: e.reciprocal(out=st[:, 18:19], in_=psO[:, 128:129]), [b_psO], [b_st])
                ts(osn, psO[:, 0:128], st[:, 18:19], None, ALU.mult, None, [b_psO, b_st], [b_osn])
                tr(psT[:, 0:128], osn, [b_osn], [b_psT])
                evac(osT, psT[:, 0:128], [b_psT], [b_osT])
                mm(psX[:, 0:128], w_uvb[:, h, :], osT, True, True, [b_w_uvb, b_osT], [b_psX])
                evac(mixT[:, 4 + h, isl], psX[:, 0:128], [b_psX], [b_mixT])
        ar.release(m2)

        cp(9)
        sc_diff = 64.0 ** -0.5
        m3 = ar.mark()
        wsls = [ar.push([128, 8, 384], BF16, "wsl3_%d" % i) for i in range(2)]
        qT, b_qT = ar.push([128, SEQ], BF16, "qT")
        kT, b_kT = ar.push([128, SEQ], BF16, "kT")
        Vd, b_Vd = ar.push([128, NT, 128], BF16, "Vd")
        Pm0, b_Pm0 = ar.push([128, SEQ], F32, "Pm0")
        Pm1, b_Pm1 = ar.push([128, SEQ], F32, "Pm1")
        Ab, b_Ab = ar.push([128, SEQ], BF16, "Ab3")
        AT, b_AT = ar.push([128, NT, 128], BF16, "AT3")
        of32, b_of32 = ar.push([128, 128], F32, "of32")
        odb, b_odb = ar.push([128, 128], BF16, "odb")
        for h in range(4):
            wsl, b_wsl = wsls[h % 2]
            for q in range(3):
                S.dma("sp", wsl[:, :, q * 128:(q + 1) * 128], w_in_v[:, :, q * 512 + h * 128:q * 512 + (h + 1) * 128],
                      [b_w_in_bf], [b_wsl], key="wsl3_%d" % (h % 2))
            for tc in range(4):
                cs = slice(tc * 512, (tc + 1) * 512)
                for (dst, b_dst, off) in ((qT[:, cs], b_qT, 0), (kT[:, cs], b_kT, 128)):
                    for k in range(8):
                        mm(psX[:, 0:512], wsl[:, k, off:off + 128], hT[:, k, cs], k == 0, k == 7, [b_wsl, b_hT], [b_psX])
                    evac(dst, psX[:, 0:512], [b_psX], [b_dst])
            for j in range(NT):
                js = slice(j * 128, (j + 1) * 128)
                for k in range(8):
                    mm(psO[:, 0:128], hT[:, k, js], wsl[:, k, 256:384], k == 0, k == 7, [b_wsl, b_hT], [b_psO])
                vcopy(Vd[:, j, :], psO[:, 0:128], [b_psO], [b_Vd])
            Pms = ((Pm0, b_Pm0), (Pm1, b_Pm1))
            for i in range(NT):
                Si = (i + 1) * 128
                isl = slice(i * 128, (i + 1) * 128)
                lo = max(0, (i - 1) * 128)
                ebv = EB[:, h, 128:256] if i == 0 else EB[:, h, 0:256]
                for m in range(2):
                    Pmm, b_Pmm = Pms[m]
                    ps_ = slice(m * 64, (m + 1) * 64)
                    for (a0, a1) in chunks(Si):
                        mm(psS[:, a0:a1], qT[ps_, isl], kT[ps_, a0:a1], True, True, [b_qT, b_kT], [b_psS])
                    rmax(st[:, 20 + m:21 + m], psS[:, 0:Si], [b_psS], [b_st])
                    ts(st[:, 22 + m:23 + m], st[:, 20 + m:21 + m], -sc_diff, None, ALU.mult, None, [b_st], [b_st])
                    actf(Pmm[:, 0:Si], psS[:, 0:Si], AF.Exp, [b_psS, b_st], [b_Pmm], bias=st[:, 22 + m:23 + m],
                         scale=sc_diff)
                    tt(Pmm[:, lo:Si], Pmm[:, lo:Si], ebv, ALU.mult, [b_Pmm, b_EB], [b_Pmm])
                    rsum(st[:, 24 + m:25 + m], Pmm[:, 0:Si], [b_Pmm], [b_st])
                    S.op("dve", lambda e: e.reciprocal(out=st[:, 26 + m:27 + m], in_=st[:, 24 + m:25 + m]), [b_st], [b_st])
                tt(st[:, 28:29], st[:, 27:28], lamt[:, 5:6], ALU.mult, [b_st, b_lamt], [b_st])
                actf(Pm0[:, 0:Si], Pm0[:, 0:Si], AF.Copy, [b_Pm0, b_st], [b_Pm0], scale=st[:, 26:27])
                stt(Ab[:, 0:Si], Pm1[:, 0:Si], st[:, 28:29], Pm0[:, 0:Si], ALU.mult, ALU.add, [b_Pm0, b_Pm1, b_st], [b_Ab])
                transposes_to(lambda g, n: AT[:, g:g + n, :].rearrange("p a b -> p (a b)"), Ab, i + 1, [b_Ab], [b_AT])
                for jj in range(i + 1):
                    mm(psO[:, 0:128], AT[:, jj, :], Vd[:, jj, :], jj == 0, jj == i, [b_AT, b_Vd], [b_psO])
                vcopy(of32, psO[:, 0:128], [b_psO], [b_of32])
                actf(junk[:, 0:128], of32, AF.Square, [b_of32], [b_junk])
                rsum(st[:, 30:31], junk[:, 0:128], [b_junk], [b_st])
                rstd_from_ss(30, 31, 32, 128, [b_st])
                stt(odb, of32, st[:, 32:33], gsub[:], ALU.mult, ALU.mult, [b_of32, b_gsub, b_st], [b_odb])
                tr(psT[:, 0:128], odb, [b_odb], [b_psT])
                evac(mixT[:, h, isl], psT[:, 0:128], [b_psT], [b_mixT])
        ar.release(m3)

        cp(10)
        m4 = ar.mark()
        w_outb, b_w_outb = ar.push([128, 8, D], BF16, "w_outb")
        gate_a, b_gate_a = ar.push([128, D], F32, "gate_a")
        x1t = [ar.push([128, D], F32, "x1t%d" % i) for i in range(2)]
        S.dma("sp", w_outb, w_out_v, [b_w_out_bf], [b_w_outb])
        S.dma("sp", gate_a, mod_d[b, 2 * D:3 * D].partition_broadcast(128), [b_mod_d], [b_gate_a])
        for j in range(NT):
            js = slice(j * 128, (j + 1) * 128)
            xt, b_xt = xin[j % 2]
            x1, b_x1 = x1t[j % 2]
            S.dma("sp", xt, x_d[b, js, :], [], [b_xt], key="xin%d" % (j % 2))
            for hf in range(2):
                for ch in range(8):
                    mm(psX[:, hf * 512:(hf + 1) * 512], mixT[:, ch, js], w_outb[:, ch, hf * 512:(hf + 1) * 512],
                       ch == 0, ch == 7, [b_mixT, b_w_outb], [b_psX])
            tt(x1, psX[:], gate_a, ALU.mult, [b_psX, b_gate_a], [b_x1])
            tt(x1, x1, xt, ALU.add, [b_x1, b_xt], [b_x1])
            S.dma("sp", x1_d[js, :], x1, [b_x1], [b_x1_d])
        ar.release(mseq)

        cp(11)
        m5 = ar.mark()
        w_pqb, b_w_pqb = ar.push([128, 8, D], BF16, "w_pqb")
        modf, b_modf = ar.push([128, 3, D], F32, "modf")
        gfin, b_gfin = ar.push([128, D], F32, "gfin")
        junk, b_junk = ar.push([128, D], F32, "junk5")
        x1g = [ar.push([128, D], F32, "x1g%d" % i) for i in range(2)]
        xn, b_xn = ar.push([128, D], F32, "xn5")
        hb, b_hb = ar.push([128, D], BF16, "hb5")
        h2T, b_h2T = ar.push([128, 8, 256], BF16, "h2T")
        pqT, b_pqT = ar.push([128, 8, 256], BF16, "pqT")
        scs, b_scs = ar.push([128, 2048], F32, "scs")
        LOOKAHEAD = os.environ.get("LOOKAHEAD", "1") == "1"
        NB = 3
        ets = [ar.push([128, 8, 128], F32, "et%d" % i) for i in range(NB)]
        Wh = [ar.push([128, 8, 128], BF16, "Wh%d" % i) for i in range(NB)]
        Wcs = [ar.push([128, 8, 128], BF16, "Wc%d" % i) for i in range(2)]
        sms = [ar.push([128, 16], F32, "sm%d" % i) for i in range(2)]
        b_psZ = [Buf("psZ0"), Buf("psZ1")]
        WT, b_WT = ar.push([128, 128, 256], BF16, "WT")
        UTb = [ar.push([128, 8, 128], BF16, "UTb%d" % i) for i in range(3)]
        Vbb = [ar.push([128, D], BF16, "Vbb%d" % i) for i in range(3)]
        Gt = [ar.push([128, 256], F32, "Gt%d" % i) for i in range(2)]
        GW = [ar.push([128, 256], BF16, "GW%d" % i) for i in range(2)]
        s16, b_s16 = ar.push([128, 16, 16], F32, "s16")
        cand, b_cand = ar.push([128, 8, 256], F32, "cand")
        wk, b_wk = ar.push([128, 256], F32, "wk")
        topv, b_topv = ar.push([128, 8, 16], F32, "topv")
        dv_, b_dv = ar.push([128, 8, 16], F32, "dv")
        zz, b_zz = ar.push([128, 16], F32, "zz")
        S.dma("sp", w_pqb, w_pq_v, [b_w_pq_bf], [b_w_pqb])
        S.dma("sp", modf.rearrange("p a b -> p (a b)"), mod_d[b, 3 * D:6 * D].partition_broadcast(128), [b_mod_d], [b_modf])
        S.dma("sp", gfin, g_ffn_d.partition_broadcast(128), [], [b_gfin])
        stt(modf[:, 1, :], modf[:, 1, :], 1.0, gfin, ALU.add, ALU.mult, [b_modf, b_gfin], [b_modf])
        S.dma("sp", gfin, g_fin_d.partition_broadcast(128), [], [b_gfin])
        sc4 = scs.rearrange("p (h q n) -> p h q n", h=8, q=2)
        s4 = s16.rearrange("p (h q) r -> p h q r", q=2)
        for g in range(SEQ // 256):
            for jj in range(2):
                j = 2 * g + jj
                x1, b_x1 = x1g[jj]
                S.dma("sp", x1, x1_d[j * 128:(j + 1) * 128, :], [b_x1_d], [b_x1], key="x1g%d" % jj)
                actf(junk, x1, AF.Square, [b_x1], [b_junk])
                rsum(st[:, 0:1], junk, [b_junk], [b_st])
                rstd_from_ss(0, 1, 2, D, [b_st])
                actf(xn, x1, AF.Copy, [b_x1, b_st], [b_xn], scale=st[:, 2:3])
                tt(xn, xn, modf[:, 1, :], ALU.mult, [b_xn, b_modf], [b_xn])
                tt(hb, xn, modf[:, 0, :], ALU.add, [b_xn, b_modf], [b_hb])
                for k in range(8):
                    tr(psT[:, k * 128:(k + 1) * 128], hb[:, k * 128:(k + 1) * 128], [b_hb], [b_psT])
                evac(h2T[:, :, jj * 128:(jj + 1) * 128], psT[:].rearrange("p (k t) -> p k t", k=8), [b_psT], [b_h2T])
            for hh in range(8):
                for k in range(8):
                    mm(psX[:, 0:256], w_pqb[:, k, hh * 128:(hh + 1) * 128], h2T[:, k, :], k == 0, k == 7,
                       [b_w_pqb, b_h2T], [b_psX])
                evac(pqT[:, hh, :], psX[:, 0:256], [b_psX], [b_pqT])
            def prep_items(jj, via_psO):
                tsl = slice(jj * 128, (jj + 1) * 128)
                sm, b_sm = sms[jj]
                items = []
                if via_psO:
                    def f_sc(qd):
                        for u in range(2):
                            hh = 2 * qd + u
                            mm(psO[:, u * 256:(u + 1) * 256], pqT[:, hh, tsl], keysBD[:, hh, :], True, True,
                               [b_pqT, b_keysBD], [b_psO])
                        vcopy(scs[:, qd * 512:(qd + 1) * 512], psO[:], [b_psO], [b_scs])
                    items += [(lambda qd=qd: f_sc(qd)) for qd in range(4)]
                else:
                    def f_sc_all():
                        for hh in range(8):
                            mm(psS[:, hh * 256:(hh + 1) * 256], pqT[:, hh, tsl], keysBD[:, hh, :], True, True,
                               [b_pqT, b_keysBD], [b_psS] + b_psZ)
                        acopy(scs[:, 0:1024], psS[:, 0:1024], [b_psS], [b_scs])
                        vcopy(scs[:, 1024:2048], psS[:, 1024:2048], [b_psS], [b_scs])
                    items.append(f_sc_all)

                def f_hp(hp):
                    src = scs[:, hp * 128:(hp + 1) * 128]
                    S.op("dve", lambda e: e.max(out=s16[:, hp, 0:8], in_=src), [b_scs], [b_s16])
                    S.op("dve", lambda e: e.match_replace(out=wk[:, 0:128], in_to_replace=s16[:, hp, 0:8],
                                                          in_values=src, imm_value=NEG), [b_scs, b_s16], [b_wk])
                    S.op("dve", lambda e: e.max(out=s16[:, hp, 8:16], in_=wk[:, 0:128]), [b_wk], [b_s16])
                items += [(lambda hp=hp: f_hp(hp)) for hp in range(16)]

                def f_cand():
                    tt(cand.rearrange("p h (i j) -> p h i j", i=16),
                       s4[:, :, 0, :].unsqueeze(3).to_broadcast([128, 8, 16, 16]),
                       s4[:, :, 1, :].unsqueeze(2).to_broadcast([128, 8, 16, 16]), ALU.add, [b_s16], [b_cand])
                items.append(f_cand)

                def f_top(hh):
                    S.op("dve", lambda e: e.max(out=topv[:, hh, 0:8], in_=cand[:, hh, :]), [b_cand], [b_topv])
                    S.op("dve", lambda e: e.match_replace(out=wk[:, 0:256], in_to_replace=topv[:, hh, 0:8],
                                                          in_values=cand[:, hh, :], imm_value=NEG),
                         [b_cand, b_topv], [b_wk])
                    S.op("dve", lambda e: e.max(out=topv[:, hh, 8:16], in_=wk[:, 0:256]), [b_wk], [b_topv])
                items += [(lambda hh=hh: f_top(hh)) for hh in range(8)]

                def f_fin():
                    vcopy(zz[:, 0:8], topv[:, :, 15], [b_topv], [b_zz])
                    tt(dv_, topv, zz[:, 0:8].unsqueeze(2).to_broadcast([128, 8, 16]), ALU.subtract, [b_topv, b_zz], [b_dv])
                    actf(dv_, dv_, AF.Exp, [b_dv], [b_dv])
                    rsum(zz[:, 8:16], dv_, [b_dv], [b_zz])
                    S.op("dve", lambda e: e.reciprocal(out=sm[:, 8:16], in_=zz[:, 8:16]), [b_zz], [b_sm])
                    ts(sm[:, 8:16], sm[:, 8:16], 1.0 - 1.0e-4, None, ALU.mult, None, [b_sm], [b_sm])
                    actf(zz[:, 8:16], zz[:, 8:16], AF.Ln, [b_zz], [b_zz])
                    stt(sm[:, 0:8], zz[:, 8:16], -1.0, zz[:, 0:8], ALU.mult, ALU.subtract, [b_zz], [b_sm])
                items.append(f_fin)
                return items

            def wbuild(jj, queue):
                tsl = slice(jj * 128, (jj + 1) * 128)
                sm, b_sm = sms[jj]
                steps = [(ci, hh) for ci in range(16) for hh in range(8)]
                NSTEP = len(steps)

                def fzp(gi):
                    ci, hh = steps[gi]
                    k2 = gi % 2
                    wr = [b_psZ[k2]] + ([b_psS] if gi < 2 else [])
                    for q in range(2):
                        o_ = psS[:, k2 * 1024 + q * 512:k2 * 1024 + (q + 1) * 512]
                        n0 = ci * 8 + q * 4
                        mm(o_, pqT[:, hh, tsl], keysBD[:, hh, 128:256].unsqueeze(1).to_broadcast([128, 4, 128]), True, False,
                           [b_pqT, b_keysBD], wr)
                        mm(o_, pqT[:, hh, tsl], keysBD[:, hh, n0:n0 + 4].unsqueeze(2).to_broadcast([128, 4, 128]), False, True,
                           [b_pqT, b_keysBD], wr)

                def fe(gi):
                    ci, hh = steps[gi]
                    k2 = gi % 2
                    et, b_et = ets[gi % NB]
                    actf(et.rearrange("p a b -> p (a b)"), psS[:, k2 * 1024:(k2 + 1) * 1024], AF.Exp, [b_psZ[k2], b_sm], [b_et],
                         bias=sm[:, hh:hh + 1])

                def fs(gi):
                    ci, hh = steps[gi]
                    et, b_et = ets[gi % NB]
                    Whh, b_Whh = Wh[gi % NB]
                    stt(Whh, et, sm[:, 8 + hh:9 + hh], et, ALU.is_ge, ALU.mult, [b_et, b_sm], [b_Whh])
                    Whf = Whh.rearrange("p a b -> p (a b)")
                    for q in range(2):
                        mm(psX[:, q * 512:(q + 1) * 512], ident[:], Whf[:, q * 512:(q + 1) * 512], hh == 0, hh == 7,
                           [b_ident, b_Whh], [b_psX])
                    if hh == 7:
                        Wc, b_Wc = Wcs[ci % 2]
                        acopy(Wc.rearrange("p a b -> p (a b)"), psX[:], [b_psX], [b_Wc])
                        for q in range(8):
                            tr(psT[:, q * 128:(q + 1) * 128], Wc[:, q, :], [b_Wc], [b_psT])
                        acopy(WT[:, ci * 8:(ci + 1) * 8, tsl], psT[:].rearrange("p (a b) -> p a b", a=8), [b_psT], [b_WT])
                for sidx in range(NSTEP + 2):
                    if sidx < NSTEP:
                        fzp(sidx)
                    if 0 <= sidx - 1 < NSTEP:
                        fe(sidx - 1)
                    if 0 <= sidx - 2 < NSTEP:
                        fs(sidx - 2)
                    if queue and sidx % 3 == 2:
                        queue.pop(0)()
                while queue:
                    queue.pop(0)()

            for f in prep_items(0, False):
                f()
            wbuild(0, prep_items(1, True) if not os.environ.get("NO_PREP_OVERLAP") else [])
            if os.environ.get("NO_PREP_OVERLAP"):
                for f in prep_items(1, True):
                    f()
            wbuild(1, [])
            def mA(n1):
                (ut, b_ut), (vt, b_vt) = UTb[n1 % 3], Vbb[n1 % 3]
                S.dma("sp", ut.rearrange("p a b -> p (a b)"), UT_d[n1], [b_UT_d], [b_ut], key="ut%d" % (n1 % 3))
                S.dma("sp", vt, Vb_d[n1 * 128:(n1 + 1) * 128, :], [b_Vb_d], [b_vt], key="vt%d" % (n1 % 3))
                pa = psX[:, (n1 % 2) * 512:(n1 % 2) * 512 + 256]
                for k in range(8):
                    mm(pa, ut[:, k, :], h2T[:, k, :], k == 0, k == 7, [b_ut, b_h2T], [b_psX])
                Gn, b_Gn = Gt[n1 % 2]
                actf(Gn, pa, AF.Gelu, [b_psX], [b_Gn])

            def mB(n1):
                (vt, b_vt) = Vbb[n1 % 3]
                Gn, b_Gn = Gt[n1 % 2]
                GWn, b_GWn = GW[n1 % 2]
                tt(GWn, Gn, WT[:, n1, :], ALU.mult, [b_Gn, b_WT], [b_GWn])
                for jj in range(2):
                    for hf in range(2):
                        mm(psS[:, jj * 1024 + hf * 512:jj * 1024 + (hf + 1) * 512], GWn[:, jj * 128:(jj + 1) * 128],
                           vt[:, hf * 512:(hf + 1) * 512], n1 == 0, n1 == 127, [b_GWn, b_vt], [b_psS] + b_psZ)
            if LOOKAHEAD:
                mA(0)
                for n1 in range(128):
                    if n1 + 1 < 128:
                        mA(n1 + 1)
                    mB(n1)
            else:
                for n1 in range(128):
                    mA(n1)
                    mB(n1)
            for jj in range(2):
                j = 2 * g + jj
                x1, b_x1 = x1g[jj]
                tt(xn, psS[:, jj * 1024:(jj + 1) * 1024], modf[:, 2, :], ALU.mult, [b_psS, b_modf], [b_xn])
                tt(xn, xn, x1, ALU.add, [b_xn, b_x1], [b_xn])
                actf(junk, xn, AF.Square, [b_xn], [b_junk])
                rsum(st[:, 0:1], junk, [b_junk], [b_st])
                rstd_from_ss(0, 1, 2, D, [b_st])
                actf(junk, xn, AF.Copy, [b_xn, b_st], [b_junk], scale=st[:, 2:3])
                tt(junk, junk, gfin, ALU.mult, [b_junk, b_gfin], [b_junk])
                S.dma("sp", out_d[b, j * 128:(j + 1) * 128, :], junk, [b_junk], [])
        ar.release(m5)


_CACHE = {}


def kernel(**inputs):
    nseq = 4
    if "nc" not in _CACHE:
        _CACHE["nc"] = build(nseq)[0]
    nc = _CACHE["nc"]
    f = lambda a: np.ascontiguousarray(np.asarray(a, dtype=np.float32))
    x = f(inputs["x"])
    c = f(inputs["c"])
    shared = {
        "w_ada": f(inputs["w_ada"])[0], "b_ada": f(inputs["b_ada"])[0], "g_norm_mix": f(inputs["g_norm_mix"])[0],
        "w_in": f(inputs["w_in"])[0], "lam_q1": f(inputs["lam_q1"])[0], "lam_k1": f(inputs["lam_k1"])[0],
        "lam_q2": f(inputs["lam_q2"])[0], "lam_k2": f(inputs["lam_k2"])[0], "g_subln": f(inputs["g_subln"])[0],
        "g_kv_norm": f(inputs["g_kv_norm"])[0], "w_uv": f(inputs["w_uv"])[0], "w_out": f(inputs["w_out"])[0],
        "g_norm_ffn": f(inputs["g_norm_ffn"])[0], "w_peer_q": f(inputs["w_peer_q"])[0],
        "peer_keys": f(inputs["peer_keys"])[0], "peer_u": f(inputs["peer_u"])[0], "peer_v": f(inputs["peer_v"])[0],
        "rel_bias": f(inputs["rel_bias"]), "g_final": f(inputs["g_final"]), "bidx": const_bidx(),
    }
    in_maps = []
    for i in range(NCORES):
        m = dict(shared)
        m["x"] = x[i * nseq:(i + 1) * nseq]
        m["c"] = c[i * nseq:(i + 1) * nseq]
        in_maps.append(m)
    res = run_bass_kernel_spmd(nc, in_maps, core_ids=list(range(NCORES)))
    return np.concatenate([r["out"] for r in res.results], axis=0).astype(np.float32)
```
